# Optimizing a Trainium2 kernel written in Bass

```python
import math
import jax
import jax.numpy as jnp
from jax import lax
import numpy as np

D_MODEL = 1024
BATCH = 8
SEQ = 4096
DEPTH = 1

D_MIX = D_MODEL
CONV_CH = D_MIX // 2
CONV_GROUPS = 8
CONV_K = 3
N_HEADS = 8
N_KV_HEADS = 2
HEAD_DIM = 64
ATTN_WIDTH = N_HEADS * HEAD_DIM
KV_WIDTH = N_KV_HEADS * HEAD_DIM
IN_WIDTH = 3 * CONV_CH + ATTN_WIDTH + 2 * KV_WIDTH
WINDOW = 128
BLOCK = 128
N_BUCKETS = 32
MAX_DISTANCE = 128
PEER_HEADS = 8
PEER_NKEYS = 128
PEER_EXPERTS = PEER_NKEYS * PEER_NKEYS
PEER_DK = 128
PEER_TOPK = 16
PEER_CHUNK = 128
EPS = 1e-6

kernel_name = 'hymba_conv_swa_peer_adaln_block'


def rms_norm(x, g):
    xf = x.astype(jnp.float32)
    y = xf * lax.rsqrt(jnp.mean(xf * xf, axis=-1, keepdims=True) + EPS)
    return (y * g.astype(jnp.float32)).astype(x.dtype)


def group_rms_norm(y, g, n_groups):
    w = y.shape[-1]
    yg = y.reshape(y.shape[:-1] + (n_groups, w // n_groups))
    return rms_norm(yg, g.reshape(n_groups, w // n_groups)).reshape(y.shape)


def t5_bucket(dist):
    max_exact = N_BUCKETS // 2
    d = jnp.maximum(dist, 1).astype(jnp.float32)
    large = max_exact + (jnp.log(d / max_exact) / math.log(MAX_DISTANCE / max_exact)
                         * (N_BUCKETS - max_exact)).astype(jnp.int32)
    large = jnp.minimum(large, N_BUCKETS - 1)
    return jnp.where(dist < max_exact, dist, large)


def short_conv(b_gate, c_gate, h, conv_w):
    u = c_gate * h
    s = u.shape[1]
    up = jnp.pad(u, ((0, 0), (CONV_K - 1, 0), (0, 0)))
    y = conv_w[0] * up[:, 0:s]
    for j in range(1, CONV_K):
        y = y + conv_w[j] * up[:, j:j + s]
    return b_gate * y


def window_attention(q, k, v, rel_bias, sinks):
    bsz, s = q.shape[0], q.shape[1]
    nb = s // BLOCK
    grp = N_HEADS // N_KV_HEADS
    qb = q.reshape(bsz, nb, BLOCK, N_KV_HEADS, grp, HEAD_DIM)
    kb = k.reshape(bsz, nb, BLOCK, N_KV_HEADS, HEAD_DIM)
    vb = v.reshape(bsz, nb, BLOCK, N_KV_HEADS, HEAD_DIM)

    def with_prev(t):
        prev = jnp.concatenate([jnp.zeros_like(t[:, :1]), t[:, :-1]], axis=1)
        return jnp.concatenate([prev, t], axis=2)

    kw = with_prev(kb)
    vw = with_prev(vb)
    scores = jnp.einsum('bnqkgd,bnjkd->bnkgqj', qb, kw).astype(jnp.float32) * (HEAD_DIM ** -0.5)

    qi = jnp.arange(BLOCK)[:, None]
    kj = jnp.arange(2 * BLOCK)[None, :]
    dist = qi + BLOCK - kj
    bias = rel_bias.astype(jnp.float32)[t5_bucket(jnp.maximum(dist, 0))]
    bias = bias.transpose(2, 0, 1).reshape(N_KV_HEADS, grp, BLOCK, 2 * BLOCK)
    blk = jnp.arange(nb)[:, None, None]
    valid = (dist >= 0) & (dist < WINDOW) & (blk * BLOCK - BLOCK + kj >= 0)
    scores = jnp.where(valid[None, :, None, None], scores + bias, -jnp.inf)

    sink = sinks.astype(jnp.float32).reshape(N_KV_HEADS, grp)[None, None, :, :, None]
    m = jnp.maximum(scores.max(axis=-1), sink)
    p = jnp.exp(scores - m[..., None])
    denom = p.sum(axis=-1) + jnp.exp(sink - m)
    o = jnp.einsum('bnkgqj,bnjkd->bnkgqd', p, vw.astype(jnp.float32)) / denom[..., None]
    o = o.transpose(0, 1, 4, 2, 3, 5).reshape(bsz, s, ATTN_WIDTH)
    return o.astype(q.dtype)


def peer_ffn(h, wq, keys, u_tab, v_tab):
    bsz, s, d = h.shape

    def chunk(ht):
        t = ht.shape[0]
        q = (ht @ wq).reshape(t, PEER_HEADS, 2, PEER_DK)
        sa = jnp.einsum('thd,hnd->thn', q[:, :, 0], keys[0]).astype(jnp.float32)
        sb = jnp.einsum('thd,hnd->thn', q[:, :, 1], keys[1]).astype(jnp.float32)
        va, ia = lax.top_k(sa, PEER_TOPK)
        vb, ib = lax.top_k(sb, PEER_TOPK)
        cand = (va[..., :, None] + vb[..., None, :]).reshape(t, PEER_HEADS, PEER_TOPK * PEER_TOPK)
        cidx = (ia[..., :, None] * PEER_NKEYS + ib[..., None, :]).reshape(t, PEER_HEADS, PEER_TOPK * PEER_TOPK)
        top, pos = lax.top_k(cand, PEER_TOPK)
        eidx = jnp.take_along_axis(cidx, pos, axis=-1)
        g = jax.nn.softmax(top, axis=-1).astype(ht.dtype)
        u = u_tab[eidx]
        v = v_tab[eidx]
        a = jax.nn.gelu(jnp.einsum('thkd,td->thk', u, ht))
        return jnp.einsum('thk,thkd->td', g * a, v)

    out = lax.map(chunk, h.reshape(-1, PEER_CHUNK, d))
    return out.reshape(bsz, s, d)


def setup_inputs(seed: int = 0) -> dict:
    key = jax.random.key(seed)
    ks = jax.random.split(key, 20)
    f32 = jnp.float32
    nrm = lambda k, shp, sc: jax.random.normal(k, shp, f32) * sc
    return {
        'x': nrm(ks[0], (BATCH, SEQ, D_MODEL), 1.0),
        'c': nrm(ks[1], (BATCH, D_MODEL), 1.0),
        'w_ada': nrm(ks[2], (DEPTH, D_MODEL, 6 * D_MODEL), 0.5 * D_MODEL ** -0.5),
        'b_ada': nrm(ks[3], (DEPTH, 6 * D_MODEL), 0.02),
        'norm1_g': 1.0 + nrm(ks[4], (DEPTH, D_MODEL), 0.1),
        'w_in': nrm(ks[5], (DEPTH, D_MODEL, IN_WIDTH), D_MODEL ** -0.5),
        'conv_w': nrm(ks[6], (DEPTH, CONV_K, CONV_CH), CONV_K ** -0.5),
        'q_norm_g': 1.0 + nrm(ks[7], (DEPTH, HEAD_DIM), 0.1),
        'k_norm_g': 1.0 + nrm(ks[8], (DEPTH, HEAD_DIM), 0.1),
        'sinks': nrm(ks[9], (DEPTH, N_HEADS), 0.5),
        'rel_bias': nrm(ks[10], (N_BUCKETS, N_HEADS), 0.5),
        'conv_out_g': 1.0 + nrm(ks[11], (DEPTH, CONV_CH), 0.1),
        'attn_out_g': 1.0 + nrm(ks[12], (DEPTH, ATTN_WIDTH), 0.1),
        'w_out': nrm(ks[13], (DEPTH, D_MIX, D_MODEL), D_MIX ** -0.5),
        'norm2_g': 1.0 + nrm(ks[14], (DEPTH, D_MODEL), 0.1),
        'peer_wq': nrm(ks[15], (DEPTH, D_MODEL, PEER_HEADS * 2 * PEER_DK), D_MODEL ** -0.5),
        'peer_keys': nrm(ks[16], (DEPTH, 2, PEER_HEADS, PEER_NKEYS, PEER_DK), PEER_DK ** -0.5),
        'peer_u': nrm(ks[17], (DEPTH, PEER_EXPERTS, D_MODEL), D_MODEL ** -0.5),
        'peer_v': nrm(ks[18], (DEPTH, PEER_EXPERTS, D_MODEL), 0.5),
    }


def reference(x, c, w_ada, b_ada, norm1_g, w_in, conv_w, q_norm_g, k_norm_g, sinks, rel_bias,
              conv_out_g, attn_out_g, w_out, norm2_g, peer_wq, peer_keys, peer_u, peer_v):
    bsz, s, _ = x.shape
    cond = jax.nn.silu(c)
    splits = [CONV_CH, 2 * CONV_CH, 3 * CONV_CH, 3 * CONV_CH + ATTN_WIDTH,
              3 * CONV_CH + ATTN_WIDTH + KV_WIDTH]
    for l in range(DEPTH):
        mod = cond @ w_ada[l] + b_ada[l]
        sh1, sc1, g1, sh2, sc2, g2 = jnp.split(mod, 6, axis=-1)

        h = rms_norm(x, norm1_g[l]) * (1.0 + sc1[:, None]) + sh1[:, None]
        proj = h @ w_in[l]
        b_gate, c_gate, hc, q, k, v = jnp.split(proj, splits, axis=-1)
        y_conv = short_conv(b_gate, c_gate, hc, conv_w[l])
        q = rms_norm(q.reshape(bsz, s, N_HEADS, HEAD_DIM), q_norm_g[l])
        k = rms_norm(k.reshape(bsz, s, N_KV_HEADS, HEAD_DIM), k_norm_g[l])
        v = v.reshape(bsz, s, N_KV_HEADS, HEAD_DIM)
        y_attn = window_attention(q, k, v, rel_bias, sinks[l])
        mixed = jnp.concatenate([group_rms_norm(y_conv, conv_out_g[l], CONV_GROUPS),
                                 group_rms_norm(y_attn, attn_out_g[l], N_HEADS)], axis=-1)
        x = x + g1[:, None] * (mixed @ w_out[l])

        h2 = rms_norm(x, norm2_g[l]) * (1.0 + sc2[:, None]) + sh2[:, None]
        x = x + g2[:, None] * peer_ffn(h2, peer_wq[l], peer_keys[l], peer_u[l], peer_v[l])
    return x
```

```python
from contextlib import ExitStack
import math
import numpy as np
import concourse.bass as bass
import concourse.mybir as mybir
from concourse.bass_utils import run_bass_kernel_spmd

F32 = mybir.dt.float32
BF16 = mybir.dt.bfloat16
I32 = mybir.dt.int32
U32 = mybir.dt.uint32
AF = mybir.ActivationFunctionType
ALU = mybir.AluOpType
AX = mybir.AxisListType

ENGS = ("pe", "act", "dve", "pool", "sp")
D = 1024
SEQ = 4096
T = 128
EPS = 1e-6
NEG = -30000.0


class Buf:
    __slots__ = ("name", "writer", "readers")

    def __init__(self, name):
        self.name = name
        self.writer = None
        self.readers = []


class _Dummy:
    def then_inc(self, *a, **k):
        return self


class _Spy:
    def __init__(self):
        self.calls = []

    def __getattr__(self, name):
        def f(*a, **kw):
            self.calls.append((name, a, kw))
            return _Dummy()
        return f


def _dt_bytes(dt):
    return 2 if dt == BF16 else 4


def _op_cost(eng, kind, fn):
    spy = _Spy()
    try:
        fn(spy)
    except Exception:
        return (0.3, 0.0)
    if not spy.calls:
        return (0.3, 0.0)
    name, a, kw = spy.calls[-1]
    aps = [v for v in list(a) + list(kw.values()) if hasattr(v, "shape") and hasattr(v, "dtype")]
    elems, nbytes, narrow = 1, 0, True
    for v in aps:
        shp = tuple(v.shape)
        fr = 1
        for d_ in shp[1:]:
            fr *= d_
        elems = max(elems, fr)
        nbytes = max(nbytes, fr * shp[0] * _dt_bytes(v.dtype))
        if v.dtype != BF16:
            narrow = False
    if kind == "dma":
        lat = 2.0 + nbytes / 3.0e5
        if name == "indirect_dma_start":
            return (1.4, lat)
        return ((1.0 if eng == "pool" else 0.15), lat)
    if eng == "pe":
        out = kw.get("out", a[0] if a else None)
        n = 128
        if out is not None and hasattr(out, "shape"):
            n = 1
            for d_ in tuple(out.shape)[1:]:
                n *= d_
        return (0.03 + 0.00042 * n, 0.15)
    if eng == "act":
        return (0.2 + 0.00085 * elems, 0.0)
    if name in ("max", "max_index", "match_replace"):
        return (0.15 + 0.0012 * elems, 0.0)
    return (0.08 + (0.0006 if narrow else 0.00105) * elems, 0.0)


class Prog:
    def __init__(self, nc, es):
        self.nc = nc
        self.es = es
        self.q = {e: [] for e in ENGS}
        self.sems = {}
        self.count = {}
        self.waited = {e: {} for e in ENGS}
        for e in ENGS:
            self._sem("eng_" + e)
        self.rec = None
        self.eng_time = {}

    def record(self, f, *args):
        self.rec = []
        f(*args)
        r, self.rec = self.rec, None
        return r

    def commit(self, item):
        kind, eng, fn, reads, writes, slot = item[:6]
        if kind == "op":
            self.op(eng, fn, reads, writes)
        else:
            self.dma(eng, fn, slot, reads, writes)

    def commit_scheduled(self, ops):
        n = len(ops)
        deps = [set() for _ in range(n)]
        last_w, readers, last_slot = {}, {}, {}
        for i, it in enumerate(ops):
            kind, eng, fn, reads, writes, slot = it[:6]
            for b in reads:
                if id(b) in last_w:
                    deps[i].add(last_w[id(b)])
            for b in writes:
                if id(b) in last_w:
                    deps[i].add(last_w[id(b)])
                for r in readers.get(id(b), ()):
                    deps[i].add(r)
            if kind == "dma":
                if slot in last_slot:
                    deps[i].add(last_slot[slot])
                last_slot[slot] = i
            for b in reads:
                readers.setdefault(id(b), []).append(i)
            for b in writes:
                last_w[id(b)] = i
                readers[id(b)] = []
            deps[i].discard(i)
        users = [[] for _ in range(n)]
        indeg = [0] * n
        for i in range(n):
            indeg[i] = len(deps[i])
            for d_ in deps[i]:
                users[d_].append(i)
        t0 = max(self.eng_time.values()) if self.eng_time else 0.0
        eng_free = {e: max(self.eng_time.get(e, 0.0), t0 - 3.0) for e in ENGS}
        finish = [0.0] * n
        ready_t = [t0 - 3.0] * n
        ready = [i for i in range(n) if indeg[i] == 0]
        done = 0
        while ready:
            best, best_key = None, None
            for i in ready:
                st = max(eng_free[ops[i][1]], ready_t[i])
                key = (st, i)
                if best_key is None or key < best_key:
                    best, best_key = i, key
            i = best
            ready.remove(i)
            eng = ops[i][1]
            occ, lat = ops[i][6]
            st = best_key[0]
            eng_free[eng] = st + occ
            finish[i] = st + occ + lat
            self.commit(ops[i])
            done += 1
            for u in users[i]:
                hop = 0.05 if (ops[u][1] == eng and ops[i][0] == "op") else 0.2
                ready_t[u] = max(ready_t[u], finish[i] + hop)
                indeg[u] -= 1
                if indeg[u] == 0:
                    ready.append(u)
        assert done == n
        self.eng_time = eng_free

    def commit_interleaved(self, a, b):
        ia = ib = 0
        na, nb = len(a), len(b)
        while ia < na or ib < nb:
            if ib >= nb or (ia < na and ia * nb <= ib * na):
                self.commit(a[ia]); ia += 1
            else:
                self.commit(b[ib]); ib += 1

    def _sem(self, key):
        if key not in self.sems:
            self.sems[key] = self.es.enter_context(self.nc.semaphore("s_" + key))
            self.count[key] = 0
        return self.sems[key]

    def sbuf(self, name, shape, dt):
        return self.es.enter_context(self.nc.sbuf_tensor("sb_" + name, list(shape), dt))

    def psum(self, name, shape, dt):
        return self.es.enter_context(self.nc.psum_tensor("ps_" + name, list(shape), dt))

    def _deps(self, eng, reads, writes):
        need = {}
        for b in reads:
            if b.writer is not None:
                k, v = b.writer
                need[k] = max(need.get(k, 0), v)
        for b in writes:
            if b.writer is not None:
                k, v = b.writer
                need[k] = max(need.get(k, 0), v)
            for (k, v) in b.readers:
                need[k] = max(need.get(k, 0), v)
        waits = []
        wd = self.waited[eng]
        for k, v in need.items():
            if eng == "pe" and k == "eng_pe":
                continue
            if wd.get(k, 0) >= v:
                continue
            wd[k] = v
            waits.append((self.sems[k], v))
        return waits

    def _commit(self, ticket, reads, writes):
        for b in reads:
            b.readers.append(ticket)
        for b in writes:
            b.writer = ticket
            b.readers = []

    def op(self, eng, fn, reads=(), writes=()):
        if self.rec is not None:
            self.rec.append(("op", eng, fn, tuple(reads), tuple(writes), None, _op_cost(eng, "op", fn)))
            return None
        waits = self._deps(eng, reads, writes)
        key = "eng_" + eng
        self.count[key] += 1
        ticket = (key, self.count[key])
        self.q[eng].append((waits, fn, (self.sems[key], 1)))
        self._commit(ticket, reads, writes)
        return ticket

    def dma(self, eng, fn, slot, reads=(), writes=()):
        if self.rec is not None:
            self.rec.append(("dma", eng, fn, tuple(reads), tuple(writes), slot, _op_cost(eng, "dma", fn)))
            return None
        waits = self._deps(eng, reads, writes)
        key = "dma_" + slot
        self._sem(key)
        prev = self.count[key]
        if prev > 0 and self.waited[eng].get(key, 0) < prev:
            self.waited[eng][key] = prev
            waits.append((self.sems[key], prev))
        self.count[key] += 16
        ticket = (key, self.count[key])
        self.q[eng].append((waits, fn, (self.sems[key], 16)))
        self._commit(ticket, reads, writes)
        return ticket

    def final_wait(self, eng, bufs):
        waits = self._deps(eng, (), bufs)
        self.q[eng].append((waits, None, None))

    def emit(self):
        nc = self.nc
        q = self.q

        def replay(e, lst):
            for waits, fn, inc in lst:
                for (s, v) in waits:
                    e.wait_ge(s, v)
                if fn is None:
                    continue
                ins = fn(e)
                if inc is not None:
                    ins.then_inc(inc[0], inc[1])

        with nc.Block() as blk:
            @blk.tensor
            def _(e):
                replay(e, q["pe"])

            @blk.scalar
            def _(e):
                replay(e, q["act"])

            @blk.vector
            def _(e):
                replay(e, q["dve"])

            @blk.gpsimd
            def _(e):
                replay(e, q["pool"])

            @blk.sync
            def _(e):
                replay(e, q["sp"])


def build(n_tiles=32, dbg=False, stop_after=None, sched=True):
    nc = bass.Bass("TRN2", target_bir_lowering=False)

    def din(name, shape, dt=F32):
        return nc.dram_tensor(name, list(shape), dt, kind="ExternalInput").ap()

    x_d = din("x", [SEQ, D])
    c_d = din("c_t", [128, 8])
    wada_d = din("w_ada", [D, 6 * D])
    bada_d = din("b_ada", [1, 6 * D])
    n1g_d = din("norm1_g", [1, D])
    n2g_d = din("norm2_g", [1, D])
    win_d = din("w_in", [D, 2304])
    cw_d = din("conv_w_t", [128, 12])
    qg_d = din("qg_t", [128, 1])
    kg_d = din("kg_t", [128, 1])
    sinks_d = din("sinks", [1, 8])
    rb_d = din("rel_bias", [1, 256])
    cg_d = din("conv_g_t", [128, 4])
    ag_d = din("attn_g_t", [128, 4])
    wout_d = din("w_out", [D, D])
    wq_d = din("peer_wq", [D, 2048])
    keysT_d = din("keysT", [128, 16 * 128])
    uv_d = din("uv", [16384, 2048])
    ident_d = din("ident", [128, 128])
    bones_d = din("blockones", [128, 128])
    iota_d = din("iota16", [128, 16])
    ohs_d = din("ohs", [32, 128, 256])
    negm_d = din("negmask", [128, 256])
    y_d = nc.dram_tensor("y", [SEQ, D], F32, kind="ExternalOutput").ap()
    dbg_d = {}

    def dbg_out(name, shape, dt=F32):
        dbg_d[name] = nc.dram_tensor("dbg_" + name, list(shape), dt, kind="ExternalOutput").ap()
        return dbg_d[name]

    with ExitStack() as es:
        P = Prog(nc, es)
        _bufs = {}
        tap_bufs = []
        _regs = {}

        def bc_reg(e):
            if isinstance(e, _Spy):
                return 16383
            if "bc" not in _regs:
                _regs["bc"] = e.to_reg(16383)
            return _regs["bc"]

        def dtap(name, ap, buf, shape, dt=F32):
            if not dbg:
                return
            od = dbg_out(name, shape, dt)
            P.dma("sp", lambda e: e.dma_start(out=od, in_=ap), "dbg_" + name, reads=[buf])
            tap_bufs.append(buf)

        def SB(name, shape, dt=F32):
            t = P.sbuf(name, shape, dt)
            b = Buf(name)
            _bufs[name] = b
            return t, b

        def PS(name, shape, dt=F32):
            t = P.psum(name, shape, dt)
            b = Buf(name)
            return t, b

        ident, b_ident = SB("ident", [128, 128], BF16)
        bones, b_bones = SB("bones", [128, 128], BF16)
        iota16, b_iota = SB("iota16", [128, 16])
        Win, b_Win = SB("Win", [128, 8, 2304], BF16)
        Wk2, b_Wk2 = SB("Wk2", [128, 8, 256], BF16)
        Wout, b_Wout = SB("Wout", [128, 8, 1024], BF16)
        Wq, b_Wq = SB("Wq", [128, 8, 2048], BF16)
        keysT, b_keysT = SB("keysT", [128, 16, 128], BF16)
        bc, b_bc = SB("bc", [128, 4 * D])
        bias_all, b_bias = SB("bias_all", [128, 8, 256])
        cw, b_cw = SB("cw", [128, 12])
        qg, b_qg = SB("qg", [128, 1])
        kg, b_kg = SB("kg", [128, 1])
        cg, b_cg = SB("cg", [128, 4])
        ag, b_ag = SB("ag", [128, 4])
        sink_b, b_sink = SB("sink_b", [128, 8])
        rb_b, b_rb = SB("rb_b", [128, 256])
        c_t, b_ct = SB("c_t", [128, 8])
        cond, b_cond = SB("cond", [128, 8])
        brow = [SB("brow%d" % i, [1, 128]) for i in range(2)]
        mrow = [SB("mrow%d" % i, [1, 128]) for i in range(2)]
        ones_row, b_ones = SB("ones_row", [1, 128])

        tmpf, b_tmpf = SB("tmpf", [128, D])
        g1_tmp, b_g1tmp = tmpf, b_tmpf
        NG = 7
        gb = [SB("gb%d" % i, [128, 2048], BF16) for i in range(NG)]
        xs = [SB("xs%d" % i, [128, D]) for i in range(2)]
        stg = xs
        pr, b_pr = SB("pr", [128, 18, 128])
        h2bs = [SB("h2b%d" % i, [128, D], BF16) for i in range(2)]
        g2_tmp, b_g2tmp = pr[:].rearrange("p c n -> p (c n)")[:, 0:D], b_pr
        uvb_d = nc.dram_tensor("uvb", [16384, 2048], BF16, kind="Internal").ap()
        b_uvb = Buf("uvb")

        PA, b_PA = PS("PA", [128, 512])
        PB, b_PB = PS("PB", [128, 512])
        PC, b_PC = PS("PC", [128, 512])
        PD, b_PD = PS("PD", [128, 512])
        PT, b_PT = PS("PT", [128, 1024], BF16)
        PT2, b_PT2 = PT, b_PT
        acc0, b_acc0 = PS("acc0", [128, 512])
        acc1, b_acc1 = PS("acc1", [128, 512])
        PSc, b_PSc0 = PS("PSc", [128, 2, 256])
        b_PSc = [b_PSc0, b_PSc0]
        PO, b_PO = PD, b_PD

        win_v = win_d.rearrange("(kc p) n -> p kc n", p=128)
        wout_v = wout_d.rearrange("(kc p) n -> p kc n", p=128)
        wq_v = wq_d.rearrange("(kc p) n -> p kc n", p=128)
        wada_v = wada_d.rearrange("(kc p) n -> p kc n", p=128)

        def ld(eng, out_ap, in_ap, slot, wbuf):
            P.dma(eng, lambda e: e.dma_start(out=out_ap, in_=in_ap), slot, writes=[wbuf])

        ld("pool", ident[:], ident_d, "c0", b_ident)
        ld("pool", bones[:], bones_d, "c1", b_bones)
        ld("sp", iota16[:], iota_d, "c2", b_iota)
        ld("sp", cw[:], cw_d, "c3", b_cw)
        ld("sp", qg[:], qg_d, "c4", b_qg)
        ld("sp", kg[:], kg_d, "c5", b_kg)
        ld("sp", cg[:], cg_d, "c6", b_cg)
        ld("sp", ag[:], ag_d, "c7", b_ag)
        ld("sp", c_t[:], c_d, "c8", b_ct)
        ld("sp", sink_b[:], sinks_d.partition_broadcast(128), "c9", b_sink)
        ld("sp", rb_b[:], rb_d.partition_broadcast(128), "c10", b_rb)
        for kc in range(8):
            ld("pool", Win[:, kc, :], win_v[:, kc, :], "w%d" % (kc % 4), b_Win)
        for kv in range(2):
            for r in range(2):
                ld("pool", Wk2[:, :, kv * 128 + r * 64: kv * 128 + r * 64 + 64],
                   win_v[:, :, 2048 + kv * 64: 2048 + kv * 64 + 64], "w%d" % (kv * 2 + r), b_Wk2)
        for kc in range(8):
            ld("pool", Wout[:, kc, :], wout_v[:, kc, :], "w%d" % (kc % 4), b_Wout)
        for kc in range(8):
            ld("pool", Wq[:, kc, :], wq_v[:, kc, :], "w%d" % (kc % 4), b_Wq)
        ld("pool", keysT[:].rearrange("p c n -> p (c n)"), keysT_d, "w0", b_keysT)
        CR = 256
        for cidx in range(16384 // CR):
            P.dma("pool", lambda e, cidx=cidx: e.dma_start(
                out=uvb_d[cidx * CR:(cidx + 1) * CR, 0:D], in_=uv_d[cidx * CR:(cidx + 1) * CR, 0:D]),
                "cv%d" % (cidx % 4), writes=[b_uvb])

        P.op("act", lambda e: e.activation(out=cond[:], in_=c_t[:], func=AF.Silu),
             reads=[b_ct], writes=[b_cond])
        P.op("dve", lambda e: e.memset(ones_row[:], 1.0), writes=[b_ones])
        CW = 128
        NCH = 6 * D // CW
        for ch in range(NCH):
            g_t, g_b = stg[ch % 2]
            wv = g_t[:].rearrange("p (k n) -> p k n", k=8)
            ld("sp", wv, wada_v[:, :, ch * CW:(ch + 1) * CW], "ada%d" % (ch % 2), g_b)
            br_t, br_b = brow[ch % 2]
            mr_t, mr_b = mrow[ch % 2]
            ld("sp", br_t[:, 0:CW], bada_d[:, ch * CW:(ch + 1) * CW], "bada%d" % (ch % 2), br_b)
            pbank, pbuf = (PA, b_PA) if ch % 2 == 0 else (PB, b_PB)
            for kc in range(8):
                P.op("pe", lambda e, kc=kc, wv=wv, pbank=pbank: e.matmul(
                    pbank[0:1, 0:CW], lhsT=cond[:, kc:kc + 1], rhs=wv[:, kc, :],
                    start=(kc == 0), stop=(kc == 7)),
                    reads=[b_cond, g_b], writes=[pbuf])
            P.op("dve", lambda e, pbank=pbank, br_t=br_t, mr_t=mr_t: e.tensor_tensor(
                out=mr_t[:, 0:CW], in0=pbank[0:1, 0:CW], in1=br_t[:, 0:CW], op=ALU.add),
                reads=[pbuf, br_b], writes=[mr_b])
            pbank2, pbuf2 = (PC, b_PC) if ch % 2 == 0 else (PD, b_PD)
            P.op("pe", lambda e, pbank2=pbank2, mr_t=mr_t: e.matmul(
                pbank2[:, 0:CW], lhsT=ones_row[0:1, :], rhs=mr_t[0:1, 0:CW],
                start=True, stop=True), reads=[b_ones, mr_b], writes=[pbuf2])
            col0 = ch * CW
            if col0 < 2 * D:
                dst = bc[:, col0:col0 + CW]
                dbuf = b_bc
            elif col0 < 3 * D:
                dst = g1_tmp[:, col0 - 2 * D:col0 - 2 * D + CW]
                dbuf = b_g1tmp
            elif col0 < 5 * D:
                dst = bc[:, col0 - D:col0 - D + CW]
                dbuf = b_bc
            else:
                dst = g2_tmp[:, col0 - 5 * D:col0 - 5 * D + CW]
                dbuf = b_g2tmp
            P.op("act", lambda e, pbank2=pbank2, dst=dst: e.copy(out=dst, in_=pbank2[:, 0:CW]),
                 reads=[pbuf2], writes=[dbuf])
        sh1_b, sc1_b = bc[:, 0:D], bc[:, D:2 * D]
        sh2_b, sc2_b = bc[:, 2 * D:3 * D], bc[:, 3 * D:4 * D]
        for kc in range(8):
            P.op("dve", lambda e, kc=kc: e.tensor_tensor(
                out=Wout[:, kc, :], in0=Wout[:, kc, :], in1=g1_tmp[:, 0:D], op=ALU.mult),
                reads=[b_Wout, b_g1tmp], writes=[b_Wout])
        for (sc_ap, ng_d, slot) in ((sc1_b, n1g_d, 0), (sc2_b, n2g_d, 1)):
            g_t, g_b = stg[slot]
            ld("sp", g_t[:, 0:D], ng_d.partition_broadcast(128), "ng%d" % slot, g_b)
            P.op("dve", lambda e, sc_ap=sc_ap, g_t=g_t: e.scalar_tensor_tensor(
                out=sc_ap, in0=sc_ap, scalar=1.0, in1=g_t[:, 0:D], op0=ALU.add, op1=ALU.mult),
                reads=[b_bc, g_b], writes=[b_bc])
        P.op("dve", lambda e: e.tensor_scalar(out=qg[:], in0=qg[:], scalar1=0.125, scalar2=None,
                                              op0=ALU.mult), reads=[b_qg], writes=[b_qg])

        for cidx in range(128):
            s_t, s_b = stg[cidx % 2]
            o_t, o_b = h2bs[cidx % 2]
            ld("sp", s_t[:], uv_d[cidx * 128:(cidx + 1) * 128, D:2 * D], "vc%d" % (cidx % 2), s_b)
            P.op("dve", lambda e, s_t=s_t, o_t=o_t: e.tensor_tensor(out=o_t[:], in0=s_t[:], in1=g2_tmp, op=ALU.mult),
                 reads=[s_b, b_g2tmp], writes=[o_b])
            P.dma("sp", lambda e, cidx=cidx, o_t=o_t: e.dma_start(
                out=uvb_d[cidx * 128:(cidx + 1) * 128, D:2 * D], in_=o_t[:]), "vo%d" % (cidx % 2),
                reads=[o_b], writes=[b_uvb])

        P.op("dve", lambda e: e.memset(bias_all[:], 0.0), writes=[b_bias])
        for b in range(32):
            g_t, g_b = stg[b % 2]
            ld("sp", g_t[:, 0:256], ohs_d[b], "oh%d" % (b % 2), g_b)
            for h in range(8):
                P.op("dve", lambda e, b=b, h=h, g_t=g_t: e.scalar_tensor_tensor(
                    out=bias_all[:, h, :], in0=g_t[:, 0:256], scalar=rb_b[:, b * 8 + h: b * 8 + h + 1],
                    in1=bias_all[:, h, :], op0=ALU.mult, op1=ALU.add),
                    reads=[g_b, b_rb, b_bias], writes=[b_bias])
        g_t, g_b = stg[0]
        ld("sp", g_t[:, 0:256], negm_d, "oh0", g_b)
        for h in range(8):
            P.op("dve", lambda e, h=h, g_t=g_t: e.tensor_add(
                out=bias_all[:, h, :], in0=bias_all[:, h, :], in1=g_t[:, 0:256]),
                reads=[g_b, b_bias], writes=[b_bias])

        jh, b_jh = SB("jh", [128, 2 * D], BF16)
        junkb, b_junkb = jh[:, 0:D], b_jh
        hb, b_hb = jh[:, D:2 * D], b_jh
        hT, b_hT = SB("hT", [128, 8, 128], BF16)
        a2, b_a2 = SB("a2", [128, 128])
        ga2, b_ga2 = SB("ga2", [128, 128])
        a2_bufs = [Buf("a2_%d" % i) for i in range(8)]
        ga2_bufs = [Buf("ga2_%d" % i) for i in range(8)]
        wgtfs = [SB("wgtf%d" % i, [128, 128]) for i in range(2)]
        dg = [SB("dg%d" % i, [128, 128], BF16) for i in range(4)]
        st1, b_st1 = SB("st1", [128, 4])
        vbuf = [SB("vbuf%d" % i, [128, 128], BF16) for i in range(2)]
        kTb = [SB("kTb%d" % i, [128, 2, 128], BF16) for i in range(2)]
        _ub = SB("ub", [128, 4, 130])
        ub = [_ub, _ub]
        ucar, b_ucar = SB("ucar", [128, 4, 2])
        yc, b_yc = SB("yc", [128, 4, 128])
        sqb, b_sqb = SB("sqb", [128, 6, 128], BF16)
        rst, b_rst = SB("rst", [128, 6, 128])
        mixT, b_mixT = hT, b_hT
        qT, b_qT = SB("qT", [128, 4, 128], BF16)
        s_sb = [SB("s_sb%d" % i, [128, 256]) for i in range(2)]
        p_sb = [SB("p_sb%d" % i, [128, 256], BF16) for i in range(2)]
        pT_sb = [SB("pT_sb%d" % i, [128, 2, 128], BF16) for i in range(2)]
        hst = [SB("hst%d" % i, [128, 8]) for i in range(2)]
        rden, b_rden = SB("rden", [128, 8])
        on, b_on = yc[:].rearrange("p c (a b) -> p (c a) b", b=64), b_yc
        onb, b_onb = SB("onb", [128, 512], BF16)
        ssa, b_ssa = SB("ssa", [128, 8])
        qTp, b_qTp = jh[:].rearrange("p (c n) -> p c n", c=16), b_jh
        S, b_S = pr[:, 0:16, :], b_pr
        _s2 = SB("S2_0", [128, 128])
        S2 = [_s2, _s2]
        V1, b_V1 = SB("V1", [128, 16, 16])
        I1, b_I1 = SB("I1", [128, 16, 16], U32)
        I1f, b_I1f = SB("I1f", [128, 16, 16])
        rstf = rst[:].rearrange("p c n -> p (c n)")
        ycf = yc[:].rearrange("p c n -> p (c n)")
        onf = on[:].rearrange("p c n -> p (c n)")
        cand = [(rstf[:, 0:256].rearrange("p (a b) -> p a b", a=16), b_rst),
                (rstf[:, 256:512].rearrange("p (a b) -> p a b", a=16), b_rst)]
        cand2 = [(rstf[:, 512:768], b_rst), (ycf[:, 0:256], b_yc)]
        T2, b_T2 = SB("T2", [128, 8, 16])
        pos, b_pos = SB("pos", [128, 8, 16], U32)
        k2u, b_k2u = SB("k2u", [128, 8, 16], U32)
        k1f, b_k1f = SB("k1f", [128, 8, 16])
        k2f, b_k2f = SB("k2f", [128, 8, 16])
        eq = [(ycf[:, 256:512].rearrange("p (a b) -> p a b", a=16), b_yc),
              (onf[:, 0:256].rearrange("p (a b) -> p a b", a=16), b_on)]
        ia_s, b_ia_s = SB("ia_s", [128, 8, 16])
        ib_s, b_ib_s = SB("ib_s", [128, 8, 16])
        eidf, b_eidf = s_sb[0][0][:, 0:128], s_sb[0][1]
        eidis = [SB("eidi%d" % i, [128, 128], I32) for i in range(2)]
        wgt, b_wgt = s_sb[1][0][:, 128:256].rearrange("p (h k) -> p h k", h=8), s_sb[1][1]
        wsum, b_wsum = SB("wsum", [128, 8])
        negt, b_negt = SB("negt", [128, 8])
        a_sb, b_a = s_sb[0][0][:, 128:256], s_sb[0][1]
        ga_sb, b_ga = s_sb[1][0][:, 0:128], s_sb[1][1]
        acc, b_acc = tmpf, b_tmpf

        def rms_stats(src_ap, src_buf, n_free, out_col_ap, out_buf):
            P.op("act", lambda e: e.activation(out=junkb[:, 0:n_free], in_=src_ap, func=AF.Square,
                                               accum_out=out_col_ap),
                 reads=[src_buf], writes=[b_junkb, out_buf])
            rsqrt_inplace(out_col_ap, out_buf, 1.0 / n_free)

        def rsqrt_inplace(ap, buf, scale, src_ap=None, src_buf=None):
            s_ap = ap if src_ap is None else src_ap
            rd = [buf] if src_buf is None else [src_buf]
            P.op("dve", lambda e: e.tensor_scalar(out=ap, in0=s_ap, scalar1=scale, scalar2=EPS,
                                                  op0=ALU.mult, op1=ALU.add), reads=rd, writes=[buf])
            P.op("act", lambda e: e.activation(out=ap, in_=ap, func=AF.Ln), reads=[buf], writes=[buf])
            P.op("act", lambda e: e.activation(out=ap, in_=ap, func=AF.Exp, scale=-0.5), reads=[buf], writes=[buf])

        def transpose8(src, src_buf, dst, dst_buf, nchunk=8, ptile=None, pbuf=None):
            ptile = PT if ptile is None else ptile
            pbuf = b_PT if pbuf is None else pbuf
            for kc in range(nchunk):
                P.op("pe", lambda e, kc=kc: e.transpose(
                    out=ptile[:, kc * 128:(kc + 1) * 128], in_=src[:, kc * 128:(kc + 1) * 128],
                    identity=ident[:]), reads=[src_buf, b_ident], writes=[pbuf])
            if dst is not None:
                P.op("act", lambda e: e.copy(out=dst[:].rearrange("p c n -> p (c n)"),
                                             in_=ptile[:, 0:nchunk * 128]),
                     reads=[pbuf], writes=[dst_buf])

        out_bufs = []

        def front(n):
            xt, b_xt = xs[n % 2]
            x1, b_x1 = xt, b_xt
            cur, prv = n % 2, (n - 1) % 2
            h2b, b_h2b = h2bs[cur]
            eidi, b_eidi = eidis[cur]
            wgtf, b_wgtf = wgtfs[cur]
            ld("sp", xt[:], x_d[n * T:(n + 1) * T, :], "x%d" % cur, b_xt)

            rms_stats(xt[:], b_xt, D, st1[:, 0:1], b_st1)
            P.op("dve", lambda e, xt=xt: e.scalar_tensor_tensor(
                out=tmpf[:], in0=xt[:], scalar=st1[:, 0:1], in1=sc1_b, op0=ALU.mult, op1=ALU.mult),
                reads=[b_xt, b_st1, b_bc], writes=[b_tmpf])
            P.op("dve", lambda e: e.tensor_add(out=hb[:], in0=tmpf[:], in1=sh1_b),
                 reads=[b_tmpf, b_bc], writes=[b_hb])
            if n == 0:
                dtap("hb", hb[:], b_hb, [128, D], BF16)
                dtap("bc", bc[:], b_bc, [128, 4 * D])
            transpose8(hb, b_hb, hT, b_hT)
            if n == 0:
                dtap("hT", hT[:], b_hT, [128, 8, 128], BF16)

            def wcols(j):
                if j < 16:
                    return Win, b_Win, j * 128
                return Wk2, b_Wk2, (j - 16) * 128
            for g in range(5):
                pbank, pbuf = (PA, b_PA) if g % 2 == 0 else (PB, b_PB)
                chunks = list(range(g * 4, min(g * 4 + 4, 18)))
                for i, j in enumerate(chunks):
                    wt, wb, c0 = wcols(j)
                    for kc in range(8):
                        P.op("pe", lambda e, i=i, kc=kc, wt=wt, c0=c0, pbank=pbank: e.matmul(
                            pbank[:, i * 128:(i + 1) * 128], lhsT=wt[:, kc, c0:c0 + 128], rhs=hT[:, kc, :],
                            start=(kc == 0), stop=(kc == 7)), reads=[wb, b_hT], writes=[pbuf])
                nn = len(chunks)
                P.op("act", lambda e, g=g, nn=nn, pbank=pbank: e.copy(
                    out=pr[:, g * 4:g * 4 + nn, :].rearrange("p c n -> p (c n)"), in_=pbank[:, 0:nn * 128]),
                    reads=[pbuf], writes=[b_pr])
            vt, b_vt = vbuf[cur]
            for kc in range(8):
                P.op("pe", lambda e, kc=kc: e.matmul(
                    PC[:, 0:128], lhsT=hT[:, kc, :], rhs=Win[:, kc, 2176:2304],
                    start=(kc == 0), stop=(kc == 7)), reads=[b_Win, b_hT], writes=[b_PC])
            P.op("act", lambda e, vt=vt: e.copy(out=vt[:], in_=PC[:, 0:128]), reads=[b_PC], writes=[b_vt])

            if n == 0:
                dtap("pr", pr[:], b_pr, [128, 18, 128])
                dtap("vt", vt[:], b_vt, [128, 128], BF16)
            ut, b_ut = ub[cur]
            upt, b_upt = ub[prv]
            if n == 0:
                P.op("dve", lambda e, ut=ut: e.memset(ut[:, :, 0:2], 0.0), writes=[b_ut])
            else:
                P.op("dve", lambda e, ut=ut: e.tensor_copy(out=ut[:, :, 0:2], in_=ucar[:]),
                     reads=[b_ucar], writes=[b_ut])
            P.op("dve", lambda e, ut=ut: e.tensor_tensor(
                out=ut[:, :, 2:130], in0=pr[:, 4:8, :], in1=pr[:, 8:12, :], op=ALU.mult),
                reads=[b_pr], writes=[b_ut])
            for j in range(4):
                P.op("dve", lambda e, j=j, ut=ut: e.tensor_scalar(
                    out=yc[:, j, :], in0=ut[:, j, 2:130], scalar1=cw[:, j * 3 + 2:j * 3 + 3], scalar2=None,
                    op0=ALU.mult), reads=[b_ut, b_cw], writes=[b_yc])
                for tap in (1, 0):
                    P.op("dve", lambda e, j=j, tap=tap, ut=ut: e.scalar_tensor_tensor(
                        out=yc[:, j, :], in0=ut[:, j, tap:tap + 128], scalar=cw[:, j * 3 + tap:j * 3 + tap + 1],
                        in1=yc[:, j, :], op0=ALU.mult, op1=ALU.add), reads=[b_ut, b_cw, b_yc], writes=[b_yc])
            P.op("dve", lambda e: e.tensor_tensor(out=yc[:], in0=yc[:], in1=pr[:, 0:4, :], op=ALU.mult),
                 reads=[b_yc, b_pr], writes=[b_yc])
            P.op("dve", lambda e, ut=ut: e.tensor_copy(out=ucar[:], in_=ut[:, :, 128:130]),
                 reads=[b_ut], writes=[b_ucar])
            P.op("act", lambda e: e.activation(out=sqb[:, 0:4, :], in_=yc[:], func=AF.Square),
                 reads=[b_yc], writes=[b_sqb])
            for j in range(4):
                P.op("pe", lambda e, j=j: e.matmul(PD[:, j * 128:(j + 1) * 128], lhsT=bones[:], rhs=sqb[:, j, :],
                                                   start=True, stop=True), reads=[b_bones, b_sqb], writes=[b_PD])
            rsqrt_inplace(rst[:, 0:4, :].rearrange("p c n -> p (c n)"), b_rst, 1.0 / 64,
                          src_ap=PD[:, 0:512], src_buf=b_PD)
            for j in range(4):
                P.op("dve", lambda e, j=j: e.scalar_tensor_tensor(
                    out=mixT[:, j, :], in0=yc[:, j, :], scalar=cg[:, j:j + 1], in1=rst[:, j, :],
                    op0=ALU.mult, op1=ALU.mult), reads=[b_yc, b_cg, b_rst], writes=[b_mixT])

            P.op("act", lambda e: e.activation(out=sqb[:], in_=pr[:, 12:18, :], func=AF.Square),
                 reads=[b_pr], writes=[b_sqb])
            for j in range(4):
                P.op("pe", lambda e, j=j: e.matmul(PD[:, j * 128:(j + 1) * 128], lhsT=bones[:], rhs=sqb[:, j, :],
                                                   start=True, stop=True), reads=[b_bones, b_sqb], writes=[b_PD])
            rsqrt_inplace(rst[:, 0:4, :].rearrange("p c n -> p (c n)"), b_rst, 1.0 / 64,
                          src_ap=PD[:, 0:512], src_buf=b_PD)
            for j in range(2):
                P.op("pe", lambda e, j=j: e.matmul(PC[:, j * 128:(j + 1) * 128], lhsT=bones[:], rhs=sqb[:, 4 + j, :],
                                                   start=True, stop=True), reads=[b_bones, b_sqb], writes=[b_PC])
            rsqrt_inplace(rst[:, 4:6, :].rearrange("p c n -> p (c n)"), b_rst, 1.0 / 64,
                          src_ap=PC[:, 0:256], src_buf=b_PC)
            for j in range(4):
                P.op("dve", lambda e, j=j: e.scalar_tensor_tensor(
                    out=qT[:, j, :], in0=pr[:, 12 + j, :], scalar=qg[:, 0:1], in1=rst[:, j, :],
                    op0=ALU.mult, op1=ALU.mult), reads=[b_pr, b_qg, b_rst], writes=[b_qT])
            kt, b_kt = kTb[cur]
            kpt, b_kpt = kTb[prv]
            for j in range(2):
                P.op("dve", lambda e, j=j, kt=kt: e.scalar_tensor_tensor(
                    out=kt[:, j, :], in0=pr[:, 16 + j, :], scalar=kg[:, 0:1], in1=rst[:, 4 + j, :],
                    op0=ALU.mult, op1=ALU.mult), reads=[b_pr, b_kg, b_rst], writes=[b_kt])

            if n == 0:
                dtap("yc", yc[:], b_yc, [128, 4, 128])
                dtap("mixT_conv", mixT[:, 0:4, :], b_mixT, [128, 4, 128], BF16)
                dtap("qT", qT[:], b_qT, [128, 4, 128], BF16)
                dtap("kT", kt[:], b_kt, [128, 2, 128], BF16)
            vpt, b_vpt = vbuf[prv]
            for h in range(8):
                kv, r, qc = h // 4, h % 2, h // 2
                sl = h % 2
                pl, ph = 64 * r, 64 * r + 64
                st_, b_s = s_sb[sl]
                pt_, b_p = p_sb[sl]
                pTt, b_pTt = pT_sb[sl]
                hs, b_hs = hst[sl]
                c_lo = 0 if n > 0 else 128
                if n > 0:
                    P.op("pe", lambda e, sl=sl, qc=qc, kv=kv, pl=pl, ph=ph, kpt=kpt: e.matmul(
                        PSc[:, sl, 0:128], lhsT=qT[pl:ph, qc, :], rhs=kpt[pl:ph, kv, :], start=True, stop=True),
                        reads=[b_qT, b_kpt], writes=[b_PSc[sl]])
                P.op("pe", lambda e, sl=sl, qc=qc, kv=kv, pl=pl, ph=ph, kt=kt: e.matmul(
                    PSc[:, sl, 128:256], lhsT=qT[pl:ph, qc, :], rhs=kt[pl:ph, kv, :], start=True, stop=True),
                    reads=[b_qT, b_kt], writes=[b_PSc[sl]])
                P.op("dve", lambda e, sl=sl, h=h, st_=st_, c_lo=c_lo: e.tensor_tensor(
                    out=st_[:, c_lo:256], in0=PSc[:, sl, c_lo:256], in1=bias_all[:, h, c_lo:256], op=ALU.add),
                    reads=[b_PSc[sl], b_bias], writes=[b_s])
                P.op("dve", lambda e, st_=st_, hs=hs, c_lo=c_lo: e.reduce_max(
                    out=hs[:, 0:1], in_=st_[:, c_lo:256], axis=AX.X), reads=[b_s], writes=[b_hs])
                P.op("dve", lambda e, hs=hs, h=h: e.tensor_scalar(
                    out=hs[:, 1:2], in0=hs[:, 0:1], scalar1=sink_b[:, h:h + 1], scalar2=-1.0,
                    op0=ALU.max, op1=ALU.mult), reads=[b_hs, b_sink], writes=[b_hs])
                P.op("act", lambda e, st_=st_, pt_=pt_, hs=hs, c_lo=c_lo: e.activation(
                    out=pt_[:, c_lo:256], in_=st_[:, c_lo:256], func=AF.Exp, bias=hs[:, 1:2],
                    accum_out=hs[:, 2:3]), reads=[b_s, b_hs], writes=[b_p, b_hs])
                P.op("act", lambda e, hs=hs, h=h: e.activation(
                    out=hs[:, 3:4], in_=sink_b[:, h:h + 1], func=AF.Exp, bias=hs[:, 1:2]),
                    reads=[b_sink, b_hs], writes=[b_hs])
                P.op("dve", lambda e, hs=hs: e.tensor_add(out=hs[:, 4:5], in0=hs[:, 2:3], in1=hs[:, 3:4]),
                     reads=[b_hs], writes=[b_hs])
                P.op("dve", lambda e, hs=hs, h=h: e.reciprocal(out=rden[:, h:h + 1], in_=hs[:, 4:5]),
                     reads=[b_hs], writes=[b_rden])
                halves = (0, 1) if n > 0 else (1,)
                for hf in halves:
                    P.op("pe", lambda e, hf=hf, sl=sl, pt_=pt_: e.transpose(
                        out=PT2[:, (sl * 2 + hf) * 128:(sl * 2 + hf + 1) * 128], in_=pt_[:, hf * 128:(hf + 1) * 128],
                        identity=ident[:]), reads=[b_p, b_ident], writes=[b_PT2])
                lo = 0 if n > 0 else 1
                P.op("act", lambda e, sl=sl, pTt=pTt, lo=lo: e.copy(
                    out=pTt[:, lo:2, :].rearrange("p c n -> p (c n)"),
                    in_=PT2[:, (sl * 2 + lo) * 128:(sl * 2 + 2) * 128]), reads=[b_PT2], writes=[b_pTt])
                if n > 0:
                    P.op("pe", lambda e, h=h, kv=kv, pTt=pTt, vpt=vpt: e.matmul(
                        PO[:, h * 64:(h + 1) * 64], lhsT=pTt[:, 0, :], rhs=vpt[:, kv * 64:(kv + 1) * 64],
                        start=True, stop=False), reads=[b_pTt, b_vpt], writes=[b_PO])
                P.op("pe", lambda e, h=h, kv=kv, pTt=pTt, vt=vt, first=(n == 0): e.matmul(
                    PO[:, h * 64:(h + 1) * 64], lhsT=pTt[:, 1, :], rhs=vt[:, kv * 64:(kv + 1) * 64],
                    start=first, stop=True), reads=[b_pTt, b_vt], writes=[b_PO])
            for h in range(8):
                P.op("dve", lambda e, h=h: e.tensor_scalar(
                    out=on[:, h, :], in0=PO[:, h * 64:(h + 1) * 64], scalar1=rden[:, h:h + 1], scalar2=None,
                    op0=ALU.mult), reads=[b_PO, b_rden], writes=[b_on])
            for h in range(8):
                P.op("act", lambda e, h=h: e.activation(
                    out=junkb[:, 0:64], in_=on[:, h, :], func=AF.Square, accum_out=ssa[:, h:h + 1]),
                    reads=[b_on], writes=[b_junkb, b_ssa])
            rsqrt_inplace(ssa[:], b_ssa, 1.0 / 64)
            for h in range(8):
                P.op("dve", lambda e, h=h: e.tensor_scalar(
                    out=onb[:, h * 64:(h + 1) * 64], in0=on[:, h, :], scalar1=ssa[:, h:h + 1], scalar2=None,
                    op0=ALU.mult), reads=[b_on, b_ssa], writes=[b_onb])
            transpose8(onb, b_onb, None, None, nchunk=4)
            for j in range(4):
                P.op("dve", lambda e, j=j: e.tensor_scalar(
                    out=mixT[:, 4 + j, :], in0=PT[:, j * 128:(j + 1) * 128], scalar1=ag[:, j:j + 1], scalar2=None,
                    op0=ALU.mult), reads=[b_PT, b_ag], writes=[b_mixT])

            if n == 0:
                dtap("on", on[:], b_on, [128, 8, 64])
                dtap("rden", rden[:], b_rden, [128, 8])
                dtap("mixT", mixT[:], b_mixT, [128, 8, 128], BF16)
            for half in range(2):
                pbank, pbuf = (PA, b_PA) if half == 0 else (PB, b_PB)
                for c in range(8):
                    P.op("pe", lambda e, c=c, half=half, pbank=pbank: e.matmul(
                        pbank[:, :], lhsT=mixT[:, c, :], rhs=Wout[:, c, half * 512:(half + 1) * 512],
                        start=(c == 0), stop=(c == 7)), reads=[b_mixT, b_Wout], writes=[pbuf])
                P.op("dve", lambda e, half=half, pbank=pbank, xt=xt: e.tensor_tensor(
                    out=x1[:, half * 512:(half + 1) * 512], in0=pbank[:, :],
                    in1=xt[:, half * 512:(half + 1) * 512], op=ALU.add),
                    reads=[pbuf, b_xt], writes=[b_x1])

            rms_stats(x1[:], b_x1, D, st1[:, 1:2], b_st1)
            P.op("dve", lambda e: e.scalar_tensor_tensor(
                out=tmpf[:], in0=x1[:], scalar=st1[:, 1:2], in1=sc2_b, op0=ALU.mult, op1=ALU.mult),
                reads=[b_x1, b_st1, b_bc], writes=[b_tmpf])
            P.op("dve", lambda e: e.tensor_add(out=h2b[:], in0=tmpf[:], in1=sh2_b),
                 reads=[b_tmpf, b_bc], writes=[b_h2b])
            transpose8(h2b, b_h2b, hT, b_hT)

            for g in range(4):
                pbank, pbuf = (PA, b_PA) if g % 2 == 0 else (PB, b_PB)
                for i in range(4):
                    c = g * 4 + i
                    for kc in range(8):
                        P.op("pe", lambda e, i=i, c=c, kc=kc, pbank=pbank: e.matmul(
                            pbank[:, i * 128:(i + 1) * 128], lhsT=Wq[:, kc, c * 128:(c + 1) * 128], rhs=hT[:, kc, :],
                            start=(kc == 0), stop=(kc == 7)), reads=[b_Wq, b_hT], writes=[pbuf])
                P.op("act", lambda e, g=g, pbank=pbank: e.copy(
                    out=qTp[:, g * 4:(g + 1) * 4, :].rearrange("p c n -> p (c n)"), in_=pbank[:, :]),
                    reads=[pbuf], writes=[b_qTp])
            for g in range(4):
                pbank, pbuf = (PC, b_PC) if g % 2 == 0 else (PD, b_PD)
                for i in range(4):
                    c = g * 4 + i
                    P.op("pe", lambda e, i=i, c=c, pbank=pbank: e.matmul(
                        pbank[:, i * 128:(i + 1) * 128], lhsT=qTp[:, c, :], rhs=keysT[:, c, :],
                        start=True, stop=True), reads=[b_qTp, b_keysT], writes=[pbuf])
                P.op("act", lambda e, g=g, pbank=pbank: e.copy(
                    out=S[:, g * 4:(g + 1) * 4, :].rearrange("p c n -> p (c n)"), in_=pbank[:, :]),
                    reads=[pbuf], writes=[b_S])

            for c in range(16):
                s2t, b_s2 = S2[c % 2]
                P.op("dve", lambda e, c=c: e.max(out=V1[:, c, 0:8], in_=S[:, c, :]), reads=[b_S], writes=[b_V1])
                P.op("dve", lambda e, c=c: e.max_index(out=I1[:, c, 0:8], in_max=V1[:, c, 0:8], in_values=S[:, c, :]),
                     reads=[b_S, b_V1], writes=[b_I1])
                P.op("dve", lambda e, c=c, s2t=s2t: e.match_replace(
                    out=s2t[:], in_to_replace=V1[:, c, 0:8], in_values=S[:, c, :], imm_value=-1e30),
                    reads=[b_S, b_V1], writes=[b_s2])
                P.op("dve", lambda e, c=c, s2t=s2t: e.max(out=V1[:, c, 8:16], in_=s2t[:]), reads=[b_s2], writes=[b_V1])
                P.op("dve", lambda e, c=c, s2t=s2t: e.max_index(
                    out=I1[:, c, 8:16], in_max=V1[:, c, 8:16], in_values=s2t[:]),
                    reads=[b_s2, b_V1], writes=[b_I1])
            P.op("dve", lambda e: e.tensor_copy(out=I1f[:], in_=I1[:]), reads=[b_I1], writes=[b_I1f])

            for h in range(8):
                ct, b_c = cand[h % 2]
                c2t, b_c2 = cand2[h % 2]
                P.op("dve", lambda e, h=h, ct=ct: e.tensor_tensor(
                    out=ct[:], in0=V1[:, 2 * h, :].unsqueeze(2).to_broadcast([128, 16, 16]),
                    in1=V1[:, 2 * h + 1, :].unsqueeze(1).to_broadcast([128, 16, 16]), op=ALU.add),
                    reads=[b_V1], writes=[b_c])
                cf = ct[:].rearrange("p a b -> p (a b)")
                P.op("dve", lambda e, h=h, cf=cf: e.max(out=T2[:, h, 0:8], in_=cf), reads=[b_c], writes=[b_T2])
                P.op("dve", lambda e, h=h, cf=cf: e.max_index(out=pos[:, h, 0:8], in_max=T2[:, h, 0:8], in_values=cf),
                     reads=[b_c, b_T2], writes=[b_pos])
                P.op("dve", lambda e, h=h, cf=cf, c2t=c2t: e.match_replace(
                    out=c2t[:], in_to_replace=T2[:, h, 0:8], in_values=cf, imm_value=-1e30),
                    reads=[b_c, b_T2], writes=[b_c2])
                P.op("dve", lambda e, h=h, c2t=c2t: e.max(out=T2[:, h, 8:16], in_=c2t[:]), reads=[b_c2], writes=[b_T2])
                P.op("dve", lambda e, h=h, c2t=c2t: e.max_index(
                    out=pos[:, h, 8:16], in_max=T2[:, h, 8:16], in_values=c2t[:]),
                    reads=[b_c2, b_T2], writes=[b_pos])
            P.op("dve", lambda e: e.tensor_single_scalar(out=k2u[:], in_=pos[:], scalar=15, op=ALU.bitwise_and),
                 reads=[b_pos], writes=[b_k2u])
            P.op("dve", lambda e: e.tensor_single_scalar(out=pos[:], in_=pos[:], scalar=4, op=ALU.logical_shift_right),
                 reads=[b_pos], writes=[b_pos])
            P.op("dve", lambda e: e.tensor_copy(out=k1f[:], in_=pos[:]), reads=[b_pos], writes=[b_k1f])
            P.op("dve", lambda e: e.tensor_copy(out=k2f[:], in_=k2u[:]), reads=[b_k2u], writes=[b_k2f])
            cnt = 0
            for h in range(8):
                for side, (kf, b_kf, dst, b_dst) in enumerate(((k1f, b_k1f, ia_s, b_ia_s), (k2f, b_k2f, ib_s, b_ib_s))):
                    et, b_e = eq[cnt % 2]
                    cnt += 1
                    P.op("dve", lambda e, h=h, kf=kf, et=et: e.tensor_tensor(
                        out=et[:], in0=iota16[:, :].unsqueeze(1).to_broadcast([128, 16, 16]),
                        in1=kf[:, h, :].unsqueeze(2).to_broadcast([128, 16, 16]), op=ALU.is_equal),
                        reads=[b_iota, b_kf], writes=[b_e])
                    P.op("dve", lambda e, h=h, side=side, et=et: e.tensor_tensor(
                        out=et[:], in0=et[:],
                        in1=I1f[:, 2 * h + side, :].unsqueeze(1).to_broadcast([128, 16, 16]), op=ALU.mult),
                        reads=[b_e, b_I1f], writes=[b_e])
                    P.op("dve", lambda e, h=h, dst=dst, et=et: e.reduce_sum(
                        out=dst[:, h, :], in_=et[:], axis=AX.X), reads=[b_e], writes=[b_dst])
            P.op("dve", lambda e: e.scalar_tensor_tensor(
                out=eidf[:], in0=ia_s[:].rearrange("p h k -> p (h k)"), scalar=128.0,
                in1=ib_s[:].rearrange("p h k -> p (h k)"), op0=ALU.mult, op1=ALU.add),
                reads=[b_ia_s, b_ib_s], writes=[b_eidf])
            P.op("dve", lambda e: e.tensor_copy(out=eidi[:], in_=eidf[:]), reads=[b_eidf], writes=[b_eidi])
            P.op("dve", lambda e: e.tensor_scalar(out=negt[:], in0=T2[:, :, 0], scalar1=-1.0, scalar2=None,
                                                  op0=ALU.mult), reads=[b_T2], writes=[b_negt])
            for h in range(8):
                P.op("act", lambda e, h=h: e.activation(
                    out=wgt[:, h, :], in_=T2[:, h, :], func=AF.Exp, bias=negt[:, h:h + 1],
                    accum_out=wsum[:, h:h + 1]), reads=[b_T2, b_negt], writes=[b_wgt, b_wsum])
            P.op("dve", lambda e: e.reciprocal(out=wsum[:], in_=wsum[:]), reads=[b_wsum], writes=[b_wsum])
            for h in range(8):
                P.op("dve", lambda e, h=h: e.tensor_scalar(
                    out=wgtf[:, h * 16:(h + 1) * 16], in0=wgt[:, h, :], scalar1=wsum[:, h:h + 1], scalar2=None,
                    op0=ALU.mult), reads=[b_wgt, b_wsum], writes=[b_wgtf])

            if dbg and n == 0:
                for nm, t_, b_, shp, dt_ in (("eidi", eidi, b_eidi, [128, 128], I32),
                                             ("wgt", wgtf, b_wgtf, [128, 128], F32),
                                             ("h2f", h2b, b_h2b, [128, D], BF16),
                                             ("S", S, b_S, [128, 16, 128], F32)):
                    od = dbg_out(nm, shp, dt_)
                    P.dma("sp", lambda e, od=od, t_=t_: e.dma_start(out=od, in_=t_[:]), "dbg_" + nm, reads=[b_])
                    out_bufs.append(b_)

        def back(n):
            xt, b_xt = xs[n % 2]
            cur = n % 2
            h2b, b_h2b = h2bs[cur]
            eidi, b_eidi = eidis[cur]
            wgtf, b_wgtf = wgtfs[cur]
            LAG = 2

            def stage_a(k):
                g_t, g_b = gb[k % NG]
                b_a2 = a2_bufs[k % 8]
                b_ga2 = ga2_bufs[k % 8]
                P.dma("pool", lambda e, k=k, g_t=g_t: e.indirect_dma_start(
                    out=g_t[:], out_offset=None, in_=uvb_d,
                    in_offset=bass.IndirectOffsetOnAxis(ap=eidi[:, k:k + 1], axis=0),
                    bounds_check=bc_reg(e), oob_is_err=False), "g%d" % (k % NG),
                    reads=[b_eidi, b_uvb], writes=[g_b])
                P.op("dve", lambda e, k=k, g_t=g_t: e.tensor_tensor(
                    out=g_t[:, 0:D], in0=g_t[:, 0:D], in1=h2b[:], op=ALU.mult),
                    reads=[g_b, b_h2b], writes=[g_b])
                P.op("act", lambda e, k=k, g_t=g_t: e.activation(
                    out=g_t[:, 0:D], in_=g_t[:, 0:D], func=AF.Copy, accum_out=a2[:, k:k + 1]),
                    reads=[g_b], writes=[g_b, b_a2])
                P.op("act", lambda e, k=k: e.activation(
                    out=ga2[:, k:k + 1], in_=a2[:, k:k + 1], func=AF.Gelu_apprx_tanh),
                    reads=[b_a2], writes=[b_ga2])

            def stage_b(k):
                g_t, g_b = gb[k % NG]
                d_t, d_b = dg[k % 4]
                b_ga2 = ga2_bufs[k % 8]
                P.op("dve", lambda e, k=k, d_t=d_t: e.tensor_scalar(
                    out=d_t[:], in0=ident[:], scalar1=ga2[:, k:k + 1], scalar2=wgtf[:, k:k + 1],
                    op0=ALU.mult, op1=ALU.mult), reads=[b_ident, b_ga2, b_wgtf], writes=[d_b])
                for half, (ap_, ab_) in enumerate(((acc0, b_acc0), (acc1, b_acc1))):
                    P.op("pe", lambda e, k=k, half=half, ap_=ap_, d_t=d_t, g_t=g_t: e.matmul(
                        ap_[:, :], lhsT=d_t[:], rhs=g_t[:, D + half * 512:D + (half + 1) * 512],
                        start=(k == 0), stop=(k == 127)), reads=[d_b, g_b], writes=[ab_])

            for k in range(128 + LAG):
                if k < 128:
                    stage_a(k)
                if k >= LAG:
                    stage_b(k - LAG)
            for half, (ap_, ab_) in enumerate(((acc0, b_acc0), (acc1, b_acc1))):
                P.op("dve", lambda e, half=half, ap_=ap_: e.tensor_tensor(
                    out=xt[:, half * 512:(half + 1) * 512], in0=ap_[:, :],
                    in1=xt[:, half * 512:(half + 1) * 512], op=ALU.add),
                    reads=[ab_, b_xt], writes=[b_xt])
            P.dma("sp", lambda e: e.dma_start(out=y_d[n * T:(n + 1) * T, :], in_=xt[:]),
                  "y%d" % cur, reads=[b_xt])
            out_bufs.append(b_xt)

        rec_f = P.record(front, 0)
        if sched:
            P.commit_scheduled(rec_f)
        else:
            for it in rec_f:
                P.commit(it)
        for n in range(n_tiles):
            rec_b = P.record(back, n)
            rec_f = P.record(front, n + 1) if n + 1 < n_tiles else []
            if sched:
                P.commit_scheduled(rec_b + rec_f)
            else:
                P.commit_interleaved(rec_b, rec_f)

        P.final_wait("sp", out_bufs + tap_bufs)
        P.emit()
    return nc, list(dbg_d.keys())


def _t5_bucket_static():
    qi = np.arange(128)[:, None]
    kj = np.arange(256)[None, :]
    dist = qi + 128 - kj
    valid = (dist >= 0) & (dist < 128)
    d0 = np.maximum(dist, 0)
    max_exact = 16
    dd = np.maximum(d0, 1).astype(np.float32)
    large = max_exact + (np.log(dd / np.float32(max_exact)) / np.float32(math.log(128 / max_exact))
                         * np.float32(32 - max_exact)).astype(np.int32)
    large = np.minimum(large, 31)
    bucket = np.where(d0 < max_exact, d0, large)
    ohs = np.zeros((32, 128, 256), np.float32)
    for b in range(32):
        ohs[b] = ((bucket == b) & valid).astype(np.float32)
    negmask = np.where(valid, 0.0, NEG).astype(np.float32)
    return ohs, negmask


def _host_layout(inp, n_tok=SEQ):
    f = lambda a: np.ascontiguousarray(np.asarray(a, dtype=np.float32))
    ohs, negmask = _t5_bucket_static()
    bones = np.zeros((128, 128), np.float32)
    bones[:64, :64] = 1.0
    bones[64:, 64:] = 1.0
    keys = f(inp["peer_keys"])[0]
    keysT = np.ascontiguousarray(keys.transpose(3, 1, 0, 2)).reshape(128, 16 * 128)
    uv = np.ascontiguousarray(np.concatenate([f(inp["peer_u"])[0], f(inp["peer_v"])[0]], axis=1))
    shared = {
        "w_ada": f(inp["w_ada"])[0],
        "b_ada": f(inp["b_ada"]).reshape(1, 6 * D),
        "norm1_g": f(inp["norm1_g"]).reshape(1, D),
        "norm2_g": f(inp["norm2_g"]).reshape(1, D),
        "w_in": f(inp["w_in"])[0],
        "conv_w_t": np.ascontiguousarray(f(inp["conv_w"])[0].reshape(3, 4, 128).transpose(2, 1, 0)).reshape(128, 12),
        "qg_t": np.ascontiguousarray(np.tile(f(inp["q_norm_g"])[0], 2).reshape(128, 1)),
        "kg_t": np.ascontiguousarray(np.tile(f(inp["k_norm_g"])[0], 2).reshape(128, 1)),
        "sinks": f(inp["sinks"]).reshape(1, 8),
        "rel_bias": f(inp["rel_bias"]).reshape(1, 256),
        "conv_g_t": np.ascontiguousarray(f(inp["conv_out_g"])[0].reshape(4, 128).T),
        "attn_g_t": np.ascontiguousarray(f(inp["attn_out_g"])[0].reshape(4, 128).T),
        "w_out": f(inp["w_out"])[0],
        "peer_wq": f(inp["peer_wq"])[0],
        "keysT": keysT,
        "uv": uv,
        "ident": np.eye(128, dtype=np.float32),
        "blockones": bones,
        "iota16": np.ascontiguousarray(np.tile(np.arange(16, dtype=np.float32), (128, 1))),
        "ohs": ohs,
        "negmask": negmask,
    }
    x = f(inp["x"])
    c = f(inp["c"])
    maps = []
    for b in range(x.shape[0]):
        m = dict(shared)
        m["x"] = np.ascontiguousarray(x[b, :SEQ])
        m["c_t"] = np.ascontiguousarray(c[b].reshape(8, 128).T)
        maps.append(m)
    return maps


def kernel(**inputs):
    maps = _host_layout(inputs)
    nc, _ = build(n_tiles=SEQ // T)
    res = run_bass_kernel_spmd(nc, maps, core_ids=list(range(8)))
    out = np.stack([np.asarray(r["y"], dtype=np.float32) for r in res.results], axis=0)
    return out
```

```python
from contextlib import ExitStack
import math
import numpy as np
import concourse.bass as bass
import concourse.mybir as mybir
from concourse.bass_utils import run_bass_kernel_spmd

F32 = mybir.dt.float32
BF16 = mybir.dt.bfloat16
I32 = mybir.dt.int32
U32 = mybir.dt.uint32
AF = mybir.ActivationFunctionType
ALU = mybir.AluOpType
AX = mybir.AxisListType

ENGS = ("pe", "act", "dve", "pool", "sp")
D = 1024
SEQ = 4096
T = 128
EPS = 1e-6
NEG = -30000.0


class Buf:
    __slots__ = ("name", "writer", "readers")

    def __init__(self, name):
        self.name = name
        self.writer = None
        self.readers = []


class _Dummy:
    def then_inc(self, *a, **k):
        return self


class _Spy:
    def __init__(self):
        self.calls = []

    def __getattr__(self, name):
        def f(*a, **kw):
            self.calls.append((name, a, kw))
            return _Dummy()
        return f


def _dt_bytes(dt):
    return 2 if dt == BF16 else 4


def _op_cost(eng, kind, fn):
    spy = _Spy()
    try:
        fn(spy)
    except Exception:
        return (0.3, 0.0)
    if not spy.calls:
        return (0.3, 0.0)
    name, a, kw = spy.calls[-1]
    aps = [v for v in list(a) + list(kw.values()) if hasattr(v, "shape") and hasattr(v, "dtype")]
    elems, nbytes, narrow = 1, 0, True
    for v in aps:
        shp = tuple(v.shape)
        fr = 1
        for d_ in shp[1:]:
            fr *= d_
        elems = max(elems, fr)
        nbytes = max(nbytes, fr * shp[0] * _dt_bytes(v.dtype))
        if v.dtype != BF16:
            narrow = False
    if kind == "dma":
        o = kw.get("out", a[0] if a else None)
        if o is not None and hasattr(o, "shape"):
            nbytes = _dt_bytes(o.dtype)
            for d_ in tuple(o.shape):
                nbytes *= d_
        lat = 2.0 + nbytes / 3.0e5
        if name == "indirect_dma_start":
            return (1.4, lat)
        return ((1.0 if eng == "pool" else 0.15), lat)
    if eng == "pe":
        out = kw.get("out", a[0] if a else None)
        n = 128
        if out is not None and hasattr(out, "shape"):
            n = 1
            for d_ in tuple(out.shape)[1:]:
                n *= d_
        return (0.03 + 0.00042 * n, 0.15)
    if eng == "act":
        return (0.2 + 0.00085 * elems, 0.0)
    if name in ("max", "max_index", "match_replace"):
        return (0.15 + 0.0012 * elems, 0.0)
    return (0.08 + (0.0006 if narrow else 0.00105) * elems, 0.0)


class Prog:
    def __init__(self, nc, es):
        self.nc = nc
        self.es = es
        self.q = {e: [] for e in ENGS}
        self.sems = {}
        self.count = {}
        self.waited = {e: {} for e in ENGS}
        for e in ENGS:
            self._sem("eng_" + e)
        self.rec = None
        self.eng_time = {}

    def record(self, f, *args):
        self.rec = []
        f(*args)
        r, self.rec = self.rec, None
        return r

    def commit(self, item):
        kind, eng, fn, reads, writes, slot = item[:6]
        if kind == "op":
            self.op(eng, fn, reads, writes)
        else:
            self.dma(eng, fn, slot, reads, writes)

    def commit_scheduled(self, ops):
        n = len(ops)
        deps = [set() for _ in range(n)]
        last_w, readers, last_slot = {}, {}, {}
        for i, it in enumerate(ops):
            kind, eng, fn, reads, writes, slot = it[:6]
            for b in reads:
                if id(b) in last_w:
                    deps[i].add(last_w[id(b)])
            for b in writes:
                if id(b) in last_w:
                    deps[i].add(last_w[id(b)])
                for r in readers.get(id(b), ()):
                    deps[i].add(r)
            if kind == "dma":
                if slot in last_slot:
                    deps[i].add(last_slot[slot])
                last_slot[slot] = i
            for b in reads:
                readers.setdefault(id(b), []).append(i)
            for b in writes:
                last_w[id(b)] = i
                readers[id(b)] = []
            deps[i].discard(i)
        users = [[] for _ in range(n)]
        indeg = [0] * n
        for i in range(n):
            indeg[i] = len(deps[i])
            for d_ in deps[i]:
                users[d_].append(i)
        t0 = max(self.eng_time.values()) if self.eng_time else 0.0
        eng_free = {e: max(self.eng_time.get(e, 0.0), t0 - 3.0) for e in ENGS}
        finish = [0.0] * n
        ready_t = [t0 - 3.0] * n
        ready = [i for i in range(n) if indeg[i] == 0]
        done = 0
        while ready:
            best, best_key = None, None
            for i in ready:
                st = max(eng_free[ops[i][1]], ready_t[i])
                key = (st, i)
                if best_key is None or key < best_key:
                    best, best_key = i, key
            i = best
            ready.remove(i)
            eng = ops[i][1]
            occ, lat = ops[i][6]
            st = best_key[0]
            eng_free[eng] = st + occ
            finish[i] = st + occ + lat
            self.commit(ops[i])
            done += 1
            for u in users[i]:
                hop = 0.05 if (ops[u][1] == eng and ops[i][0] == "op") else 0.2
                ready_t[u] = max(ready_t[u], finish[i] + hop)
                indeg[u] -= 1
                if indeg[u] == 0:
                    ready.append(u)
        assert done == n
        self.eng_time = eng_free

    def commit_interleaved(self, a, b):
        ia = ib = 0
        na, nb = len(a), len(b)
        while ia < na or ib < nb:
            if ib >= nb or (ia < na and ia * nb <= ib * na):
                self.commit(a[ia]); ia += 1
            else:
                self.commit(b[ib]); ib += 1

    def _sem(self, key):
        if key not in self.sems:
            self.sems[key] = self.es.enter_context(self.nc.semaphore("s_" + key))
            self.count[key] = 0
        return self.sems[key]

    def sbuf(self, name, shape, dt):
        return self.es.enter_context(self.nc.sbuf_tensor("sb_" + name, list(shape), dt))

    def psum(self, name, shape, dt):
        return self.es.enter_context(self.nc.psum_tensor("ps_" + name, list(shape), dt))

    def _deps(self, eng, reads, writes):
        need = {}
        for b in reads:
            if b.writer is not None:
                k, v = b.writer
                need[k] = max(need.get(k, 0), v)
        for b in writes:
            if b.writer is not None:
                k, v = b.writer
                need[k] = max(need.get(k, 0), v)
            for (k, v) in b.readers:
                need[k] = max(need.get(k, 0), v)
        waits = []
        wd = self.waited[eng]
        for k, v in need.items():
            if eng == "pe" and k == "eng_pe":
                continue
            if wd.get(k, 0) >= v:
                continue
            wd[k] = v
            waits.append((self.sems[k], v))
        return waits

    def _commit(self, ticket, reads, writes):
        for b in reads:
            b.readers.append(ticket)
        for b in writes:
            b.writer = ticket
            b.readers = []

    def op(self, eng, fn, reads=(), writes=()):
        if self.rec is not None:
            self.rec.append(("op", eng, fn, tuple(reads), tuple(writes), None, _op_cost(eng, "op", fn)))
            return None
        waits = self._deps(eng, reads, writes)
        key = "eng_" + eng
        self.count[key] += 1
        ticket = (key, self.count[key])
        self.q[eng].append((waits, fn, (self.sems[key], 1)))
        self._commit(ticket, reads, writes)
        return ticket

    def dma(self, eng, fn, slot, reads=(), writes=()):
        if self.rec is not None:
            self.rec.append(("dma", eng, fn, tuple(reads), tuple(writes), slot, _op_cost(eng, "dma", fn)))
            return None
        waits = self._deps(eng, reads, writes)
        key = "dma_" + slot
        self._sem(key)
        prev = self.count[key]
        if prev > 0 and self.waited[eng].get(key, 0) < prev:
            self.waited[eng][key] = prev
            waits.append((self.sems[key], prev))
        self.count[key] += 16
        ticket = (key, self.count[key])
        self.q[eng].append((waits, fn, (self.sems[key], 16)))
        self._commit(ticket, reads, writes)
        return ticket

    def final_wait(self, eng, bufs):
        waits = self._deps(eng, (), bufs)
        self.q[eng].append((waits, None, None))

    def emit(self):
        nc = self.nc
        q = self.q

        def replay(e, lst):
            for waits, fn, inc in lst:
                for (s, v) in waits:
                    e.wait_ge(s, v)
                if fn is None:
                    continue
                ins = fn(e)
                if inc is not None:
                    ins.then_inc(inc[0], inc[1])

        with nc.Block() as blk:
            @blk.tensor
            def _(e):
                replay(e, q["pe"])

            @blk.scalar
            def _(e):
                replay(e, q["act"])

            @blk.vector
            def _(e):
                replay(e, q["dve"])

            @blk.gpsimd
            def _(e):
                replay(e, q["pool"])

            @blk.sync
            def _(e):
                replay(e, q["sp"])


def build(n_tiles=32, dbg=False, stop_after=None, sched=True, skip=None):
    nc = bass.Bass("TRN2", target_bir_lowering=False)

    def din(name, shape, dt=F32):
        return nc.dram_tensor(name, list(shape), dt, kind="ExternalInput").ap()

    x_d = din("x", [SEQ, D])
    c_d = din("c_t", [128, 8])
    wada_d = din("w_ada", [D, 6 * D])
    bada_d = din("b_ada", [1, 6 * D])
    n1g_d = din("norm1_g", [1, D])
    n2g_d = din("norm2_g", [1, D])
    win_d = din("w_in", [D, 2304])
    cw_d = din("conv_w_t", [128, 12])
    qg_d = din("qg_t", [128, 1])
    kg_d = din("kg_t", [128, 1])
    sinks_d = din("sinks", [1, 8])
    rb_d = din("rel_bias", [1, 256])
    cg_d = din("conv_g_t", [128, 4])
    ag_d = din("attn_g_t", [128, 4])
    wout_d = din("w_out", [D, D])
    wq_d = din("peer_wq", [D, 2048])
    keysT_d = din("keysT", [128, 16 * 128])
    uv_d = din("uv", [16384, 2048])
    ident_d = din("ident", [128, 128])
    bones_d = din("blockones", [128, 128])
    iota_d = din("iota16", [128, 16])
    ohs_d = din("ohs", [32, 128, 256])
    negm_d = din("negmask", [128, 256])
    y_d = nc.dram_tensor("y", [SEQ, D], F32, kind="ExternalOutput").ap()
    dbg_d = {}

    def dbg_out(name, shape, dt=F32):
        dbg_d[name] = nc.dram_tensor("dbg_" + name, list(shape), dt, kind="ExternalOutput").ap()
        return dbg_d[name]

    with ExitStack() as es:
        P = Prog(nc, es)
        _bufs = {}
        tap_bufs = []
        _regs = {}

        def bc_reg(e):
            if isinstance(e, _Spy):
                return 16383
            if "bc" not in _regs:
                _regs["bc"] = e.to_reg(16383)
            return _regs["bc"]

        def dtap(name, ap, buf, shape, dt=F32):
            if not dbg:
                return
            od = dbg_out(name, shape, dt)
            P.dma("sp", lambda e: e.dma_start(out=od, in_=ap), "dbg_" + name, reads=[buf])
            tap_bufs.append(buf)

        def SB(name, shape, dt=F32):
            t = P.sbuf(name, shape, dt)
            b = Buf(name)
            _bufs[name] = b
            return t, b

        def PS(name, shape, dt=F32):
            t = P.psum(name, shape, dt)
            b = Buf(name)
            return t, b

        ident, b_ident = SB("ident", [128, 128], BF16)
        bones, b_bones = SB("bones", [128, 128], BF16)
        iota16, b_iota = SB("iota16", [128, 16])
        Win, b_Win = SB("Win", [128, 8, 2304], BF16)
        Wk2, b_Wk2 = SB("Wk2", [128, 8, 256], BF16)
        Wout, b_Wout = SB("Wout", [128, 8, 1024], BF16)
        Wq, b_Wq = SB("Wq", [128, 8, 2048], BF16)
        keysT, b_keysT = SB("keysT", [128, 16, 128], BF16)
        bc, b_bc = SB("bc", [128, 4 * D])
        bias_all, b_bias = SB("bias_all", [128, 8, 256])
        cw, b_cw = SB("cw", [128, 12])
        qg, b_qg = SB("qg", [128, 1])
        kg, b_kg = SB("kg", [128, 1])
        cg, b_cg = SB("cg", [128, 4])
        ag, b_ag = SB("ag", [128, 4])
        sink_b, b_sink = SB("sink_b", [128, 8])
        rb_b, b_rb = SB("rb_b", [128, 256])
        c_t, b_ct = SB("c_t", [128, 8])
        cond, b_cond = SB("cond", [128, 8])
        brow = [SB("brow%d" % i, [1, 128]) for i in range(2)]
        mrow = [SB("mrow%d" % i, [1, 128]) for i in range(2)]
        ones_row, b_ones = SB("ones_row", [1, 128])

        tmpf, b_tmpf = SB("tmpf", [128, D])
        g1_tmp, b_g1tmp = tmpf, b_tmpf
        NG = 7
        gb = [SB("gb%d" % i, [128, 2048], BF16) for i in range(NG)]
        xs = [SB("xs%d" % i, [128, D]) for i in range(2)]
        stg = xs
        pr, b_pr = SB("pr", [128, 18, 128])
        h2bs = [SB("h2b%d" % i, [128, D], BF16) for i in range(2)]
        g2_tmp, b_g2tmp = pr[:].rearrange("p c n -> p (c n)")[:, 0:D], b_pr
        uvb_d = nc.dram_tensor("uvb", [16384, 2048], BF16, kind="Internal").ap()
        b_uvb_u = [Buf("uvb_u%d" % i) for i in range(4)]
        b_uvb_v = [Buf("uvb_v%d" % i) for i in range(6)]
        b_uvb_all = b_uvb_u + b_uvb_v

        PA, b_PA = PS("PA", [128, 512])
        PB, b_PB = PS("PB", [128, 512])
        PC, b_PC = PS("PC", [128, 512])
        PD, b_PD = PS("PD", [128, 512])
        PT, b_PT = PS("PT", [128, 1024], BF16)
        PT2, b_PT2 = PT, b_PT
        acc0, b_acc0 = PS("acc0", [128, 512])
        acc1, b_acc1 = PS("acc1", [128, 512])
        PSc, b_PSc0 = PS("PSc", [128, 2, 256])
        b_PSc = [b_PSc0, b_PSc0]
        PO, b_PO = PD, b_PD

        win_v = win_d.rearrange("(kc p) n -> p kc n", p=128)
        wout_v = wout_d.rearrange("(kc p) n -> p kc n", p=128)
        wq_v = wq_d.rearrange("(kc p) n -> p kc n", p=128)
        wada_v = wada_d.rearrange("(kc p) n -> p kc n", p=128)

        def ld(eng, out_ap, in_ap, slot, wbuf):
            P.dma(eng, lambda e: e.dma_start(out=out_ap, in_=in_ap), slot, writes=[wbuf])

        ld("pool", ident[:], ident_d, "c0", b_ident)
        ld("pool", bones[:], bones_d, "c1", b_bones)
        ld("sp", iota16[:], iota_d, "c2", b_iota)
        ld("sp", cw[:], cw_d, "c3", b_cw)
        ld("sp", qg[:], qg_d, "c4", b_qg)
        ld("sp", kg[:], kg_d, "c5", b_kg)
        ld("sp", cg[:], cg_d, "c6", b_cg)
        ld("sp", ag[:], ag_d, "c7", b_ag)
        ld("sp", c_t[:], c_d, "c8", b_ct)
        ld("sp", sink_b[:], sinks_d.partition_broadcast(128), "c9", b_sink)
        ld("sp", rb_b[:], rb_d.partition_broadcast(128), "c10", b_rb)
        for kc in range(8):
            ld("pool", Win[:, kc, :], win_v[:, kc, :], "w%d" % (kc % 4), b_Win)
        for kv in range(2):
            for r in range(2):
                ld("pool", Wk2[:, :, kv * 128 + r * 64: kv * 128 + r * 64 + 64],
                   win_v[:, :, 2048 + kv * 64: 2048 + kv * 64 + 64], "w%d" % (kv * 2 + r), b_Wk2)
        for kc in range(8):
            ld("pool", Wout[:, kc, :], wout_v[:, kc, :], "w%d" % (kc % 4), b_Wout)
        for kc in range(8):
            ld("pool", Wq[:, kc, :], wq_v[:, kc, :], "w%d" % (kc % 4), b_Wq)
        ld("pool", keysT[:].rearrange("p c n -> p (c n)"), keysT_d, "w0", b_keysT)
        CR = 256
        for cidx in range(16384 // CR):
            P.dma("pool", lambda e, cidx=cidx: e.dma_start(
                out=uvb_d[cidx * CR:(cidx + 1) * CR, 0:D], in_=uv_d[cidx * CR:(cidx + 1) * CR, 0:D]),
                "cv%d" % (cidx % 4), writes=[b_uvb_u[cidx % 4]])

        P.op("act", lambda e: e.activation(out=cond[:], in_=c_t[:], func=AF.Silu),
             reads=[b_ct], writes=[b_cond])
        P.op("dve", lambda e: e.memset(ones_row[:], 1.0), writes=[b_ones])
        CW = 128
        NCH = 6 * D // CW
        for ch in range(NCH):
            g_t, g_b = stg[ch % 2]
            wv = g_t[:].rearrange("p (k n) -> p k n", k=8)
            ld("sp", wv, wada_v[:, :, ch * CW:(ch + 1) * CW], "ada%d" % (ch % 2), g_b)
            br_t, br_b = brow[ch % 2]
            mr_t, mr_b = mrow[ch % 2]
            ld("sp", br_t[:, 0:CW], bada_d[:, ch * CW:(ch + 1) * CW], "bada%d" % (ch % 2), br_b)
            pbank, pbuf = (PA, b_PA) if ch % 2 == 0 else (PB, b_PB)
            for kc in range(8):
                P.op("pe", lambda e, kc=kc, wv=wv, pbank=pbank: e.matmul(
                    pbank[0:1, 0:CW], lhsT=cond[:, kc:kc + 1], rhs=wv[:, kc, :],
                    start=(kc == 0), stop=(kc == 7)),
                    reads=[b_cond, g_b], writes=[pbuf])
            P.op("dve", lambda e, pbank=pbank, br_t=br_t, mr_t=mr_t: e.tensor_tensor(
                out=mr_t[:, 0:CW], in0=pbank[0:1, 0:CW], in1=br_t[:, 0:CW], op=ALU.add),
                reads=[pbuf, br_b], writes=[mr_b])
            pbank2, pbuf2 = (PC, b_PC) if ch % 2 == 0 else (PD, b_PD)
            P.op("pe", lambda e, pbank2=pbank2, mr_t=mr_t: e.matmul(
                pbank2[:, 0:CW], lhsT=ones_row[0:1, :], rhs=mr_t[0:1, 0:CW],
                start=True, stop=True), reads=[b_ones, mr_b], writes=[pbuf2])
            col0 = ch * CW
            if col0 < 2 * D:
                dst = bc[:, col0:col0 + CW]
                dbuf = b_bc
            elif col0 < 3 * D:
                dst = g1_tmp[:, col0 - 2 * D:col0 - 2 * D + CW]
                dbuf = b_g1tmp
            elif col0 < 5 * D:
                dst = bc[:, col0 - D:col0 - D + CW]
                dbuf = b_bc
            else:
                dst = g2_tmp[:, col0 - 5 * D:col0 - 5 * D + CW]
                dbuf = b_g2tmp
            P.op("act", lambda e, pbank2=pbank2, dst=dst: e.copy(out=dst, in_=pbank2[:, 0:CW]),
                 reads=[pbuf2], writes=[dbuf])
        sh1_b, sc1_b = bc[:, 0:D], bc[:, D:2 * D]
        sh2_b, sc2_b = bc[:, 2 * D:3 * D], bc[:, 3 * D:4 * D]
        for kc in range(8):
            P.op("dve", lambda e, kc=kc: e.tensor_tensor(
                out=Wout[:, kc, :], in0=Wout[:, kc, :], in1=g1_tmp[:, 0:D], op=ALU.mult),
                reads=[b_Wout, b_g1tmp], writes=[b_Wout])
        for (sc_ap, ng_d, slot) in ((sc1_b, n1g_d, 0), (sc2_b, n2g_d, 1)):
            g_t, g_b = stg[slot]
            ld("sp", g_t[:, 0:D], ng_d.partition_broadcast(128), "ng%d" % slot, g_b)
            P.op("dve", lambda e, sc_ap=sc_ap, g_t=g_t: e.scalar_tensor_tensor(
                out=sc_ap, in0=sc_ap, scalar=1.0, in1=g_t[:, 0:D], op0=ALU.add, op1=ALU.mult),
                reads=[b_bc, g_b], writes=[b_bc])
        P.op("dve", lambda e: e.tensor_scalar(out=qg[:], in0=qg[:], scalar1=0.125, scalar2=None,
                                              op0=ALU.mult), reads=[b_qg], writes=[b_qg])

        P.op("dve", lambda e: e.memset(bias_all[:], 0.0), writes=[b_bias])
        for b in range(32):
            g_t, g_b = stg[b % 2]
            ld("sp", g_t[:, 0:256], ohs_d[b], "oh%d" % (b % 2), g_b)
            for h in range(8):
                P.op("dve", lambda e, b=b, h=h, g_t=g_t: e.scalar_tensor_tensor(
                    out=bias_all[:, h, :], in0=g_t[:, 0:256], scalar=rb_b[:, b * 8 + h: b * 8 + h + 1],
                    in1=bias_all[:, h, :], op0=ALU.mult, op1=ALU.add),
                    reads=[g_b, b_rb, b_bias], writes=[b_bias])
        g_t, g_b = stg[0]
        ld("sp", g_t[:, 0:256], negm_d, "oh0", g_b)
        for h in range(8):
            P.op("dve", lambda e, h=h, g_t=g_t: e.tensor_add(
                out=bias_all[:, h, :], in0=bias_all[:, h, :], in1=g_t[:, 0:256]),
                reads=[g_b, b_bias], writes=[b_bias])

        vin = [stg[0], stg[1], (tmpf, b_tmpf)]
        vout = [h2bs[0], h2bs[1]] + [(gb[i][0][:, 0:D], Buf("vo_a%d" % i)) for i in range(2)] \
            + [(gb[i][0][:, D:2 * D], Buf("vo_b%d" % i)) for i in range(2)]
        for cidx in range(128):
            s_t, s_b = vin[cidx % 3]
            o_t, o_b = vout[cidx % 6]
            ld("sp", s_t[:, :], uv_d[cidx * 128:(cidx + 1) * 128, D:2 * D], "vc%d" % (cidx % 3), s_b)
            P.op("dve", lambda e, s_t=s_t, o_t=o_t: e.tensor_tensor(out=o_t[:, :], in0=s_t[:, :], in1=g2_tmp, op=ALU.mult),
                 reads=[s_b, b_g2tmp], writes=[o_b])
            P.dma("act", lambda e, cidx=cidx, o_t=o_t: e.dma_start(
                out=uvb_d[cidx * 128:(cidx + 1) * 128, D:2 * D], in_=o_t[:, :]), "vo%d" % (cidx % 6),
                reads=[o_b], writes=[b_uvb_v[cidx % 6]])

        jh, b_jh = SB("jh", [128, 2 * D], BF16)
        junkb, b_junkb = jh[:, 0:D], b_jh
        hb, b_hb = jh[:, D:2 * D], b_jh
        hT, b_hT = SB("hT", [128, 8, 128], BF16)
        a2, b_a2 = SB("a2", [128, 128])
        ga2, b_ga2 = SB("ga2", [128, 128])
        a2_bufs = [Buf("a2_%d" % i) for i in range(8)]
        ga2_bufs = [Buf("ga2_%d" % i) for i in range(8)]
        wgtfs = [SB("wgtf%d" % i, [128, 128]) for i in range(2)]
        dg = [SB("dg%d" % i, [128, 128], BF16) for i in range(4)]
        st1, b_st1 = SB("st1", [128, 4])
        vbuf = [SB("vbuf%d" % i, [128, 128], BF16) for i in range(2)]
        kTb = [SB("kTb%d" % i, [128, 2, 128], BF16) for i in range(2)]
        _ub = SB("ub", [128, 4, 130])
        ub = [_ub, _ub]
        ucar, b_ucar = SB("ucar", [128, 4, 2])
        yc, b_yc = SB("yc", [128, 4, 128])
        sqb, b_sqb = SB("sqb", [128, 6, 128], BF16)
        rst, b_rst = SB("rst", [128, 6, 128])
        mixT, b_mixT = hT, b_hT
        qT, b_qT = SB("qT", [128, 4, 128], BF16)
        s_sb = [SB("s_sb%d" % i, [128, 256]) for i in range(2)]
        p_sb = [SB("p_sb%d" % i, [128, 256], BF16) for i in range(2)]
        pT_sb = [SB("pT_sb%d" % i, [128, 2, 128], BF16) for i in range(2)]
        hst = [SB("hst%d" % i, [128, 8]) for i in range(2)]
        rden, b_rden = SB("rden", [128, 8])
        on, b_on = yc[:].rearrange("p c (a b) -> p (c a) b", b=64), b_yc
        onb, b_onb = SB("onb", [128, 512], BF16)
        ssa, b_ssa = SB("ssa", [128, 8])
        qTp, b_qTp = jh[:].rearrange("p (c n) -> p c n", c=16), b_jh
        S, b_S = pr[:, 0:16, :], b_pr
        _s2 = SB("S2_0", [128, 128])
        S2 = [_s2, _s2]
        V1, b_V1 = SB("V1", [128, 16, 16])
        I1, b_I1 = SB("I1", [128, 16, 16], U32)
        I1f, b_I1f = SB("I1f", [128, 16, 16])
        rstf = rst[:].rearrange("p c n -> p (c n)")
        ycf = yc[:].rearrange("p c n -> p (c n)")
        onf = on[:].rearrange("p c n -> p (c n)")
        cand = [(rstf[:, 0:256].rearrange("p (a b) -> p a b", a=16), b_rst),
                (rstf[:, 256:512].rearrange("p (a b) -> p a b", a=16), b_rst)]
        cand2 = [(rstf[:, 512:768], b_rst), (ycf[:, 0:256], b_yc)]
        T2, b_T2 = SB("T2", [128, 8, 16])
        pos, b_pos = SB("pos", [128, 8, 16], U32)
        k2u, b_k2u = SB("k2u", [128, 8, 16], U32)
        k1f, b_k1f = SB("k1f", [128, 8, 16])
        k2f, b_k2f = SB("k2f", [128, 8, 16])
        eq = [(ycf[:, 256:512].rearrange("p (a b) -> p a b", a=16), b_yc),
              (onf[:, 0:256].rearrange("p (a b) -> p a b", a=16), b_on)]
        ia_s, b_ia_s = SB("ia_s", [128, 8, 16])
        ib_s, b_ib_s = SB("ib_s", [128, 8, 16])
        eidf, b_eidf = s_sb[0][0][:, 0:128], s_sb[0][1]
        eidis = [SB("eidi%d" % i, [128, 128], I32) for i in range(2)]
        wgt, b_wgt = s_sb[1][0][:, 128:256].rearrange("p (h k) -> p h k", h=8), s_sb[1][1]
        wsum, b_wsum = SB("wsum", [128, 8])
        negt, b_negt = SB("negt", [128, 8])
        a_sb, b_a = s_sb[0][0][:, 128:256], s_sb[0][1]
        ga_sb, b_ga = s_sb[1][0][:, 0:128], s_sb[1][1]
        acc, b_acc = tmpf, b_tmpf

        def rms_stats(src_ap, src_buf, n_free, out_col_ap, out_buf):
            P.op("act", lambda e: e.activation(out=junkb[:, 0:n_free], in_=src_ap, func=AF.Square,
                                               accum_out=out_col_ap),
                 reads=[src_buf], writes=[b_junkb, out_buf])
            rsqrt_inplace(out_col_ap, out_buf, 1.0 / n_free)

        def rsqrt_inplace(ap, buf, scale, src_ap=None, src_buf=None):
            s_ap = ap if src_ap is None else src_ap
            rd = [buf] if src_buf is None else [src_buf]
            P.op("dve", lambda e: e.tensor_scalar(out=ap, in0=s_ap, scalar1=scale, scalar2=EPS,
                                                  op0=ALU.mult, op1=ALU.add), reads=rd, writes=[buf])
            P.op("act", lambda e: e.activation(out=ap, in_=ap, func=AF.Ln), reads=[buf], writes=[buf])
            P.op("act", lambda e: e.activation(out=ap, in_=ap, func=AF.Exp, scale=-0.5), reads=[buf], writes=[buf])

        def transpose8(src, src_buf, dst, dst_buf, nchunk=8, ptile=None, pbuf=None):
            ptile = PT if ptile is None else ptile
            pbuf = b_PT if pbuf is None else pbuf
            for kc in range(nchunk):
                P.op("pe", lambda e, kc=kc: e.transpose(
                    out=ptile[:, kc * 128:(kc + 1) * 128], in_=src[:, kc * 128:(kc + 1) * 128],
                    identity=ident[:]), reads=[src_buf, b_ident], writes=[pbuf])
            if dst is not None:
                P.op("act", lambda e: e.copy(out=dst[:].rearrange("p c n -> p (c n)"),
                                             in_=ptile[:, 0:nchunk * 128]),
                     reads=[pbuf], writes=[dst_buf])

        out_bufs = []

        def front(n):
            xt, b_xt = xs[n % 2]
            x1, b_x1 = xt, b_xt
            cur, prv = n % 2, (n - 1) % 2
            h2b, b_h2b = h2bs[cur]
            eidi, b_eidi = eidis[cur]
            wgtf, b_wgtf = wgtfs[cur]
            ld("sp", xt[:], x_d[n * T:(n + 1) * T, :], "x%d" % cur, b_xt)

            rms_stats(xt[:], b_xt, D, st1[:, 0:1], b_st1)
            P.op("dve", lambda e, xt=xt: e.scalar_tensor_tensor(
                out=tmpf[:], in0=xt[:], scalar=st1[:, 0:1], in1=sc1_b, op0=ALU.mult, op1=ALU.mult),
                reads=[b_xt, b_st1, b_bc], writes=[b_tmpf])
            P.op("dve", lambda e: e.tensor_add(out=hb[:], in0=tmpf[:], in1=sh1_b),
                 reads=[b_tmpf, b_bc], writes=[b_hb])
            if n == 0:
                dtap("hb", hb[:], b_hb, [128, D], BF16)
                dtap("bc", bc[:], b_bc, [128, 4 * D])
            transpose8(hb, b_hb, hT, b_hT)
            if n == 0:
                dtap("hT", hT[:], b_hT, [128, 8, 128], BF16)

            def wcols(j):
                if j < 16:
                    return Win, b_Win, j * 128
                return Wk2, b_Wk2, (j - 16) * 128
            for g in range(5):
                pbank, pbuf = (PA, b_PA) if g % 2 == 0 else (PB, b_PB)
                chunks = list(range(g * 4, min(g * 4 + 4, 18)))
                for i, j in enumerate(chunks):
                    wt, wb, c0 = wcols(j)
                    for kc in range(8):
                        P.op("pe", lambda e, i=i, kc=kc, wt=wt, c0=c0, pbank=pbank: e.matmul(
                            pbank[:, i * 128:(i + 1) * 128], lhsT=wt[:, kc, c0:c0 + 128], rhs=hT[:, kc, :],
                            start=(kc == 0), stop=(kc == 7)), reads=[wb, b_hT], writes=[pbuf])
                nn = len(chunks)
                P.op("act", lambda e, g=g, nn=nn, pbank=pbank: e.copy(
                    out=pr[:, g * 4:g * 4 + nn, :].rearrange("p c n -> p (c n)"), in_=pbank[:, 0:nn * 128]),
                    reads=[pbuf], writes=[b_pr])
            vt, b_vt = vbuf[cur]
            for kc in range(8):
                P.op("pe", lambda e, kc=kc: e.matmul(
                    PC[:, 0:128], lhsT=hT[:, kc, :], rhs=Win[:, kc, 2176:2304],
                    start=(kc == 0), stop=(kc == 7)), reads=[b_Win, b_hT], writes=[b_PC])
            P.op("act", lambda e, vt=vt: e.copy(out=vt[:], in_=PC[:, 0:128]), reads=[b_PC], writes=[b_vt])

            if n == 0:
                dtap("pr", pr[:], b_pr, [128, 18, 128])
                dtap("vt", vt[:], b_vt, [128, 128], BF16)
            ut, b_ut = ub[cur]
            upt, b_upt = ub[prv]
            if n == 0:
                P.op("dve", lambda e, ut=ut: e.memset(ut[:, :, 0:2], 0.0), writes=[b_ut])
            else:
                P.op("dve", lambda e, ut=ut: e.tensor_copy(out=ut[:, :, 0:2], in_=ucar[:]),
                     reads=[b_ucar], writes=[b_ut])
            P.op("dve", lambda e, ut=ut: e.tensor_tensor(
                out=ut[:, :, 2:130], in0=pr[:, 4:8, :], in1=pr[:, 8:12, :], op=ALU.mult),
                reads=[b_pr], writes=[b_ut])
            for j in range(4):
                P.op("dve", lambda e, j=j, ut=ut: e.tensor_scalar(
                    out=yc[:, j, :], in0=ut[:, j, 2:130], scalar1=cw[:, j * 3 + 2:j * 3 + 3], scalar2=None,
                    op0=ALU.mult), reads=[b_ut, b_cw], writes=[b_yc])
                for tap in (1, 0):
                    P.op("dve", lambda e, j=j, tap=tap, ut=ut: e.scalar_tensor_tensor(
                        out=yc[:, j, :], in0=ut[:, j, tap:tap + 128], scalar=cw[:, j * 3 + tap:j * 3 + tap + 1],
                        in1=yc[:, j, :], op0=ALU.mult, op1=ALU.add), reads=[b_ut, b_cw, b_yc], writes=[b_yc])
            P.op("dve", lambda e: e.tensor_tensor(out=yc[:], in0=yc[:], in1=pr[:, 0:4, :], op=ALU.mult),
                 reads=[b_yc, b_pr], writes=[b_yc])
            P.op("dve", lambda e, ut=ut: e.tensor_copy(out=ucar[:], in_=ut[:, :, 128:130]),
                 reads=[b_ut], writes=[b_ucar])
            P.op("act", lambda e: e.activation(out=sqb[:, 0:4, :], in_=yc[:], func=AF.Square),
                 reads=[b_yc], writes=[b_sqb])
            for j in range(4):
                P.op("pe", lambda e, j=j: e.matmul(PD[:, j * 128:(j + 1) * 128], lhsT=bones[:], rhs=sqb[:, j, :],
                                                   start=True, stop=True), reads=[b_bones, b_sqb], writes=[b_PD])
            rsqrt_inplace(rst[:, 0:4, :].rearrange("p c n -> p (c n)"), b_rst, 1.0 / 64,
                          src_ap=PD[:, 0:512], src_buf=b_PD)
            for j in range(4):
                P.op("dve", lambda e, j=j: e.scalar_tensor_tensor(
                    out=mixT[:, j, :], in0=yc[:, j, :], scalar=cg[:, j:j + 1], in1=rst[:, j, :],
                    op0=ALU.mult, op1=ALU.mult), reads=[b_yc, b_cg, b_rst], writes=[b_mixT])

            P.op("act", lambda e: e.activation(out=sqb[:], in_=pr[:, 12:18, :], func=AF.Square),
                 reads=[b_pr], writes=[b_sqb])
            for j in range(4):
                P.op("pe", lambda e, j=j: e.matmul(PD[:, j * 128:(j + 1) * 128], lhsT=bones[:], rhs=sqb[:, j, :],
                                                   start=True, stop=True), reads=[b_bones, b_sqb], writes=[b_PD])
            rsqrt_inplace(rst[:, 0:4, :].rearrange("p c n -> p (c n)"), b_rst, 1.0 / 64,
                          src_ap=PD[:, 0:512], src_buf=b_PD)
            for j in range(2):
                P.op("pe", lambda e, j=j: e.matmul(PC[:, j * 128:(j + 1) * 128], lhsT=bones[:], rhs=sqb[:, 4 + j, :],
                                                   start=True, stop=True), reads=[b_bones, b_sqb], writes=[b_PC])
            rsqrt_inplace(rst[:, 4:6, :].rearrange("p c n -> p (c n)"), b_rst, 1.0 / 64,
                          src_ap=PC[:, 0:256], src_buf=b_PC)
            for j in range(4):
                P.op("dve", lambda e, j=j: e.scalar_tensor_tensor(
                    out=qT[:, j, :], in0=pr[:, 12 + j, :], scalar=qg[:, 0:1], in1=rst[:, j, :],
                    op0=ALU.mult, op1=ALU.mult), reads=[b_pr, b_qg, b_rst], writes=[b_qT])
            kt, b_kt = kTb[cur]
            kpt, b_kpt = kTb[prv]
            for j in range(2):
                P.op("dve", lambda e, j=j, kt=kt: e.scalar_tensor_tensor(
                    out=kt[:, j, :], in0=pr[:, 16 + j, :], scalar=kg[:, 0:1], in1=rst[:, 4 + j, :],
                    op0=ALU.mult, op1=ALU.mult), reads=[b_pr, b_kg, b_rst], writes=[b_kt])

            if n == 0:
                dtap("yc", yc[:], b_yc, [128, 4, 128])
                dtap("mixT_conv", mixT[:, 0:4, :], b_mixT, [128, 4, 128], BF16)
                dtap("qT", qT[:], b_qT, [128, 4, 128], BF16)
                dtap("kT", kt[:], b_kt, [128, 2, 128], BF16)
            vpt, b_vpt = vbuf[prv]
            for h in range(8):
                kv, r, qc = h // 4, h % 2, h // 2
                sl = h % 2
                pl, ph = 64 * r, 64 * r + 64
                st_, b_s = s_sb[sl]
                pt_, b_p = p_sb[sl]
                pTt, b_pTt = pT_sb[sl]
                hs, b_hs = hst[sl]
                c_lo = 0 if n > 0 else 128
                if n > 0:
                    P.op("pe", lambda e, sl=sl, qc=qc, kv=kv, pl=pl, ph=ph, kpt=kpt: e.matmul(
                        PSc[:, sl, 0:128], lhsT=qT[pl:ph, qc, :], rhs=kpt[pl:ph, kv, :], start=True, stop=True),
                        reads=[b_qT, b_kpt], writes=[b_PSc[sl]])
                P.op("pe", lambda e, sl=sl, qc=qc, kv=kv, pl=pl, ph=ph, kt=kt: e.matmul(
                    PSc[:, sl, 128:256], lhsT=qT[pl:ph, qc, :], rhs=kt[pl:ph, kv, :], start=True, stop=True),
                    reads=[b_qT, b_kt], writes=[b_PSc[sl]])
                P.op("dve", lambda e, sl=sl, h=h, st_=st_, c_lo=c_lo: e.tensor_tensor(
                    out=st_[:, c_lo:256], in0=PSc[:, sl, c_lo:256], in1=bias_all[:, h, c_lo:256], op=ALU.add),
                    reads=[b_PSc[sl], b_bias], writes=[b_s])
                P.op("dve", lambda e, st_=st_, hs=hs, c_lo=c_lo: e.reduce_max(
                    out=hs[:, 0:1], in_=st_[:, c_lo:256], axis=AX.X), reads=[b_s], writes=[b_hs])
                P.op("dve", lambda e, hs=hs, h=h: e.tensor_scalar(
                    out=hs[:, 1:2], in0=hs[:, 0:1], scalar1=sink_b[:, h:h + 1], scalar2=-1.0,
                    op0=ALU.max, op1=ALU.mult), reads=[b_hs, b_sink], writes=[b_hs])
                P.op("act", lambda e, st_=st_, pt_=pt_, hs=hs, c_lo=c_lo: e.activation(
                    out=pt_[:, c_lo:256], in_=st_[:, c_lo:256], func=AF.Exp, bias=hs[:, 1:2],
                    accum_out=hs[:, 2:3]), reads=[b_s, b_hs], writes=[b_p, b_hs])
                P.op("act", lambda e, hs=hs, h=h: e.activation(
                    out=hs[:, 3:4], in_=sink_b[:, h:h + 1], func=AF.Exp, bias=hs[:, 1:2]),
                    reads=[b_sink, b_hs], writes=[b_hs])
                P.op("dve", lambda e, hs=hs: e.tensor_add(out=hs[:, 4:5], in0=hs[:, 2:3], in1=hs[:, 3:4]),
                     reads=[b_hs], writes=[b_hs])
                P.op("dve", lambda e, hs=hs, h=h: e.reciprocal(out=rden[:, h:h + 1], in_=hs[:, 4:5]),
                     reads=[b_hs], writes=[b_rden])
                halves = (0, 1) if n > 0 else (1,)
                for hf in halves:
                    P.op("pe", lambda e, hf=hf, sl=sl, pt_=pt_: e.transpose(
                        out=PT2[:, (sl * 2 + hf) * 128:(sl * 2 + hf + 1) * 128], in_=pt_[:, hf * 128:(hf + 1) * 128],
                        identity=ident[:]), reads=[b_p, b_ident], writes=[b_PT2])
                lo = 0 if n > 0 else 1
                P.op("act", lambda e, sl=sl, pTt=pTt, lo=lo: e.copy(
                    out=pTt[:, lo:2, :].rearrange("p c n -> p (c n)"),
                    in_=PT2[:, (sl * 2 + lo) * 128:(sl * 2 + 2) * 128]), reads=[b_PT2], writes=[b_pTt])
                if n > 0:
                    P.op("pe", lambda e, h=h, kv=kv, pTt=pTt, vpt=vpt: e.matmul(
                        PO[:, h * 64:(h + 1) * 64], lhsT=pTt[:, 0, :], rhs=vpt[:, kv * 64:(kv + 1) * 64],
                        start=True, stop=False), reads=[b_pTt, b_vpt], writes=[b_PO])
                P.op("pe", lambda e, h=h, kv=kv, pTt=pTt, vt=vt, first=(n == 0): e.matmul(
                    PO[:, h * 64:(h + 1) * 64], lhsT=pTt[:, 1, :], rhs=vt[:, kv * 64:(kv + 1) * 64],
                    start=first, stop=True), reads=[b_pTt, b_vt], writes=[b_PO])
            for h in range(8):
                P.op("dve", lambda e, h=h: e.tensor_scalar(
                    out=on[:, h, :], in0=PO[:, h * 64:(h + 1) * 64], scalar1=rden[:, h:h + 1], scalar2=None,
                    op0=ALU.mult), reads=[b_PO, b_rden], writes=[b_on])
            for h in range(8):
                P.op("act", lambda e, h=h: e.activation(
                    out=junkb[:, 0:64], in_=on[:, h, :], func=AF.Square, accum_out=ssa[:, h:h + 1]),
                    reads=[b_on], writes=[b_junkb, b_ssa])
            rsqrt_inplace(ssa[:], b_ssa, 1.0 / 64)
            for h in range(8):
                P.op("dve", lambda e, h=h: e.tensor_scalar(
                    out=onb[:, h * 64:(h + 1) * 64], in0=on[:, h, :], scalar1=ssa[:, h:h + 1], scalar2=None,
                    op0=ALU.mult), reads=[b_on, b_ssa], writes=[b_onb])
            transpose8(onb, b_onb, None, None, nchunk=4)
            for j in range(4):
                P.op("dve", lambda e, j=j: e.tensor_scalar(
                    out=mixT[:, 4 + j, :], in0=PT[:, j * 128:(j + 1) * 128], scalar1=ag[:, j:j + 1], scalar2=None,
                    op0=ALU.mult), reads=[b_PT, b_ag], writes=[b_mixT])

            if n == 0:
                dtap("on", on[:], b_on, [128, 8, 64])
                dtap("rden", rden[:], b_rden, [128, 8])
                dtap("mixT", mixT[:], b_mixT, [128, 8, 128], BF16)
            for half in range(2):
                pbank, pbuf = (PA, b_PA) if half == 0 else (PB, b_PB)
                for c in range(8):
                    P.op("pe", lambda e, c=c, half=half, pbank=pbank: e.matmul(
                        pbank[:, :], lhsT=mixT[:, c, :], rhs=Wout[:, c, half * 512:(half + 1) * 512],
                        start=(c == 0), stop=(c == 7)), reads=[b_mixT, b_Wout], writes=[pbuf])
                P.op("dve", lambda e, half=half, pbank=pbank, xt=xt: e.tensor_tensor(
                    out=x1[:, half * 512:(half + 1) * 512], in0=pbank[:, :],
                    in1=xt[:, half * 512:(half + 1) * 512], op=ALU.add),
                    reads=[pbuf, b_xt], writes=[b_x1])

            rms_stats(x1[:], b_x1, D, st1[:, 1:2], b_st1)
            P.op("dve", lambda e: e.scalar_tensor_tensor(
                out=tmpf[:], in0=x1[:], scalar=st1[:, 1:2], in1=sc2_b, op0=ALU.mult, op1=ALU.mult),
                reads=[b_x1, b_st1, b_bc], writes=[b_tmpf])
            P.op("dve", lambda e: e.tensor_add(out=h2b[:], in0=tmpf[:], in1=sh2_b),
                 reads=[b_tmpf, b_bc], writes=[b_h2b])
            transpose8(h2b, b_h2b, hT, b_hT)

            for g in range(4):
                pbank, pbuf = (PA, b_PA) if g % 2 == 0 else (PB, b_PB)
                for i in range(4):
                    c = g * 4 + i
                    for kc in range(8):
                        P.op("pe", lambda e, i=i, c=c, kc=kc, pbank=pbank: e.matmul(
                            pbank[:, i * 128:(i + 1) * 128], lhsT=Wq[:, kc, c * 128:(c + 1) * 128], rhs=hT[:, kc, :],
                            start=(kc == 0), stop=(kc == 7)), reads=[b_Wq, b_hT], writes=[pbuf])
                P.op("act", lambda e, g=g, pbank=pbank: e.copy(
                    out=qTp[:, g * 4:(g + 1) * 4, :].rearrange("p c n -> p (c n)"), in_=pbank[:, :]),
                    reads=[pbuf], writes=[b_qTp])
            for g in range(4):
                pbank, pbuf = (PC, b_PC) if g % 2 == 0 else (PD, b_PD)
                for i in range(4):
                    c = g * 4 + i
                    P.op("pe", lambda e, i=i, c=c, pbank=pbank: e.matmul(
                        pbank[:, i * 128:(i + 1) * 128], lhsT=qTp[:, c, :], rhs=keysT[:, c, :],
                        start=True, stop=True), reads=[b_qTp, b_keysT], writes=[pbuf])
                P.op("act", lambda e, g=g, pbank=pbank: e.copy(
                    out=S[:, g * 4:(g + 1) * 4, :].rearrange("p c n -> p (c n)"), in_=pbank[:, :]),
                    reads=[pbuf], writes=[b_S])

            for c in range(16):
                s2t, b_s2 = S2[c % 2]
                P.op("dve", lambda e, c=c: e.max(out=V1[:, c, 0:8], in_=S[:, c, :]), reads=[b_S], writes=[b_V1])
                P.op("dve", lambda e, c=c: e.max_index(out=I1[:, c, 0:8], in_max=V1[:, c, 0:8], in_values=S[:, c, :]),
                     reads=[b_S, b_V1], writes=[b_I1])
                P.op("dve", lambda e, c=c, s2t=s2t: e.match_replace(
                    out=s2t[:], in_to_replace=V1[:, c, 0:8], in_values=S[:, c, :], imm_value=-1e30),
                    reads=[b_S, b_V1], writes=[b_s2])
                P.op("dve", lambda e, c=c, s2t=s2t: e.max(out=V1[:, c, 8:16], in_=s2t[:]), reads=[b_s2], writes=[b_V1])
                P.op("dve", lambda e, c=c, s2t=s2t: e.max_index(
                    out=I1[:, c, 8:16], in_max=V1[:, c, 8:16], in_values=s2t[:]),
                    reads=[b_s2, b_V1], writes=[b_I1])
            P.op("dve", lambda e: e.tensor_copy(out=I1f[:], in_=I1[:]), reads=[b_I1], writes=[b_I1f])

            for h in range(8):
                ct, b_c = cand[h % 2]
                c2t, b_c2 = cand2[h % 2]
                P.op("dve", lambda e, h=h, ct=ct: e.tensor_tensor(
                    out=ct[:], in0=V1[:, 2 * h, :].unsqueeze(2).to_broadcast([128, 16, 16]),
                    in1=V1[:, 2 * h + 1, :].unsqueeze(1).to_broadcast([128, 16, 16]), op=ALU.add),
                    reads=[b_V1], writes=[b_c])
                cf = ct[:].rearrange("p a b -> p (a b)")
                P.op("dve", lambda e, h=h, cf=cf: e.max(out=T2[:, h, 0:8], in_=cf), reads=[b_c], writes=[b_T2])
                P.op("dve", lambda e, h=h, cf=cf: e.max_index(out=pos[:, h, 0:8], in_max=T2[:, h, 0:8], in_values=cf),
                     reads=[b_c, b_T2], writes=[b_pos])
                P.op("dve", lambda e, h=h, cf=cf, c2t=c2t: e.match_replace(
                    out=c2t[:], in_to_replace=T2[:, h, 0:8], in_values=cf, imm_value=-1e30),
                    reads=[b_c, b_T2], writes=[b_c2])
                P.op("dve", lambda e, h=h, c2t=c2t: e.max(out=T2[:, h, 8:16], in_=c2t[:]), reads=[b_c2], writes=[b_T2])
                P.op("dve", lambda e, h=h, c2t=c2t: e.max_index(
                    out=pos[:, h, 8:16], in_max=T2[:, h, 8:16], in_values=c2t[:]),
                    reads=[b_c2, b_T2], writes=[b_pos])
            P.op("dve", lambda e: e.tensor_single_scalar(out=k2u[:], in_=pos[:], scalar=15, op=ALU.bitwise_and),
                 reads=[b_pos], writes=[b_k2u])
            P.op("dve", lambda e: e.tensor_single_scalar(out=pos[:], in_=pos[:], scalar=4, op=ALU.logical_shift_right),
                 reads=[b_pos], writes=[b_pos])
            P.op("dve", lambda e: e.tensor_copy(out=k1f[:], in_=pos[:]), reads=[b_pos], writes=[b_k1f])
            P.op("dve", lambda e: e.tensor_copy(out=k2f[:], in_=k2u[:]), reads=[b_k2u], writes=[b_k2f])
            cnt = 0
            for h in range(8):
                for side, (kf, b_kf, dst, b_dst) in enumerate(((k1f, b_k1f, ia_s, b_ia_s), (k2f, b_k2f, ib_s, b_ib_s))):
                    et, b_e = eq[cnt % 2]
                    cnt += 1
                    P.op("dve", lambda e, h=h, kf=kf, et=et: e.tensor_tensor(
                        out=et[:], in0=iota16[:, :].unsqueeze(1).to_broadcast([128, 16, 16]),
                        in1=kf[:, h, :].unsqueeze(2).to_broadcast([128, 16, 16]), op=ALU.is_equal),
                        reads=[b_iota, b_kf], writes=[b_e])
                    P.op("dve", lambda e, h=h, side=side, et=et: e.tensor_tensor(
                        out=et[:], in0=et[:],
                        in1=I1f[:, 2 * h + side, :].unsqueeze(1).to_broadcast([128, 16, 16]), op=ALU.mult),
                        reads=[b_e, b_I1f], writes=[b_e])
                    P.op("dve", lambda e, h=h, dst=dst, et=et: e.reduce_sum(
                        out=dst[:, h, :], in_=et[:], axis=AX.X), reads=[b_e], writes=[b_dst])
            P.op("dve", lambda e: e.scalar_tensor_tensor(
                out=eidf[:], in0=ia_s[:].rearrange("p h k -> p (h k)"), scalar=128.0,
                in1=ib_s[:].rearrange("p h k -> p (h k)"), op0=ALU.mult, op1=ALU.add),
                reads=[b_ia_s, b_ib_s], writes=[b_eidf])
            P.op("dve", lambda e: e.tensor_copy(out=eidi[:], in_=eidf[:]), reads=[b_eidf], writes=[b_eidi])
            P.op("dve", lambda e: e.tensor_scalar(out=negt[:], in0=T2[:, :, 0], scalar1=-1.0, scalar2=None,
                                                  op0=ALU.mult), reads=[b_T2], writes=[b_negt])
            for h in range(8):
                P.op("act", lambda e, h=h: e.activation(
                    out=wgt[:, h, :], in_=T2[:, h, :], func=AF.Exp, bias=negt[:, h:h + 1],
                    accum_out=wsum[:, h:h + 1]), reads=[b_T2, b_negt], writes=[b_wgt, b_wsum])
            P.op("dve", lambda e: e.reciprocal(out=wsum[:], in_=wsum[:]), reads=[b_wsum], writes=[b_wsum])
            for h in range(8):
                P.op("dve", lambda e, h=h: e.tensor_scalar(
                    out=wgtf[:, h * 16:(h + 1) * 16], in0=wgt[:, h, :], scalar1=wsum[:, h:h + 1], scalar2=None,
                    op0=ALU.mult), reads=[b_wgt, b_wsum], writes=[b_wgtf])

            if dbg and n == 0:
                for nm, t_, b_, shp, dt_ in (("eidi", eidi, b_eidi, [128, 128], I32),
                                             ("wgt", wgtf, b_wgtf, [128, 128], F32),
                                             ("h2f", h2b, b_h2b, [128, D], BF16),
                                             ("S", S, b_S, [128, 16, 128], F32)):
                    od = dbg_out(nm, shp, dt_)
                    P.dma("sp", lambda e, od=od, t_=t_: e.dma_start(out=od, in_=t_[:]), "dbg_" + nm, reads=[b_])
                    out_bufs.append(b_)

        def back(n):
            xt, b_xt = xs[n % 2]
            cur = n % 2
            h2b, b_h2b = h2bs[cur]
            eidi, b_eidi = eidis[cur]
            wgtf, b_wgtf = wgtfs[cur]
            LAG = 2

            def stage_a(k):
                g_t, g_b = gb[k % NG]
                b_a2 = a2_bufs[k % 8]
                b_ga2 = ga2_bufs[k % 8]
                P.dma("pool", lambda e, k=k, g_t=g_t: e.indirect_dma_start(
                    out=g_t[:], out_offset=None, in_=uvb_d,
                    in_offset=bass.IndirectOffsetOnAxis(ap=eidi[:, k:k + 1], axis=0),
                    bounds_check=bc_reg(e), oob_is_err=False), "g%d" % (k % NG),
                    reads=[b_eidi] + b_uvb_all, writes=[g_b])
                P.op("dve", lambda e, k=k, g_t=g_t: e.tensor_tensor(
                    out=g_t[:, 0:D], in0=g_t[:, 0:D], in1=h2b[:], op=ALU.mult),
                    reads=[g_b, b_h2b], writes=[g_b])
                P.op("act", lambda e, k=k, g_t=g_t: e.activation(
                    out=g_t[:, 0:D], in_=g_t[:, 0:D], func=AF.Copy, accum_out=a2[:, k:k + 1]),
                    reads=[g_b], writes=[g_b, b_a2])
                P.op("act", lambda e, k=k: e.activation(
                    out=ga2[:, k:k + 1], in_=a2[:, k:k + 1], func=AF.Gelu_apprx_tanh),
                    reads=[b_a2], writes=[b_ga2])

            def stage_b(k):
                g_t, g_b = gb[k % NG]
                d_t, d_b = dg[k % 4]
                b_ga2 = ga2_bufs[k % 8]
                P.op("dve", lambda e, k=k, d_t=d_t: e.tensor_scalar(
                    out=d_t[:], in0=ident[:], scalar1=ga2[:, k:k + 1], scalar2=wgtf[:, k:k + 1],
                    op0=ALU.mult, op1=ALU.mult), reads=[b_ident, b_ga2, b_wgtf], writes=[d_b])
                for half, (ap_, ab_) in enumerate(((acc0, b_acc0), (acc1, b_acc1))):
                    P.op("pe", lambda e, k=k, half=half, ap_=ap_, d_t=d_t, g_t=g_t: e.matmul(
                        ap_[:, :], lhsT=d_t[:], rhs=g_t[:, D + half * 512:D + (half + 1) * 512],
                        start=(k == 0), stop=(k == 127)), reads=[d_b, g_b], writes=[ab_])

            for k in range(128 + LAG):
                if k < 128:
                    stage_a(k)
                if k >= LAG:
                    stage_b(k - LAG)
            for half, (ap_, ab_) in enumerate(((acc0, b_acc0), (acc1, b_acc1))):
                P.op("dve", lambda e, half=half, ap_=ap_: e.tensor_tensor(
                    out=xt[:, half * 512:(half + 1) * 512], in0=ap_[:, :],
                    in1=xt[:, half * 512:(half + 1) * 512], op=ALU.add),
                    reads=[ab_, b_xt], writes=[b_xt])
            P.dma("sp", lambda e: e.dma_start(out=y_d[n * T:(n + 1) * T, :], in_=xt[:]),
                  "y%d" % cur, reads=[b_xt])
            out_bufs.append(b_xt)

        rec_f = P.record(front, 0)
        if sched:
            P.commit_scheduled(rec_f)
        else:
            for it in rec_f:
                P.commit(it)
        for n in range(n_tiles):
            rec_b = P.record(back, n) if skip != "back" else []
            rec_f = P.record(front, n + 1) if (n + 1 < n_tiles and not (skip == "front" and n >= 1)) else []
            if sched:
                P.commit_scheduled(rec_b + rec_f)
            else:
                P.commit_interleaved(rec_b, rec_f)

        P.final_wait("sp", out_bufs + tap_bufs)
        P.emit()
    return nc, list(dbg_d.keys())


def _t5_bucket_static():
    qi = np.arange(128)[:, None]
    kj = np.arange(256)[None, :]
    dist = qi + 128 - kj
    valid = (dist >= 0) & (dist < 128)
    d0 = np.maximum(dist, 0)
    max_exact = 16
    dd = np.maximum(d0, 1).astype(np.float32)
    large = max_exact + (np.log(dd / np.float32(max_exact)) / np.float32(math.log(128 / max_exact))
                         * np.float32(32 - max_exact)).astype(np.int32)
    large = np.minimum(large, 31)
    bucket = np.where(d0 < max_exact, d0, large)
    ohs = np.zeros((32, 128, 256), np.float32)
    for b in range(32):
        ohs[b] = ((bucket == b) & valid).astype(np.float32)
    negmask = np.where(valid, 0.0, NEG).astype(np.float32)
    return ohs, negmask


def _host_layout(inp, n_tok=SEQ):
    f = lambda a: np.ascontiguousarray(np.asarray(a, dtype=np.float32))
    ohs, negmask = _t5_bucket_static()
    bones = np.zeros((128, 128), np.float32)
    bones[:64, :64] = 1.0
    bones[64:, 64:] = 1.0
    keys = f(inp["peer_keys"])[0]
    keysT = np.ascontiguousarray(keys.transpose(3, 1, 0, 2)).reshape(128, 16 * 128)
    uv = np.ascontiguousarray(np.concatenate([f(inp["peer_u"])[0], f(inp["peer_v"])[0]], axis=1))
    shared = {
        "w_ada": f(inp["w_ada"])[0],
        "b_ada": f(inp["b_ada"]).reshape(1, 6 * D),
        "norm1_g": f(inp["norm1_g"]).reshape(1, D),
        "norm2_g": f(inp["norm2_g"]).reshape(1, D),
        "w_in": f(inp["w_in"])[0],
        "conv_w_t": np.ascontiguousarray(f(inp["conv_w"])[0].reshape(3, 4, 128).transpose(2, 1, 0)).reshape(128, 12),
        "qg_t": np.ascontiguousarray(np.tile(f(inp["q_norm_g"])[0], 2).reshape(128, 1)),
        "kg_t": np.ascontiguousarray(np.tile(f(inp["k_norm_g"])[0], 2).reshape(128, 1)),
        "sinks": f(inp["sinks"]).reshape(1, 8),
        "rel_bias": f(inp["rel_bias"]).reshape(1, 256),
        "conv_g_t": np.ascontiguousarray(f(inp["conv_out_g"])[0].reshape(4, 128).T),
        "attn_g_t": np.ascontiguousarray(f(inp["attn_out_g"])[0].reshape(4, 128).T),
        "w_out": f(inp["w_out"])[0],
        "peer_wq": f(inp["peer_wq"])[0],
        "keysT": keysT,
        "uv": uv,
        "ident": np.eye(128, dtype=np.float32),
        "blockones": bones,
        "iota16": np.ascontiguousarray(np.tile(np.arange(16, dtype=np.float32), (128, 1))),
        "ohs": ohs,
        "negmask": negmask,
    }
    x = f(inp["x"])
    c = f(inp["c"])
    maps = []
    for b in range(x.shape[0]):
        m = dict(shared)
        m["x"] = np.ascontiguousarray(x[b, :SEQ])
        m["c_t"] = np.ascontiguousarray(c[b].reshape(8, 128).T)
        maps.append(m)
    return maps


def kernel(**inputs):
    maps = _host_layout(inputs)
    nc, _ = build(n_tiles=SEQ // T)
    res = run_bass_kernel_spmd(nc, maps, core_ids=list(range(8)))
    out = np.stack([np.asarray(r["y"], dtype=np.float32) for r in res.results], axis=0)
    return out
```

```python
from contextlib import ExitStack
import math
import numpy as np
import concourse.bass as bass
import concourse.mybir as mybir
from concourse.bass_utils import run_bass_kernel_spmd

F32 = mybir.dt.float32
BF16 = mybir.dt.bfloat16
I32 = mybir.dt.int32
U32 = mybir.dt.uint32
AF = mybir.ActivationFunctionType
ALU = mybir.AluOpType
AX = mybir.AxisListType

ENGS = ("pe", "act", "dve", "pool", "sp")
TBL_AGE = 0.1
D = 1024
SEQ = 4096
T = 128
EPS = 1e-6
NEG = -30000.0


class Buf:
    __slots__ = ("name", "writer", "readers")

    def __init__(self, name):
        self.name = name
        self.writer = None
        self.readers = []


class _Dummy:
    def then_inc(self, *a, **k):
        return self


class _Spy:
    def __init__(self):
        self.calls = []

    def __getattr__(self, name):
        def f(*a, **kw):
            self.calls.append((name, a, kw))
            return _Dummy()
        return f


def _dt_bytes(dt):
    return 2 if dt == BF16 else 4


def _op_cost(eng, kind, fn):
    spy = _Spy()
    try:
        fn(spy)
    except Exception:
        return (0.3, 0.0)
    if not spy.calls:
        return (0.3, 0.0)
    name, a, kw = spy.calls[-1]
    aps = [v for v in list(a) + list(kw.values()) if hasattr(v, "shape") and hasattr(v, "dtype")]
    elems, nbytes, narrow = 1, 0, True
    for v in aps:
        shp = tuple(v.shape)
        fr = 1
        for d_ in shp[1:]:
            fr *= d_
        elems = max(elems, fr)
        nbytes = max(nbytes, fr * shp[0] * _dt_bytes(v.dtype))
        if v.dtype != BF16:
            narrow = False
    if kind == "dma":
        o = kw.get("out", a[0] if a else None)
        if o is not None and hasattr(o, "shape"):
            nbytes = _dt_bytes(o.dtype)
            for d_ in tuple(o.shape):
                nbytes *= d_
        lat = 2.0 + nbytes / 3.0e5
        if name == "indirect_dma_start":
            return (1.4, lat)
        return ((1.0 if eng == "pool" else 0.15), lat)
    if eng == "pe":
        out = kw.get("out", a[0] if a else None)
        n = 128
        if out is not None and hasattr(out, "shape"):
            n = 1
            for d_ in tuple(out.shape)[1:]:
                n *= d_
        return (0.03 + 0.00042 * n, 0.15)
    if eng == "act":
        f_ = kw.get("func")
        tbl = "E" if f_ in (AF.Exp, AF.Ln) else ("G" if f_ == AF.Gelu_apprx_tanh else None)
        return (0.2 + 0.00085 * elems, 0.0, tbl)
    if name in ("max", "max_index", "match_replace"):
        return (0.15 + 0.0012 * elems, 0.0)
    return (0.08 + (0.0006 if narrow else 0.00105) * elems, 0.0)


class Prog:
    def __init__(self, nc, es):
        self.nc = nc
        self.es = es
        self.q = {e: [] for e in ENGS}
        self.sems = {}
        self.count = {}
        self.waited = {e: {} for e in ENGS}
        for e in ENGS:
            self._sem("eng_" + e)
        self.rec = None
        self.eng_time = {}
        self.act_tbl = None

    def record(self, f, *args):
        self.rec = []
        f(*args)
        r, self.rec = self.rec, None
        return r

    def commit(self, item):
        kind, eng, fn, reads, writes, slot = item[:6]
        if kind == "op":
            self.op(eng, fn, reads, writes)
        else:
            self.dma(eng, fn, slot, reads, writes)

    def commit_scheduled(self, ops):
        n = len(ops)
        deps = [set() for _ in range(n)]
        last_w, readers, last_slot = {}, {}, {}
        for i, it in enumerate(ops):
            kind, eng, fn, reads, writes, slot = it[:6]
            for b in reads:
                if id(b) in last_w:
                    deps[i].add(last_w[id(b)])
            for b in writes:
                if id(b) in last_w:
                    deps[i].add(last_w[id(b)])
                for r in readers.get(id(b), ()):
                    deps[i].add(r)
            if kind == "dma":
                if slot in last_slot:
                    deps[i].add(last_slot[slot])
                last_slot[slot] = i
            for b in reads:
                readers.setdefault(id(b), []).append(i)
            for b in writes:
                last_w[id(b)] = i
                readers[id(b)] = []
            deps[i].discard(i)
        users = [[] for _ in range(n)]
        indeg = [0] * n
        for i in range(n):
            indeg[i] = len(deps[i])
            for d_ in deps[i]:
                users[d_].append(i)
        t0 = max(self.eng_time.values()) if self.eng_time else 0.0
        eng_free = {e: max(self.eng_time.get(e, 0.0), t0 - 3.0) for e in ENGS}
        finish = [0.0] * n
        ready_t = [t0 - 3.0] * n
        ready = [i for i in range(n) if indeg[i] == 0]
        done = 0
        while ready:
            best, best_key, best_st = None, None, None
            for i in ready:
                st = max(eng_free[ops[i][1]], ready_t[i])
                pen = 0.0
                c_ = ops[i][6]
                if len(c_) > 2 and c_[2] is not None and c_[2] != self.act_tbl:
                    pen = max(0.0, 1.3 - TBL_AGE * max(0.0, eng_free["act"] - ready_t[i]))
                key = (st + pen, i)
                if best_key is None or key < best_key:
                    best, best_key, best_st = i, key, st
            i = best
            ready.remove(i)
            eng = ops[i][1]
            occ, lat = ops[i][6][0], ops[i][6][1]
            st = best_st
            c_ = ops[i][6]
            if len(c_) > 2 and c_[2] is not None:
                if c_[2] != self.act_tbl:
                    occ += 1.3
                self.act_tbl = c_[2]
            eng_free[eng] = st + occ
            finish[i] = st + occ + lat
            self.commit(ops[i])
            done += 1
            for u in users[i]:
                hop = 0.05 if (ops[u][1] == eng and ops[i][0] == "op") else 0.2
                ready_t[u] = max(ready_t[u], finish[i] + hop)
                indeg[u] -= 1
                if indeg[u] == 0:
                    ready.append(u)
        assert done == n
        self.eng_time = eng_free

    def commit_interleaved(self, a, b):
        ia = ib = 0
        na, nb = len(a), len(b)
        while ia < na or ib < nb:
            if ib >= nb or (ia < na and ia * nb <= ib * na):
                self.commit(a[ia]); ia += 1
            else:
                self.commit(b[ib]); ib += 1

    def _sem(self, key):
        if key not in self.sems:
            self.sems[key] = self.es.enter_context(self.nc.semaphore("s_" + key))
            self.count[key] = 0
        return self.sems[key]

    def sbuf(self, name, shape, dt):
        return self.es.enter_context(self.nc.sbuf_tensor("sb_" + name, list(shape), dt))

    def psum(self, name, shape, dt):
        return self.es.enter_context(self.nc.psum_tensor("ps_" + name, list(shape), dt))

    def _deps(self, eng, reads, writes):
        need = {}
        for b in reads:
            if b.writer is not None:
                k, v = b.writer
                need[k] = max(need.get(k, 0), v)
        for b in writes:
            if b.writer is not None:
                k, v = b.writer
                need[k] = max(need.get(k, 0), v)
            for (k, v) in b.readers:
                need[k] = max(need.get(k, 0), v)
        waits = []
        wd = self.waited[eng]
        for k, v in need.items():
            if eng == "pe" and k == "eng_pe":
                continue
            if wd.get(k, 0) >= v:
                continue
            wd[k] = v
            waits.append((self.sems[k], v))
        return waits

    def _commit(self, ticket, reads, writes):
        for b in reads:
            b.readers.append(ticket)
        for b in writes:
            b.writer = ticket
            b.readers = []

    def op(self, eng, fn, reads=(), writes=()):
        if self.rec is not None:
            self.rec.append(("op", eng, fn, tuple(reads), tuple(writes), None, _op_cost(eng, "op", fn)))
            return None
        waits = self._deps(eng, reads, writes)
        key = "eng_" + eng
        self.count[key] += 1
        ticket = (key, self.count[key])
        self.q[eng].append((waits, fn, (self.sems[key], 1)))
        self._commit(ticket, reads, writes)
        return ticket

    def dma(self, eng, fn, slot, reads=(), writes=()):
        if self.rec is not None:
            self.rec.append(("dma", eng, fn, tuple(reads), tuple(writes), slot, _op_cost(eng, "dma", fn)))
            return None
        waits = self._deps(eng, reads, writes)
        key = "dma_" + slot
        self._sem(key)
        prev = self.count[key]
        if prev > 0 and self.waited[eng].get(key, 0) < prev:
            self.waited[eng][key] = prev
            waits.append((self.sems[key], prev))
        self.count[key] += 16
        ticket = (key, self.count[key])
        self.q[eng].append((waits, fn, (self.sems[key], 16)))
        self._commit(ticket, reads, writes)
        return ticket

    def final_wait(self, eng, bufs):
        waits = self._deps(eng, (), bufs)
        self.q[eng].append((waits, None, None))

    def emit(self):
        nc = self.nc
        q = self.q

        def replay(e, lst):
            for waits, fn, inc in lst:
                for (s, v) in waits:
                    e.wait_ge(s, v)
                if fn is None:
                    continue
                ins = fn(e)
                if inc is not None:
                    ins.then_inc(inc[0], inc[1])

        with nc.Block() as blk:
            @blk.tensor
            def _(e):
                replay(e, q["pe"])

            @blk.scalar
            def _(e):
                replay(e, q["act"])

            @blk.vector
            def _(e):
                replay(e, q["dve"])

            @blk.gpsimd
            def _(e):
                replay(e, q["pool"])

            @blk.sync
            def _(e):
                replay(e, q["sp"])


def build(n_tiles=32, dbg=False, stop_after=None, sched=True, skip=None, dot_split=4):
    nc = bass.Bass("TRN2", target_bir_lowering=False)

    def din(name, shape, dt=F32):
        return nc.dram_tensor(name, list(shape), dt, kind="ExternalInput").ap()

    x_d = din("x", [SEQ, D])
    c_d = din("c_t", [128, 8])
    wada_d = din("w_ada", [D, 6 * D])
    bada_d = din("b_ada", [1, 6 * D])
    n1g_d = din("norm1_g", [1, D])
    n2g_d = din("norm2_g", [1, D])
    win_d = din("w_in", [D, 2304])
    cw_d = din("conv_w_t", [128, 12])
    qg_d = din("qg_t", [128, 1])
    kg_d = din("kg_t", [128, 1])
    sinks_d = din("sinks", [1, 8])
    rb_d = din("rel_bias", [1, 256])
    cg_d = din("conv_g_t", [128, 4])
    ag_d = din("attn_g_t", [128, 4])
    wout_d = din("w_out", [D, D])
    wq_d = din("peer_wq", [D, 2048])
    keysT_d = din("keysT", [128, 16 * 128])
    uv_d = din("uv", [16384, 2048])
    ident_d = din("ident", [128, 128])
    bones_d = din("blockones", [128, 128])
    iota_d = din("iota16", [128, 16])
    ohs_d = din("ohs", [32, 128, 256])
    negm_d = din("negmask", [128, 256])
    y_d = nc.dram_tensor("y", [SEQ, D], F32, kind="ExternalOutput").ap()
    dbg_d = {}

    def dbg_out(name, shape, dt=F32):
        dbg_d[name] = nc.dram_tensor("dbg_" + name, list(shape), dt, kind="ExternalOutput").ap()
        return dbg_d[name]

    with ExitStack() as es:
        P = Prog(nc, es)
        _bufs = {}
        tap_bufs = []
        _regs = {}

        def bc_reg(e):
            if isinstance(e, _Spy):
                return 16383
            if "bc" not in _regs:
                _regs["bc"] = e.to_reg(16383)
            return _regs["bc"]

        def dtap(name, ap, buf, shape, dt=F32):
            if not dbg:
                return
            od = dbg_out(name, shape, dt)
            P.dma("sp", lambda e: e.dma_start(out=od, in_=ap), "dbg_" + name, reads=[buf])
            tap_bufs.append(buf)

        def SB(name, shape, dt=F32):
            t = P.sbuf(name, shape, dt)
            b = Buf(name)
            _bufs[name] = b
            return t, b

        def PS(name, shape, dt=F32):
            t = P.psum(name, shape, dt)
            b = Buf(name)
            return t, b

        ident, b_ident = SB("ident", [128, 128], BF16)
        bones, b_bones = SB("bones", [128, 128], BF16)
        iota16, b_iota = SB("iota16", [128, 16])
        Win, b_Win = SB("Win", [128, 8, 2304], BF16)
        Wk2, b_Wk2 = SB("Wk2", [128, 8, 256], BF16)
        Wout, b_Wout = SB("Wout", [128, 8, 1024], BF16)
        Wq, b_Wq = SB("Wq", [128, 8, 2048], BF16)
        keysT, b_keysT = SB("keysT", [128, 16, 128], BF16)
        bc, b_bc = SB("bc", [128, 4 * D])
        bias_all, b_bias = SB("bias_all", [128, 8, 256])
        cw, b_cw = SB("cw", [128, 12])
        qg, b_qg = SB("qg", [128, 1])
        kg, b_kg = SB("kg", [128, 1])
        cg, b_cg = SB("cg", [128, 4])
        ag, b_ag = SB("ag", [128, 4])
        sink_b, b_sink = SB("sink_b", [128, 8])
        rb_b, b_rb = SB("rb_b", [128, 256])
        c_t, b_ct = SB("c_t", [128, 8])
        cond, b_cond = SB("cond", [128, 8])
        brow = [SB("brow%d" % i, [1, 128]) for i in range(2)]
        mrow = [SB("mrow%d" % i, [1, 128]) for i in range(2)]
        ones_row, b_ones = SB("ones_row", [1, 128])

        tmpf, b_tmpf = SB("tmpf", [128, D])
        g1_tmp, b_g1tmp = tmpf, b_tmpf
        NG = 7
        gb = [SB("gb%d" % i, [128, 2048], BF16) for i in range(NG)]
        xs = [SB("xs%d" % i, [128, D]) for i in range(2)]
        stg = xs
        pr, b_pr = SB("pr", [128, 18, 128])
        h2bs = [SB("h2b%d" % i, [128, D], BF16) for i in range(2)]
        g2_tmp, b_g2tmp = pr[:].rearrange("p c n -> p (c n)")[:, 0:D], b_pr
        uvb_d = nc.dram_tensor("uvb", [16384, 2048], BF16, kind="Internal").ap()
        b_uvb_u = [Buf("uvb_u%d" % i) for i in range(4)]
        b_uvb_v = [Buf("uvb_v%d" % i) for i in range(6)]
        b_uvb_all = b_uvb_u + b_uvb_v

        PA, b_PA = PS("PA", [128, 512])
        PB, b_PB = PS("PB", [128, 512])
        PC, b_PC = PS("PC", [128, 512])
        PD, b_PD = PS("PD", [128, 512])
        PT, b_PT = PS("PT", [128, 1024], BF16)
        PT2, b_PT2 = PT, b_PT
        acc0, b_acc0 = PS("acc0", [128, 512])
        acc1, b_acc1 = PS("acc1", [128, 512])
        PSc, b_PSc0 = PS("PSc", [128, 2, 256])
        b_PSc = [b_PSc0, b_PSc0]
        PO, b_PO = PD, b_PD

        win_v = win_d.rearrange("(kc p) n -> p kc n", p=128)
        wout_v = wout_d.rearrange("(kc p) n -> p kc n", p=128)
        wq_v = wq_d.rearrange("(kc p) n -> p kc n", p=128)
        wada_v = wada_d.rearrange("(kc p) n -> p kc n", p=128)

        def ld(eng, out_ap, in_ap, slot, wbuf):
            P.dma(eng, lambda e: e.dma_start(out=out_ap, in_=in_ap), slot, writes=[wbuf])

        ld("pool", ident[:], ident_d, "c0", b_ident)
        ld("pool", bones[:], bones_d, "c1", b_bones)
        ld("sp", iota16[:], iota_d, "c2", b_iota)
        ld("sp", cw[:], cw_d, "c3", b_cw)
        ld("sp", qg[:], qg_d, "c4", b_qg)
        ld("sp", kg[:], kg_d, "c5", b_kg)
        ld("sp", cg[:], cg_d, "c6", b_cg)
        ld("sp", ag[:], ag_d, "c7", b_ag)
        ld("sp", c_t[:], c_d, "c8", b_ct)
        ld("sp", sink_b[:], sinks_d.partition_broadcast(128), "c9", b_sink)
        ld("sp", rb_b[:], rb_d.partition_broadcast(128), "c10", b_rb)
        for kc in range(8):
            ld("pool", Win[:, kc, :], win_v[:, kc, :], "w%d" % (kc % 4), b_Win)
        for kv in range(2):
            for r in range(2):
                ld("pool", Wk2[:, :, kv * 128 + r * 64: kv * 128 + r * 64 + 64],
                   win_v[:, :, 2048 + kv * 64: 2048 + kv * 64 + 64], "w%d" % (kv * 2 + r), b_Wk2)
        for kc in range(8):
            ld("pool", Wout[:, kc, :], wout_v[:, kc, :], "w%d" % (kc % 4), b_Wout)
        for kc in range(8):
            ld("pool", Wq[:, kc, :], wq_v[:, kc, :], "w%d" % (kc % 4), b_Wq)
        ld("pool", keysT[:].rearrange("p c n -> p (c n)"), keysT_d, "w0", b_keysT)
        CR = 256
        for cidx in range(16384 // CR):
            P.dma("pool", lambda e, cidx=cidx: e.dma_start(
                out=uvb_d[cidx * CR:(cidx + 1) * CR, 0:D], in_=uv_d[cidx * CR:(cidx + 1) * CR, 0:D]),
                "cv%d" % (cidx % 4), writes=[b_uvb_u[cidx % 4]])

        P.op("act", lambda e: e.activation(out=cond[:], in_=c_t[:], func=AF.Silu),
             reads=[b_ct], writes=[b_cond])
        P.op("dve", lambda e: e.memset(ones_row[:], 1.0), writes=[b_ones])
        CW = 128
        NCH = 6 * D // CW
        for ch in range(NCH):
            g_t, g_b = stg[ch % 2]
            wv = g_t[:].rearrange("p (k n) -> p k n", k=8)
            ld("sp", wv, wada_v[:, :, ch * CW:(ch + 1) * CW], "ada%d" % (ch % 2), g_b)
            br_t, br_b = brow[ch % 2]
            mr_t, mr_b = mrow[ch % 2]
            ld("sp", br_t[:, 0:CW], bada_d[:, ch * CW:(ch + 1) * CW], "bada%d" % (ch % 2), br_b)
            pbank, pbuf = (PA, b_PA) if ch % 2 == 0 else (PB, b_PB)
            for kc in range(8):
                P.op("pe", lambda e, kc=kc, wv=wv, pbank=pbank: e.matmul(
                    pbank[0:1, 0:CW], lhsT=cond[:, kc:kc + 1], rhs=wv[:, kc, :],
                    start=(kc == 0), stop=(kc == 7)),
                    reads=[b_cond, g_b], writes=[pbuf])
            P.op("dve", lambda e, pbank=pbank, br_t=br_t, mr_t=mr_t: e.tensor_tensor(
                out=mr_t[:, 0:CW], in0=pbank[0:1, 0:CW], in1=br_t[:, 0:CW], op=ALU.add),
                reads=[pbuf, br_b], writes=[mr_b])
            pbank2, pbuf2 = (PC, b_PC) if ch % 2 == 0 else (PD, b_PD)
            P.op("pe", lambda e, pbank2=pbank2, mr_t=mr_t: e.matmul(
                pbank2[:, 0:CW], lhsT=ones_row[0:1, :], rhs=mr_t[0:1, 0:CW],
                start=True, stop=True), reads=[b_ones, mr_b], writes=[pbuf2])
            col0 = ch * CW
            if col0 < 2 * D:
                dst = bc[:, col0:col0 + CW]
                dbuf = b_bc
            elif col0 < 3 * D:
                dst = g1_tmp[:, col0 - 2 * D:col0 - 2 * D + CW]
                dbuf = b_g1tmp
            elif col0 < 5 * D:
                dst = bc[:, col0 - D:col0 - D + CW]
                dbuf = b_bc
            else:
                dst = g2_tmp[:, col0 - 5 * D:col0 - 5 * D + CW]
                dbuf = b_g2tmp
            P.op("act", lambda e, pbank2=pbank2, dst=dst: e.copy(out=dst, in_=pbank2[:, 0:CW]),
                 reads=[pbuf2], writes=[dbuf])
        sh1_b, sc1_b = bc[:, 0:D], bc[:, D:2 * D]
        sh2_b, sc2_b = bc[:, 2 * D:3 * D], bc[:, 3 * D:4 * D]
        for kc in range(8):
            P.op("dve", lambda e, kc=kc: e.tensor_tensor(
                out=Wout[:, kc, :], in0=Wout[:, kc, :], in1=g1_tmp[:, 0:D], op=ALU.mult),
                reads=[b_Wout, b_g1tmp], writes=[b_Wout])
        for (sc_ap, ng_d, slot) in ((sc1_b, n1g_d, 0), (sc2_b, n2g_d, 1)):
            g_t, g_b = stg[slot]
            ld("sp", g_t[:, 0:D], ng_d.partition_broadcast(128), "ng%d" % slot, g_b)
            P.op("dve", lambda e, sc_ap=sc_ap, g_t=g_t: e.scalar_tensor_tensor(
                out=sc_ap, in0=sc_ap, scalar=1.0, in1=g_t[:, 0:D], op0=ALU.add, op1=ALU.mult),
                reads=[b_bc, g_b], writes=[b_bc])
        P.op("dve", lambda e: e.tensor_scalar(out=qg[:], in0=qg[:], scalar1=0.125, scalar2=None,
                                              op0=ALU.mult), reads=[b_qg], writes=[b_qg])

        P.op("dve", lambda e: e.memset(bias_all[:], 0.0), writes=[b_bias])
        for b in range(32):
            g_t, g_b = stg[b % 2]
            ld("sp", g_t[:, 0:256], ohs_d[b], "oh%d" % (b % 2), g_b)
            for h in range(8):
                P.op("dve", lambda e, b=b, h=h, g_t=g_t: e.scalar_tensor_tensor(
                    out=bias_all[:, h, :], in0=g_t[:, 0:256], scalar=rb_b[:, b * 8 + h: b * 8 + h + 1],
                    in1=bias_all[:, h, :], op0=ALU.mult, op1=ALU.add),
                    reads=[g_b, b_rb, b_bias], writes=[b_bias])
        g_t, g_b = stg[0]
        ld("sp", g_t[:, 0:256], negm_d, "oh0", g_b)
        for h in range(8):
            P.op("dve", lambda e, h=h, g_t=g_t: e.tensor_add(
                out=bias_all[:, h, :], in0=bias_all[:, h, :], in1=g_t[:, 0:256]),
                reads=[g_b, b_bias], writes=[b_bias])

        vin = [stg[0], stg[1], (tmpf, b_tmpf)]
        vout = [h2bs[0], h2bs[1]] + [(gb[i][0][:, 0:D], Buf("vo_a%d" % i)) for i in range(2)] \
            + [(gb[i][0][:, D:2 * D], Buf("vo_b%d" % i)) for i in range(2)]
        for cidx in range(128):
            s_t, s_b = vin[cidx % 3]
            o_t, o_b = vout[cidx % 6]
            ld("sp", s_t[:, :], uv_d[cidx * 128:(cidx + 1) * 128, D:2 * D], "vc%d" % (cidx % 3), s_b)
            P.op("dve", lambda e, s_t=s_t, o_t=o_t: e.tensor_tensor(out=o_t[:, :], in0=s_t[:, :], in1=g2_tmp, op=ALU.mult),
                 reads=[s_b, b_g2tmp], writes=[o_b])
            P.dma("act", lambda e, cidx=cidx, o_t=o_t: e.dma_start(
                out=uvb_d[cidx * 128:(cidx + 1) * 128, D:2 * D], in_=o_t[:, :]), "vo%d" % (cidx % 6),
                reads=[o_b], writes=[b_uvb_v[cidx % 6]])

        jh, b_jh = SB("jh", [128, 2 * D], BF16)
        junkb, b_junkb = jh[:, 0:D], b_jh
        hb, b_hb = jh[:, D:2 * D], b_jh
        hT, b_hT = SB("hT", [128, 8, 128], BF16)
        a2, b_a2 = SB("a2", [128, 128])
        ga2, b_ga2 = SB("ga2", [128, 128])
        a2_bufs = [Buf("a2_%d" % i) for i in range(8)]
        ga2_bufs = [Buf("ga2_%d" % i) for i in range(8)]
        wgtfs = [SB("wgtf%d" % i, [128, 128]) for i in range(2)]
        dg = [SB("dg%d" % i, [128, 128], BF16) for i in range(4)]
        st1, b_st1 = SB("st1", [128, 4])
        vbuf = [SB("vbuf%d" % i, [128, 128], BF16) for i in range(2)]
        kTb = [SB("kTb%d" % i, [128, 2, 128], BF16) for i in range(2)]
        _ub = SB("ub", [128, 4, 130])
        ub = [_ub, _ub]
        ucar, b_ucar = SB("ucar", [128, 4, 2])
        yc, b_yc = SB("yc", [128, 4, 128])
        sqb, b_sqb = SB("sqb", [128, 6, 128], BF16)
        rst, b_rst = SB("rst", [128, 6, 128])
        mixT, b_mixT = hT, b_hT
        qT, b_qT = SB("qT", [128, 4, 128], BF16)
        s_sb = [SB("s_sb%d" % i, [128, 256]) for i in range(2)]
        p_sb = [SB("p_sb%d" % i, [128, 256], BF16) for i in range(2)]
        pT_sb = [SB("pT_sb%d" % i, [128, 2, 128], BF16) for i in range(2)]
        hst = [SB("hst%d" % i, [128, 8]) for i in range(2)]
        rden, b_rden = SB("rden", [128, 8])
        on, b_on = yc[:].rearrange("p c (a b) -> p (c a) b", b=64), b_yc
        onb, b_onb = SB("onb", [128, 512], BF16)
        ssa, b_ssa = SB("ssa", [128, 8])
        qTp, b_qTp = jh[:].rearrange("p (c n) -> p c n", c=16), b_jh
        S, b_S = pr[:, 0:16, :], b_pr
        _s2 = SB("S2_0", [128, 128])
        S2 = [_s2, _s2]
        V1, b_V1 = SB("V1", [128, 16, 16])
        I1, b_I1 = SB("I1", [128, 16, 16], U32)
        I1f, b_I1f = SB("I1f", [128, 16, 16])
        rstf = rst[:].rearrange("p c n -> p (c n)")
        ycf = yc[:].rearrange("p c n -> p (c n)")
        onf = on[:].rearrange("p c n -> p (c n)")
        cand = [(rstf[:, 0:256].rearrange("p (a b) -> p a b", a=16), b_rst),
                (rstf[:, 256:512].rearrange("p (a b) -> p a b", a=16), b_rst)]
        cand2 = [(rstf[:, 512:768], b_rst), (ycf[:, 0:256], b_yc)]
        T2, b_T2 = SB("T2", [128, 8, 16])
        pos, b_pos = SB("pos", [128, 8, 16], U32)
        k2u, b_k2u = SB("k2u", [128, 8, 16], U32)
        k1f, b_k1f = SB("k1f", [128, 8, 16])
        k2f, b_k2f = SB("k2f", [128, 8, 16])
        eq = [(ycf[:, 256:512].rearrange("p (a b) -> p a b", a=16), b_yc),
              (onf[:, 0:256].rearrange("p (a b) -> p a b", a=16), b_on)]
        ia_s, b_ia_s = SB("ia_s", [128, 8, 16])
        ib_s, b_ib_s = SB("ib_s", [128, 8, 16])
        eidf, b_eidf = s_sb[0][0][:, 0:128], s_sb[0][1]
        eidis = [SB("eidi%d" % i, [128, 128], I32) for i in range(2)]
        wgt, b_wgt = s_sb[1][0][:, 128:256].rearrange("p (h k) -> p h k", h=8), s_sb[1][1]
        wsum, b_wsum = SB("wsum", [128, 8])
        negt, b_negt = SB("negt", [128, 8])
        a_sb, b_a = s_sb[0][0][:, 128:256], s_sb[0][1]
        ga_sb, b_ga = s_sb[1][0][:, 0:128], s_sb[1][1]
        acc, b_acc = tmpf, b_tmpf

        def rms_stats(src_ap, src_buf, n_free, out_col_ap, out_buf):
            P.op("act", lambda e: e.activation(out=junkb[:, 0:n_free], in_=src_ap, func=AF.Square,
                                               accum_out=out_col_ap),
                 reads=[src_buf], writes=[b_junkb, out_buf])
            rsqrt_inplace(out_col_ap, out_buf, 1.0 / n_free)

        def rsqrt_inplace(ap, buf, scale, src_ap=None, src_buf=None):
            s_ap = ap if src_ap is None else src_ap
            rd = [buf] if src_buf is None else [src_buf]
            P.op("dve", lambda e: e.tensor_scalar(out=ap, in0=s_ap, scalar1=scale, scalar2=EPS,
                                                  op0=ALU.mult, op1=ALU.add), reads=rd, writes=[buf])
            P.op("act", lambda e: e.activation(out=ap, in_=ap, func=AF.Ln), reads=[buf], writes=[buf])
            P.op("act", lambda e: e.activation(out=ap, in_=ap, func=AF.Exp, scale=-0.5), reads=[buf], writes=[buf])

        def transpose8(src, src_buf, dst, dst_buf, nchunk=8, ptile=None, pbuf=None):
            ptile = PT if ptile is None else ptile
            pbuf = b_PT if pbuf is None else pbuf
            for kc in range(nchunk):
                P.op("pe", lambda e, kc=kc: e.transpose(
                    out=ptile[:, kc * 128:(kc + 1) * 128], in_=src[:, kc * 128:(kc + 1) * 128],
                    identity=ident[:]), reads=[src_buf, b_ident], writes=[pbuf])
            if dst is not None:
                P.op("act", lambda e: e.copy(out=dst[:].rearrange("p c n -> p (c n)"),
                                             in_=ptile[:, 0:nchunk * 128]),
                     reads=[pbuf], writes=[dst_buf])

        out_bufs = []

        def front(n):
            xt, b_xt = xs[n % 2]
            x1, b_x1 = xt, b_xt
            cur, prv = n % 2, (n - 1) % 2
            h2b, b_h2b = h2bs[cur]
            eidi, b_eidi = eidis[cur]
            wgtf, b_wgtf = wgtfs[cur]
            ld("sp", xt[:], x_d[n * T:(n + 1) * T, :], "x%d" % cur, b_xt)

            rms_stats(xt[:], b_xt, D, st1[:, 0:1], b_st1)
            P.op("dve", lambda e, xt=xt: e.scalar_tensor_tensor(
                out=tmpf[:], in0=xt[:], scalar=st1[:, 0:1], in1=sc1_b, op0=ALU.mult, op1=ALU.mult),
                reads=[b_xt, b_st1, b_bc], writes=[b_tmpf])
            P.op("dve", lambda e: e.tensor_add(out=hb[:], in0=tmpf[:], in1=sh1_b),
                 reads=[b_tmpf, b_bc], writes=[b_hb])
            if n == 0:
                dtap("hb", hb[:], b_hb, [128, D], BF16)
                dtap("bc", bc[:], b_bc, [128, 4 * D])
            transpose8(hb, b_hb, hT, b_hT)
            if n == 0:
                dtap("hT", hT[:], b_hT, [128, 8, 128], BF16)

            def wcols(j):
                if j < 16:
                    return Win, b_Win, j * 128
                return Wk2, b_Wk2, (j - 16) * 128
            for g in range(5):
                pbank, pbuf = (PA, b_PA) if g % 2 == 0 else (PB, b_PB)
                chunks = list(range(g * 4, min(g * 4 + 4, 18)))
                for i, j in enumerate(chunks):
                    wt, wb, c0 = wcols(j)
                    for kc in range(8):
                        P.op("pe", lambda e, i=i, kc=kc, wt=wt, c0=c0, pbank=pbank: e.matmul(
                            pbank[:, i * 128:(i + 1) * 128], lhsT=wt[:, kc, c0:c0 + 128], rhs=hT[:, kc, :],
                            start=(kc == 0), stop=(kc == 7)), reads=[wb, b_hT], writes=[pbuf])
                nn = len(chunks)
                P.op("act", lambda e, g=g, nn=nn, pbank=pbank: e.copy(
                    out=pr[:, g * 4:g * 4 + nn, :].rearrange("p c n -> p (c n)"), in_=pbank[:, 0:nn * 128]),
                    reads=[pbuf], writes=[b_pr])
            vt, b_vt = vbuf[cur]
            for kc in range(8):
                P.op("pe", lambda e, kc=kc: e.matmul(
                    PC[:, 0:128], lhsT=hT[:, kc, :], rhs=Win[:, kc, 2176:2304],
                    start=(kc == 0), stop=(kc == 7)), reads=[b_Win, b_hT], writes=[b_PC])
            P.op("act", lambda e, vt=vt: e.copy(out=vt[:], in_=PC[:, 0:128]), reads=[b_PC], writes=[b_vt])

            if n == 0:
                dtap("pr", pr[:], b_pr, [128, 18, 128])
                dtap("vt", vt[:], b_vt, [128, 128], BF16)
            ut, b_ut = ub[cur]
            upt, b_upt = ub[prv]
            if n == 0:
                P.op("dve", lambda e, ut=ut: e.memset(ut[:, :, 0:2], 0.0), writes=[b_ut])
            else:
                P.op("dve", lambda e, ut=ut: e.tensor_copy(out=ut[:, :, 0:2], in_=ucar[:]),
                     reads=[b_ucar], writes=[b_ut])
            P.op("dve", lambda e, ut=ut: e.tensor_tensor(
                out=ut[:, :, 2:130], in0=pr[:, 4:8, :], in1=pr[:, 8:12, :], op=ALU.mult),
                reads=[b_pr], writes=[b_ut])
            for j in range(4):
                P.op("dve", lambda e, j=j, ut=ut: e.tensor_scalar(
                    out=yc[:, j, :], in0=ut[:, j, 2:130], scalar1=cw[:, j * 3 + 2:j * 3 + 3], scalar2=None,
                    op0=ALU.mult), reads=[b_ut, b_cw], writes=[b_yc])
                for tap in (1, 0):
                    P.op("dve", lambda e, j=j, tap=tap, ut=ut: e.scalar_tensor_tensor(
                        out=yc[:, j, :], in0=ut[:, j, tap:tap + 128], scalar=cw[:, j * 3 + tap:j * 3 + tap + 1],
                        in1=yc[:, j, :], op0=ALU.mult, op1=ALU.add), reads=[b_ut, b_cw, b_yc], writes=[b_yc])
            P.op("dve", lambda e: e.tensor_tensor(out=yc[:], in0=yc[:], in1=pr[:, 0:4, :], op=ALU.mult),
                 reads=[b_yc, b_pr], writes=[b_yc])
            P.op("dve", lambda e, ut=ut: e.tensor_copy(out=ucar[:], in_=ut[:, :, 128:130]),
                 reads=[b_ut], writes=[b_ucar])
            P.op("act", lambda e: e.activation(out=sqb[:, 0:4, :], in_=yc[:], func=AF.Square),
                 reads=[b_yc], writes=[b_sqb])
            for j in range(4):
                P.op("pe", lambda e, j=j: e.matmul(PD[:, j * 128:(j + 1) * 128], lhsT=bones[:], rhs=sqb[:, j, :],
                                                   start=True, stop=True), reads=[b_bones, b_sqb], writes=[b_PD])
            rsqrt_inplace(rst[:, 0:4, :].rearrange("p c n -> p (c n)"), b_rst, 1.0 / 64,
                          src_ap=PD[:, 0:512], src_buf=b_PD)
            for j in range(4):
                P.op("dve", lambda e, j=j: e.scalar_tensor_tensor(
                    out=mixT[:, j, :], in0=yc[:, j, :], scalar=cg[:, j:j + 1], in1=rst[:, j, :],
                    op0=ALU.mult, op1=ALU.mult), reads=[b_yc, b_cg, b_rst], writes=[b_mixT])

            P.op("act", lambda e: e.activation(out=sqb[:], in_=pr[:, 12:18, :], func=AF.Square),
                 reads=[b_pr], writes=[b_sqb])
            for j in range(4):
                P.op("pe", lambda e, j=j: e.matmul(PD[:, j * 128:(j + 1) * 128], lhsT=bones[:], rhs=sqb[:, j, :],
                                                   start=True, stop=True), reads=[b_bones, b_sqb], writes=[b_PD])
            rsqrt_inplace(rst[:, 0:4, :].rearrange("p c n -> p (c n)"), b_rst, 1.0 / 64,
                          src_ap=PD[:, 0:512], src_buf=b_PD)
            for j in range(2):
                P.op("pe", lambda e, j=j: e.matmul(PC[:, j * 128:(j + 1) * 128], lhsT=bones[:], rhs=sqb[:, 4 + j, :],
                                                   start=True, stop=True), reads=[b_bones, b_sqb], writes=[b_PC])
            rsqrt_inplace(rst[:, 4:6, :].rearrange("p c n -> p (c n)"), b_rst, 1.0 / 64,
                          src_ap=PC[:, 0:256], src_buf=b_PC)
            for j in range(4):
                P.op("dve", lambda e, j=j: e.scalar_tensor_tensor(
                    out=qT[:, j, :], in0=pr[:, 12 + j, :], scalar=qg[:, 0:1], in1=rst[:, j, :],
                    op0=ALU.mult, op1=ALU.mult), reads=[b_pr, b_qg, b_rst], writes=[b_qT])
            kt, b_kt = kTb[cur]
            kpt, b_kpt = kTb[prv]
            for j in range(2):
                P.op("dve", lambda e, j=j, kt=kt: e.scalar_tensor_tensor(
                    out=kt[:, j, :], in0=pr[:, 16 + j, :], scalar=kg[:, 0:1], in1=rst[:, 4 + j, :],
                    op0=ALU.mult, op1=ALU.mult), reads=[b_pr, b_kg, b_rst], writes=[b_kt])

            if n == 0:
                dtap("yc", yc[:], b_yc, [128, 4, 128])
                dtap("mixT_conv", mixT[:, 0:4, :], b_mixT, [128, 4, 128], BF16)
                dtap("qT", qT[:], b_qT, [128, 4, 128], BF16)
                dtap("kT", kt[:], b_kt, [128, 2, 128], BF16)
            vpt, b_vpt = vbuf[prv]
            for h in range(8):
                kv, r, qc = h // 4, h % 2, h // 2
                sl = h % 2
                pl, ph = 64 * r, 64 * r + 64
                st_, b_s = s_sb[sl]
                pt_, b_p = p_sb[sl]
                pTt, b_pTt = pT_sb[sl]
                hs, b_hs = hst[sl]
                c_lo = 0 if n > 0 else 128
                if n > 0:
                    P.op("pe", lambda e, sl=sl, qc=qc, kv=kv, pl=pl, ph=ph, kpt=kpt: e.matmul(
                        PSc[:, sl, 0:128], lhsT=qT[pl:ph, qc, :], rhs=kpt[pl:ph, kv, :], start=True, stop=True),
                        reads=[b_qT, b_kpt], writes=[b_PSc[sl]])
                P.op("pe", lambda e, sl=sl, qc=qc, kv=kv, pl=pl, ph=ph, kt=kt: e.matmul(
                    PSc[:, sl, 128:256], lhsT=qT[pl:ph, qc, :], rhs=kt[pl:ph, kv, :], start=True, stop=True),
                    reads=[b_qT, b_kt], writes=[b_PSc[sl]])
                P.op("dve", lambda e, sl=sl, h=h, st_=st_, c_lo=c_lo: e.tensor_tensor(
                    out=st_[:, c_lo:256], in0=PSc[:, sl, c_lo:256], in1=bias_all[:, h, c_lo:256], op=ALU.add),
                    reads=[b_PSc[sl], b_bias], writes=[b_s])
                P.op("dve", lambda e, st_=st_, hs=hs, c_lo=c_lo: e.reduce_max(
                    out=hs[:, 0:1], in_=st_[:, c_lo:256], axis=AX.X), reads=[b_s], writes=[b_hs])
                P.op("dve", lambda e, hs=hs, h=h: e.tensor_scalar(
                    out=hs[:, 1:2], in0=hs[:, 0:1], scalar1=sink_b[:, h:h + 1], scalar2=-1.0,
                    op0=ALU.max, op1=ALU.mult), reads=[b_hs, b_sink], writes=[b_hs])
                P.op("act", lambda e, st_=st_, pt_=pt_, hs=hs, c_lo=c_lo: e.activation(
                    out=pt_[:, c_lo:256], in_=st_[:, c_lo:256], func=AF.Exp, bias=hs[:, 1:2],
                    accum_out=hs[:, 2:3]), reads=[b_s, b_hs], writes=[b_p, b_hs])
                P.op("act", lambda e, hs=hs, h=h: e.activation(
                    out=hs[:, 3:4], in_=sink_b[:, h:h + 1], func=AF.Exp, bias=hs[:, 1:2]),
                    reads=[b_sink, b_hs], writes=[b_hs])
                P.op("dve", lambda e, hs=hs: e.tensor_add(out=hs[:, 4:5], in0=hs[:, 2:3], in1=hs[:, 3:4]),
                     reads=[b_hs], writes=[b_hs])
                P.op("dve", lambda e, hs=hs, h=h: e.reciprocal(out=rden[:, h:h + 1], in_=hs[:, 4:5]),
                     reads=[b_hs], writes=[b_rden])
                halves = (0, 1) if n > 0 else (1,)
                for hf in halves:
                    P.op("pe", lambda e, hf=hf, sl=sl, pt_=pt_: e.transpose(
                        out=PT2[:, (sl * 2 + hf) * 128:(sl * 2 + hf + 1) * 128], in_=pt_[:, hf * 128:(hf + 1) * 128],
                        identity=ident[:]), reads=[b_p, b_ident], writes=[b_PT2])
                lo = 0 if n > 0 else 1
                P.op("act", lambda e, sl=sl, pTt=pTt, lo=lo: e.copy(
                    out=pTt[:, lo:2, :].rearrange("p c n -> p (c n)"),
                    in_=PT2[:, (sl * 2 + lo) * 128:(sl * 2 + 2) * 128]), reads=[b_PT2], writes=[b_pTt])
                if n > 0:
                    P.op("pe", lambda e, h=h, kv=kv, pTt=pTt, vpt=vpt: e.matmul(
                        PO[:, h * 64:(h + 1) * 64], lhsT=pTt[:, 0, :], rhs=vpt[:, kv * 64:(kv + 1) * 64],
                        start=True, stop=False), reads=[b_pTt, b_vpt], writes=[b_PO])
                P.op("pe", lambda e, h=h, kv=kv, pTt=pTt, vt=vt, first=(n == 0): e.matmul(
                    PO[:, h * 64:(h + 1) * 64], lhsT=pTt[:, 1, :], rhs=vt[:, kv * 64:(kv + 1) * 64],
                    start=first, stop=True), reads=[b_pTt, b_vt], writes=[b_PO])
            for h in range(8):
                P.op("dve", lambda e, h=h: e.tensor_scalar(
                    out=on[:, h, :], in0=PO[:, h * 64:(h + 1) * 64], scalar1=rden[:, h:h + 1], scalar2=None,
                    op0=ALU.mult), reads=[b_PO, b_rden], writes=[b_on])
            for h in range(8):
                P.op("act", lambda e, h=h: e.activation(
                    out=junkb[:, 0:64], in_=on[:, h, :], func=AF.Square, accum_out=ssa[:, h:h + 1]),
                    reads=[b_on], writes=[b_junkb, b_ssa])
            rsqrt_inplace(ssa[:], b_ssa, 1.0 / 64)
            for h in range(8):
                P.op("dve", lambda e, h=h: e.tensor_scalar(
                    out=onb[:, h * 64:(h + 1) * 64], in0=on[:, h, :], scalar1=ssa[:, h:h + 1], scalar2=None,
                    op0=ALU.mult), reads=[b_on, b_ssa], writes=[b_onb])
            transpose8(onb, b_onb, None, None, nchunk=4)
            for j in range(4):
                P.op("dve", lambda e, j=j: e.tensor_scalar(
                    out=mixT[:, 4 + j, :], in0=PT[:, j * 128:(j + 1) * 128], scalar1=ag[:, j:j + 1], scalar2=None,
                    op0=ALU.mult), reads=[b_PT, b_ag], writes=[b_mixT])

            if n == 0:
                dtap("on", on[:], b_on, [128, 8, 64])
                dtap("rden", rden[:], b_rden, [128, 8])
                dtap("mixT", mixT[:], b_mixT, [128, 8, 128], BF16)
            for half in range(2):
                pbank, pbuf = (PA, b_PA) if half == 0 else (PB, b_PB)
                for c in range(8):
                    P.op("pe", lambda e, c=c, half=half, pbank=pbank: e.matmul(
                        pbank[:, :], lhsT=mixT[:, c, :], rhs=Wout[:, c, half * 512:(half + 1) * 512],
                        start=(c == 0), stop=(c == 7)), reads=[b_mixT, b_Wout], writes=[pbuf])
                P.op("dve", lambda e, half=half, pbank=pbank, xt=xt: e.tensor_tensor(
                    out=x1[:, half * 512:(half + 1) * 512], in0=pbank[:, :],
                    in1=xt[:, half * 512:(half + 1) * 512], op=ALU.add),
                    reads=[pbuf, b_xt], writes=[b_x1])

            rms_stats(x1[:], b_x1, D, st1[:, 1:2], b_st1)
            P.op("dve", lambda e: e.scalar_tensor_tensor(
                out=tmpf[:], in0=x1[:], scalar=st1[:, 1:2], in1=sc2_b, op0=ALU.mult, op1=ALU.mult),
                reads=[b_x1, b_st1, b_bc], writes=[b_tmpf])
            P.op("dve", lambda e: e.tensor_add(out=h2b[:], in0=tmpf[:], in1=sh2_b),
                 reads=[b_tmpf, b_bc], writes=[b_h2b])
            transpose8(h2b, b_h2b, hT, b_hT)

            for g in range(4):
                pbank, pbuf = (PA, b_PA) if g % 2 == 0 else (PB, b_PB)
                for i in range(4):
                    c = g * 4 + i
                    for kc in range(8):
                        P.op("pe", lambda e, i=i, c=c, kc=kc, pbank=pbank: e.matmul(
                            pbank[:, i * 128:(i + 1) * 128], lhsT=Wq[:, kc, c * 128:(c + 1) * 128], rhs=hT[:, kc, :],
                            start=(kc == 0), stop=(kc == 7)), reads=[b_Wq, b_hT], writes=[pbuf])
                P.op("act", lambda e, g=g, pbank=pbank: e.copy(
                    out=qTp[:, g * 4:(g + 1) * 4, :].rearrange("p c n -> p (c n)"), in_=pbank[:, :]),
                    reads=[pbuf], writes=[b_qTp])
            for g in range(4):
                pbank, pbuf = (PC, b_PC) if g % 2 == 0 else (PD, b_PD)
                for i in range(4):
                    c = g * 4 + i
                    P.op("pe", lambda e, i=i, c=c, pbank=pbank: e.matmul(
                        pbank[:, i * 128:(i + 1) * 128], lhsT=qTp[:, c, :], rhs=keysT[:, c, :],
                        start=True, stop=True), reads=[b_qTp, b_keysT], writes=[pbuf])
                P.op("act", lambda e, g=g, pbank=pbank: e.copy(
                    out=S[:, g * 4:(g + 1) * 4, :].rearrange("p c n -> p (c n)"), in_=pbank[:, :]),
                    reads=[pbuf], writes=[b_S])

            for c in range(16):
                s2t, b_s2 = S2[c % 2]
                P.op("dve", lambda e, c=c: e.max(out=V1[:, c, 0:8], in_=S[:, c, :]), reads=[b_S], writes=[b_V1])
                P.op("dve", lambda e, c=c: e.max_index(out=I1[:, c, 0:8], in_max=V1[:, c, 0:8], in_values=S[:, c, :]),
                     reads=[b_S, b_V1], writes=[b_I1])
                P.op("dve", lambda e, c=c, s2t=s2t: e.match_replace(
                    out=s2t[:], in_to_replace=V1[:, c, 0:8], in_values=S[:, c, :], imm_value=-1e30),
                    reads=[b_S, b_V1], writes=[b_s2])
                P.op("dve", lambda e, c=c, s2t=s2t: e.max(out=V1[:, c, 8:16], in_=s2t[:]), reads=[b_s2], writes=[b_V1])
                P.op("dve", lambda e, c=c, s2t=s2t: e.max_index(
                    out=I1[:, c, 8:16], in_max=V1[:, c, 8:16], in_values=s2t[:]),
                    reads=[b_s2, b_V1], writes=[b_I1])
            P.op("dve", lambda e: e.tensor_copy(out=I1f[:], in_=I1[:]), reads=[b_I1], writes=[b_I1f])

            for h in range(8):
                ct, b_c = cand[h % 2]
                c2t, b_c2 = cand2[h % 2]
                P.op("dve", lambda e, h=h, ct=ct: e.tensor_tensor(
                    out=ct[:], in0=V1[:, 2 * h, :].unsqueeze(2).to_broadcast([128, 16, 16]),
                    in1=V1[:, 2 * h + 1, :].unsqueeze(1).to_broadcast([128, 16, 16]), op=ALU.add),
                    reads=[b_V1], writes=[b_c])
                cf = ct[:].rearrange("p a b -> p (a b)")
                P.op("dve", lambda e, h=h, cf=cf: e.max(out=T2[:, h, 0:8], in_=cf), reads=[b_c], writes=[b_T2])
                P.op("dve", lambda e, h=h, cf=cf: e.max_index(out=pos[:, h, 0:8], in_max=T2[:, h, 0:8], in_values=cf),
                     reads=[b_c, b_T2], writes=[b_pos])
                P.op("dve", lambda e, h=h, cf=cf, c2t=c2t: e.match_replace(
                    out=c2t[:], in_to_replace=T2[:, h, 0:8], in_values=cf, imm_value=-1e30),
                    reads=[b_c, b_T2], writes=[b_c2])
                P.op("dve", lambda e, h=h, c2t=c2t: e.max(out=T2[:, h, 8:16], in_=c2t[:]), reads=[b_c2], writes=[b_T2])
                P.op("dve", lambda e, h=h, c2t=c2t: e.max_index(
                    out=pos[:, h, 8:16], in_max=T2[:, h, 8:16], in_values=c2t[:]),
                    reads=[b_c2, b_T2], writes=[b_pos])
            P.op("dve", lambda e: e.tensor_single_scalar(out=k2u[:], in_=pos[:], scalar=15, op=ALU.bitwise_and),
                 reads=[b_pos], writes=[b_k2u])
            P.op("dve", lambda e: e.tensor_single_scalar(out=pos[:], in_=pos[:], scalar=4, op=ALU.logical_shift_right),
                 reads=[b_pos], writes=[b_pos])
            P.op("dve", lambda e: e.tensor_copy(out=k1f[:], in_=pos[:]), reads=[b_pos], writes=[b_k1f])
            P.op("dve", lambda e: e.tensor_copy(out=k2f[:], in_=k2u[:]), reads=[b_k2u], writes=[b_k2f])
            cnt = 0
            for h in range(8):
                for side, (kf, b_kf, dst, b_dst) in enumerate(((k1f, b_k1f, ia_s, b_ia_s), (k2f, b_k2f, ib_s, b_ib_s))):
                    et, b_e = eq[cnt % 2]
                    cnt += 1
                    P.op("dve", lambda e, h=h, kf=kf, et=et: e.tensor_tensor(
                        out=et[:], in0=iota16[:, :].unsqueeze(1).to_broadcast([128, 16, 16]),
                        in1=kf[:, h, :].unsqueeze(2).to_broadcast([128, 16, 16]), op=ALU.is_equal),
                        reads=[b_iota, b_kf], writes=[b_e])
                    P.op("dve", lambda e, h=h, side=side, et=et: e.tensor_tensor(
                        out=et[:], in0=et[:],
                        in1=I1f[:, 2 * h + side, :].unsqueeze(1).to_broadcast([128, 16, 16]), op=ALU.mult),
                        reads=[b_e, b_I1f], writes=[b_e])
                    P.op("dve", lambda e, h=h, dst=dst, et=et: e.reduce_sum(
                        out=dst[:, h, :], in_=et[:], axis=AX.X), reads=[b_e], writes=[b_dst])
            P.op("dve", lambda e: e.scalar_tensor_tensor(
                out=eidf[:], in0=ia_s[:].rearrange("p h k -> p (h k)"), scalar=128.0,
                in1=ib_s[:].rearrange("p h k -> p (h k)"), op0=ALU.mult, op1=ALU.add),
                reads=[b_ia_s, b_ib_s], writes=[b_eidf])
            P.op("dve", lambda e: e.tensor_copy(out=eidi[:], in_=eidf[:]), reads=[b_eidf], writes=[b_eidi])
            P.op("dve", lambda e: e.tensor_scalar(out=negt[:], in0=T2[:, :, 0], scalar1=-1.0, scalar2=None,
                                                  op0=ALU.mult), reads=[b_T2], writes=[b_negt])
            for h in range(8):
                P.op("act", lambda e, h=h: e.activation(
                    out=wgt[:, h, :], in_=T2[:, h, :], func=AF.Exp, bias=negt[:, h:h + 1],
                    accum_out=wsum[:, h:h + 1]), reads=[b_T2, b_negt], writes=[b_wgt, b_wsum])
            P.op("dve", lambda e: e.reciprocal(out=wsum[:], in_=wsum[:]), reads=[b_wsum], writes=[b_wsum])
            for h in range(8):
                P.op("dve", lambda e, h=h: e.tensor_scalar(
                    out=wgtf[:, h * 16:(h + 1) * 16], in0=wgt[:, h, :], scalar1=wsum[:, h:h + 1], scalar2=None,
                    op0=ALU.mult), reads=[b_wgt, b_wsum], writes=[b_wgtf])

            if dbg and n == 0:
                for nm, t_, b_, shp, dt_ in (("eidi", eidi, b_eidi, [128, 128], I32),
                                             ("wgt", wgtf, b_wgtf, [128, 128], F32),
                                             ("h2f", h2b, b_h2b, [128, D], BF16),
                                             ("S", S, b_S, [128, 16, 128], F32)):
                    od = dbg_out(nm, shp, dt_)
                    P.dma("sp", lambda e, od=od, t_=t_: e.dma_start(out=od, in_=t_[:]), "dbg_" + nm, reads=[b_])
                    out_bufs.append(b_)

        def back(n):
            xt, b_xt = xs[n % 2]
            cur = n % 2
            h2b, b_h2b = h2bs[cur]
            eidi, b_eidi = eidis[cur]
            wgtf, b_wgtf = wgtfs[cur]
            LAG = 2
            DOT_SPLIT = dot_split

            def stage_a(k):
                g_t, g_b = gb[k % NG]
                b_a2 = a2_bufs[k % 8]
                b_ga2 = ga2_bufs[k % 8]
                P.dma("pool", lambda e, k=k, g_t=g_t: e.indirect_dma_start(
                    out=g_t[:], out_offset=None, in_=uvb_d,
                    in_offset=bass.IndirectOffsetOnAxis(ap=eidi[:, k:k + 1], axis=0),
                    bounds_check=bc_reg(e), oob_is_err=False), "g%d" % (k % NG),
                    reads=[b_eidi] + b_uvb_all, writes=[g_b])
                if k % DOT_SPLIT == DOT_SPLIT - 1:
                    P.op("dve", lambda e, k=k, g_t=g_t: e.scalar_tensor_tensor(
                        out=g_t[:, 0:D], in0=g_t[:, 0:D], scalar=1.0, in1=h2b[:], op0=ALU.mult, op1=ALU.mult,
                        accum_out=a2[:, k:k + 1]), reads=[g_b, b_h2b], writes=[g_b, b_a2])
                else:
                    P.op("dve", lambda e, k=k, g_t=g_t: e.tensor_tensor(
                        out=g_t[:, 0:D], in0=g_t[:, 0:D], in1=h2b[:], op=ALU.mult),
                        reads=[g_b, b_h2b], writes=[g_b])
                    P.op("act", lambda e, k=k, g_t=g_t: e.activation(
                        out=g_t[:, 0:D], in_=g_t[:, 0:D], func=AF.Copy, accum_out=a2[:, k:k + 1]),
                        reads=[g_b], writes=[g_b, b_a2])
                P.op("act", lambda e, k=k: e.activation(
                    out=ga2[:, k:k + 1], in_=a2[:, k:k + 1], func=AF.Gelu_apprx_tanh),
                    reads=[b_a2], writes=[b_ga2])

            def stage_b(k):
                g_t, g_b = gb[k % NG]
                d_t, d_b = dg[k % 4]
                b_ga2 = ga2_bufs[k % 8]
                P.op("dve", lambda e, k=k, d_t=d_t: e.tensor_scalar(
                    out=d_t[:], in0=ident[:], scalar1=ga2[:, k:k + 1], scalar2=wgtf[:, k:k + 1],
                    op0=ALU.mult, op1=ALU.mult), reads=[b_ident, b_ga2, b_wgtf], writes=[d_b])
                for half, (ap_, ab_) in enumerate(((acc0, b_acc0), (acc1, b_acc1))):
                    P.op("pe", lambda e, k=k, half=half, ap_=ap_, d_t=d_t, g_t=g_t: e.matmul(
                        ap_[:, :], lhsT=d_t[:], rhs=g_t[:, D + half * 512:D + (half + 1) * 512],
                        start=(k == 0), stop=(k == 127)), reads=[d_b, g_b], writes=[ab_])

            for k in range(128 + LAG):
                if k < 128:
                    stage_a(k)
                if k >= LAG:
                    stage_b(k - LAG)
            for half, (ap_, ab_) in enumerate(((acc0, b_acc0), (acc1, b_acc1))):
                P.op("dve", lambda e, half=half, ap_=ap_: e.tensor_tensor(
                    out=xt[:, half * 512:(half + 1) * 512], in0=ap_[:, :],
                    in1=xt[:, half * 512:(half + 1) * 512], op=ALU.add),
                    reads=[ab_, b_xt], writes=[b_xt])
            P.dma("sp", lambda e: e.dma_start(out=y_d[n * T:(n + 1) * T, :], in_=xt[:]),
                  "y%d" % cur, reads=[b_xt])
            out_bufs.append(b_xt)

        rec_f = P.record(front, 0)
        if sched:
            P.commit_scheduled(rec_f)
        else:
            for it in rec_f:
                P.commit(it)
        for n in range(n_tiles):
            rec_b = P.record(back, n) if skip != "back" else []
            rec_f = P.record(front, n + 1) if (n + 1 < n_tiles and not (skip == "front" and n >= 1)) else []
            if sched:
                P.commit_scheduled(rec_b + rec_f)
            else:
                P.commit_interleaved(rec_b, rec_f)

        P.final_wait("sp", out_bufs + tap_bufs)
        P.emit()
    return nc, list(dbg_d.keys())


def _t5_bucket_static():
    qi = np.arange(128)[:, None]
    kj = np.arange(256)[None, :]
    dist = qi + 128 - kj
    valid = (dist >= 0) & (dist < 128)
    d0 = np.maximum(dist, 0)
    max_exact = 16
    dd = np.maximum(d0, 1).astype(np.float32)
    large = max_exact + (np.log(dd / np.float32(max_exact)) / np.float32(math.log(128 / max_exact))
                         * np.float32(32 - max_exact)).astype(np.int32)
    large = np.minimum(large, 31)
    bucket = np.where(d0 < max_exact, d0, large)
    ohs = np.zeros((32, 128, 256), np.float32)
    for b in range(32):
        ohs[b] = ((bucket == b) & valid).astype(np.float32)
    negmask = np.where(valid, 0.0, NEG).astype(np.float32)
    return ohs, negmask


def _host_layout(inp, n_tok=SEQ):
    f = lambda a: np.ascontiguousarray(np.asarray(a, dtype=np.float32))
    ohs, negmask = _t5_bucket_static()
    bones = np.zeros((128, 128), np.float32)
    bones[:64, :64] = 1.0
    bones[64:, 64:] = 1.0
    keys = f(inp["peer_keys"])[0]
    keysT = np.ascontiguousarray(keys.transpose(3, 1, 0, 2)).reshape(128, 16 * 128)
    uv = np.ascontiguousarray(np.concatenate([f(inp["peer_u"])[0], f(inp["peer_v"])[0]], axis=1))
    shared = {
        "w_ada": f(inp["w_ada"])[0],
        "b_ada": f(inp["b_ada"]).reshape(1, 6 * D),
        "norm1_g": f(inp["norm1_g"]).reshape(1, D),
        "norm2_g": f(inp["norm2_g"]).reshape(1, D),
        "w_in": f(inp["w_in"])[0],
        "conv_w_t": np.ascontiguousarray(f(inp["conv_w"])[0].reshape(3, 4, 128).transpose(2, 1, 0)).reshape(128, 12),
        "qg_t": np.ascontiguousarray(np.tile(f(inp["q_norm_g"])[0], 2).reshape(128, 1)),
        "kg_t": np.ascontiguousarray(np.tile(f(inp["k_norm_g"])[0], 2).reshape(128, 1)),
        "sinks": f(inp["sinks"]).reshape(1, 8),
        "rel_bias": f(inp["rel_bias"]).reshape(1, 256),
        "conv_g_t": np.ascontiguousarray(f(inp["conv_out_g"])[0].reshape(4, 128).T),
        "attn_g_t": np.ascontiguousarray(f(inp["attn_out_g"])[0].reshape(4, 128).T),
        "w_out": f(inp["w_out"])[0],
        "peer_wq": f(inp["peer_wq"])[0],
        "keysT": keysT,
        "uv": uv,
        "ident": np.eye(128, dtype=np.float32),
        "blockones": bones,
        "iota16": np.ascontiguousarray(np.tile(np.arange(16, dtype=np.float32), (128, 1))),
        "ohs": ohs,
        "negmask": negmask,
    }
    x = f(inp["x"])
    c = f(inp["c"])
    maps = []
    for b in range(x.shape[0]):
        m = dict(shared)
        m["x"] = np.ascontiguousarray(x[b, :SEQ])
        m["c_t"] = np.ascontiguousarray(c[b].reshape(8, 128).T)
        maps.append(m)
    return maps


def kernel(**inputs):
    maps = _host_layout(inputs)
    nc, _ = build(n_tiles=SEQ // T)
    res = run_bass_kernel_spmd(nc, maps, core_ids=list(range(8)))
    out = np.stack([np.asarray(r["y"], dtype=np.float32) for r in res.results], axis=0)
    return out
```

```python
from contextlib import ExitStack
import math
import numpy as np
import concourse.bass as bass
import concourse.mybir as mybir
from concourse.bass_utils import run_bass_kernel_spmd

F32 = mybir.dt.float32
BF16 = mybir.dt.bfloat16
I32 = mybir.dt.int32
U32 = mybir.dt.uint32
AF = mybir.ActivationFunctionType
ALU = mybir.AluOpType
AX = mybir.AxisListType

ENGS = ("pe", "act", "dve", "pool", "sp")
TBL_AGE = 0.1
HOP_X = 0.3
D = 1024
SEQ = 4096
T = 128
EPS = 1e-6
NEG = -30000.0


class Buf:
    __slots__ = ("name", "writer", "readers")

    def __init__(self, name):
        self.name = name
        self.writer = None
        self.readers = []


class _Dummy:
    def then_inc(self, *a, **k):
        return self


class _Spy:
    def __init__(self):
        self.calls = []

    def __getattr__(self, name):
        def f(*a, **kw):
            self.calls.append((name, a, kw))
            return _Dummy()
        return f


def _dt_bytes(dt):
    return 2 if dt == BF16 else 4


def _op_cost(eng, kind, fn):
    spy = _Spy()
    try:
        fn(spy)
    except Exception:
        return (0.3, 0.0)
    if not spy.calls:
        return (0.3, 0.0)
    name, a, kw = spy.calls[-1]
    aps = [v for v in list(a) + list(kw.values()) if hasattr(v, "shape") and hasattr(v, "dtype")]
    elems, nbytes, narrow = 1, 0, True
    for v in aps:
        shp = tuple(v.shape)
        fr = 1
        for d_ in shp[1:]:
            fr *= d_
        elems = max(elems, fr)
        nbytes = max(nbytes, fr * shp[0] * _dt_bytes(v.dtype))
        if v.dtype != BF16:
            narrow = False
    if kind == "dma":
        o = kw.get("out", a[0] if a else None)
        if o is not None and hasattr(o, "shape"):
            nbytes = _dt_bytes(o.dtype)
            for d_ in tuple(o.shape):
                nbytes *= d_
        lat = 2.0 + nbytes / 3.0e5
        if name == "indirect_dma_start":
            return (1.4, lat)
        return ((1.0 if eng == "pool" else 0.15), lat)
    if eng == "pe":
        out = kw.get("out", a[0] if a else None)
        n = 128
        if out is not None and hasattr(out, "shape"):
            n = 1
            for d_ in tuple(out.shape)[1:]:
                n *= d_
        return (0.03 + 0.00042 * n, 0.15)
    if eng == "act":
        f_ = kw.get("func")
        tbl = "E" if f_ in (AF.Exp, AF.Ln) else ("G" if f_ == AF.Gelu_apprx_tanh else None)
        return (0.2 + 0.00085 * elems, 0.0, tbl)
    if eng == "pool":
        return (0.2 + 0.0021 * elems, 0.0)
    if name in ("max", "max_index", "match_replace"):
        return (0.15 + 0.0012 * elems, 0.0)
    return (0.08 + (0.0006 if narrow else 0.00105) * elems, 0.0)


class Prog:
    def __init__(self, nc, es):
        self.nc = nc
        self.es = es
        self.q = {e: [] for e in ENGS}
        self.sems = {}
        self.count = {}
        self.waited = {e: {} for e in ENGS}
        for e in ENGS:
            self._sem("eng_" + e)
        self.rec = None
        self.eng_time = {}
        self.act_tbl = None

    def record(self, f, *args):
        self.rec = []
        f(*args)
        r, self.rec = self.rec, None
        return r

    def commit(self, item):
        kind, eng, fn, reads, writes, slot = item[:6]
        if kind == "op":
            self.op(eng, fn, reads, writes)
        else:
            self.dma(eng, fn, slot, reads, writes)

    def commit_scheduled(self, ops):
        n = len(ops)
        deps = [set() for _ in range(n)]
        last_w, readers, last_slot = {}, {}, {}
        for i, it in enumerate(ops):
            kind, eng, fn, reads, writes, slot = it[:6]
            for b in reads:
                if id(b) in last_w:
                    deps[i].add(last_w[id(b)])
            for b in writes:
                if id(b) in last_w:
                    deps[i].add(last_w[id(b)])
                for r in readers.get(id(b), ()):
                    deps[i].add(r)
            if kind == "dma":
                if slot in last_slot:
                    deps[i].add(last_slot[slot])
                last_slot[slot] = i
            for b in reads:
                readers.setdefault(id(b), []).append(i)
            for b in writes:
                last_w[id(b)] = i
                readers[id(b)] = []
            deps[i].discard(i)
        users = [[] for _ in range(n)]
        indeg = [0] * n
        for i in range(n):
            indeg[i] = len(deps[i])
            for d_ in deps[i]:
                users[d_].append(i)
        t0 = max(self.eng_time.values()) if self.eng_time else 0.0
        eng_free = {e: max(self.eng_time.get(e, 0.0), t0 - 3.0) for e in ENGS}
        finish = [0.0] * n
        ready_t = [t0 - 3.0] * n
        ready = [i for i in range(n) if indeg[i] == 0]
        done = 0
        while ready:
            best, best_key, best_st = None, None, None
            for i in ready:
                st = max(eng_free[ops[i][1]], ready_t[i])
                pen = 0.0
                c_ = ops[i][6]
                if len(c_) > 2 and c_[2] is not None and c_[2] != self.act_tbl:
                    pen = max(0.0, 1.3 - TBL_AGE * max(0.0, eng_free["act"] - ready_t[i]))
                key = (st + pen, i)
                if best_key is None or key < best_key:
                    best, best_key, best_st = i, key, st
            i = best
            ready.remove(i)
            eng = ops[i][1]
            occ, lat = ops[i][6][0], ops[i][6][1]
            st = best_st
            c_ = ops[i][6]
            if len(c_) > 2 and c_[2] is not None:
                if c_[2] != self.act_tbl:
                    occ += 1.3
                self.act_tbl = c_[2]
            eng_free[eng] = st + occ
            finish[i] = st + occ + lat
            self.commit(ops[i])
            done += 1
            for u in users[i]:
                hop = 0.05 if (ops[u][1] == eng and ops[i][0] == "op") else HOP_X
                ready_t[u] = max(ready_t[u], finish[i] + hop)
                indeg[u] -= 1
                if indeg[u] == 0:
                    ready.append(u)
        assert done == n
        self.eng_time = eng_free

    def commit_interleaved(self, a, b):
        ia = ib = 0
        na, nb = len(a), len(b)
        while ia < na or ib < nb:
            if ib >= nb or (ia < na and ia * nb <= ib * na):
                self.commit(a[ia]); ia += 1
            else:
                self.commit(b[ib]); ib += 1

    def _sem(self, key):
        if key not in self.sems:
            self.sems[key] = self.es.enter_context(self.nc.semaphore("s_" + key))
            self.count[key] = 0
        return self.sems[key]

    def sbuf(self, name, shape, dt):
        return self.es.enter_context(self.nc.sbuf_tensor("sb_" + name, list(shape), dt))

    def psum(self, name, shape, dt):
        return self.es.enter_context(self.nc.psum_tensor("ps_" + name, list(shape), dt))

    def _deps(self, eng, reads, writes):
        need = {}
        for b in reads:
            if b.writer is not None:
                k, v = b.writer
                need[k] = max(need.get(k, 0), v)
        for b in writes:
            if b.writer is not None:
                k, v = b.writer
                need[k] = max(need.get(k, 0), v)
            for (k, v) in b.readers:
                need[k] = max(need.get(k, 0), v)
        waits = []
        wd = self.waited[eng]
        for k, v in need.items():
            if eng == "pe" and k == "eng_pe":
                continue
            if wd.get(k, 0) >= v:
                continue
            wd[k] = v
            waits.append((self.sems[k], v))
        return waits

    def _commit(self, ticket, reads, writes):
        for b in reads:
            b.readers.append(ticket)
        for b in writes:
            b.writer = ticket
            b.readers = []

    def op(self, eng, fn, reads=(), writes=()):
        if self.rec is not None:
            self.rec.append(("op", eng, fn, tuple(reads), tuple(writes), None, _op_cost(eng, "op", fn)))
            return None
        waits = self._deps(eng, reads, writes)
        key = "eng_" + eng
        self.count[key] += 1
        ticket = (key, self.count[key])
        self.q[eng].append((waits, fn, (self.sems[key], 1)))
        self._commit(ticket, reads, writes)
        return ticket

    def dma(self, eng, fn, slot, reads=(), writes=()):
        if self.rec is not None:
            self.rec.append(("dma", eng, fn, tuple(reads), tuple(writes), slot, _op_cost(eng, "dma", fn)))
            return None
        waits = self._deps(eng, reads, writes)
        key = "dma_" + slot
        self._sem(key)
        prev = self.count[key]
        if prev > 0 and self.waited[eng].get(key, 0) < prev:
            self.waited[eng][key] = prev
            waits.append((self.sems[key], prev))
        self.count[key] += 16
        ticket = (key, self.count[key])
        self.q[eng].append((waits, fn, (self.sems[key], 16)))
        self._commit(ticket, reads, writes)
        return ticket

    def final_wait(self, eng, bufs):
        waits = self._deps(eng, (), bufs)
        self.q[eng].append((waits, None, None))

    def emit(self):
        nc = self.nc
        q = self.q

        def replay(e, lst):
            for waits, fn, inc in lst:
                for (s, v) in waits:
                    e.wait_ge(s, v)
                if fn is None:
                    continue
                ins = fn(e)
                if inc is not None:
                    ins.then_inc(inc[0], inc[1])

        with nc.Block() as blk:
            @blk.tensor
            def _(e):
                replay(e, q["pe"])

            @blk.scalar
            def _(e):
                replay(e, q["act"])

            @blk.vector
            def _(e):
                replay(e, q["dve"])

            @blk.gpsimd
            def _(e):
                replay(e, q["pool"])

            @blk.sync
            def _(e):
                replay(e, q["sp"])


def build(n_tiles=32, dbg=False, stop_after=None, sched=True, skip=None, dot_split=8, sel_eng="dve"):
    nc = bass.Bass("TRN2", target_bir_lowering=False)

    def din(name, shape, dt=F32):
        return nc.dram_tensor(name, list(shape), dt, kind="ExternalInput").ap()

    x_d = din("x", [SEQ, D])
    c_d = din("c_t", [128, 8])
    wada_d = din("w_ada", [D, 6 * D])
    bada_d = din("b_ada", [1, 6 * D])
    n1g_d = din("norm1_g", [1, D])
    n2g_d = din("norm2_g", [1, D])
    win_d = din("w_in", [D, 2304])
    cw_d = din("conv_w_t", [128, 12])
    qg_d = din("qg_t", [128, 1])
    kg_d = din("kg_t", [128, 1])
    sinks_d = din("sinks", [1, 8])
    rb_d = din("rel_bias", [1, 256])
    cg_d = din("conv_g_t", [128, 4])
    ag_d = din("attn_g_t", [128, 4])
    wout_d = din("w_out", [D, D])
    wq_d = din("peer_wq", [D, 2048])
    keysT_d = din("keysT", [128, 16 * 128])
    uv_d = din("uv", [16384, 2048])
    ident_d = din("ident", [128, 128])
    bones_d = din("blockones", [128, 128])
    iota_d = din("iota16", [128, 16])
    ohs_d = din("ohs", [32, 128, 256])
    negm_d = din("negmask", [128, 256])
    y_d = nc.dram_tensor("y", [SEQ, D], F32, kind="ExternalOutput").ap()
    dbg_d = {}

    def dbg_out(name, shape, dt=F32):
        dbg_d[name] = nc.dram_tensor("dbg_" + name, list(shape), dt, kind="ExternalOutput").ap()
        return dbg_d[name]

    with ExitStack() as es:
        P = Prog(nc, es)
        _bufs = {}
        tap_bufs = []
        _regs = {}

        def bc_reg(e):
            if isinstance(e, _Spy):
                return 16383
            if "bc" not in _regs:
                _regs["bc"] = e.to_reg(16383)
            return _regs["bc"]

        def dtap(name, ap, buf, shape, dt=F32):
            if not dbg:
                return
            od = dbg_out(name, shape, dt)
            P.dma("sp", lambda e: e.dma_start(out=od, in_=ap), "dbg_" + name, reads=[buf])
            tap_bufs.append(buf)

        def SB(name, shape, dt=F32):
            t = P.sbuf(name, shape, dt)
            b = Buf(name)
            _bufs[name] = b
            return t, b

        def PS(name, shape, dt=F32):
            t = P.psum(name, shape, dt)
            b = Buf(name)
            return t, b

        ident, b_ident = SB("ident", [128, 128], BF16)
        bones, b_bones = SB("bones", [128, 128], BF16)
        iota16, b_iota = SB("iota16", [128, 16])
        Win, b_Win = SB("Win", [128, 8, 2304], BF16)
        Wk2, b_Wk2 = SB("Wk2", [128, 8, 256], BF16)
        Wout, b_Wout = SB("Wout", [128, 8, 1024], BF16)
        Wq, b_Wq = SB("Wq", [128, 8, 2048], BF16)
        keysT, b_keysT = SB("keysT", [128, 16, 128], BF16)
        bc, b_bc = SB("bc", [128, 4 * D])
        bias_all, b_bias = SB("bias_all", [128, 8, 256])
        cw, b_cw = SB("cw", [128, 12])
        qg, b_qg = SB("qg", [128, 1])
        kg, b_kg = SB("kg", [128, 1])
        cg, b_cg = SB("cg", [128, 4])
        ag, b_ag = SB("ag", [128, 4])
        sink_b, b_sink = SB("sink_b", [128, 8])
        rb_b, b_rb = SB("rb_b", [128, 256])
        c_t, b_ct = SB("c_t", [128, 8])
        cond, b_cond = SB("cond", [128, 8])
        ones_row, b_ones = SB("ones_row", [1, 128])

        NG = 7
        gb = [SB("gb%d" % i, [128, 2048], BF16) for i in range(NG)]
        xs = [SB("xs%d" % i, [128, D]) for i in range(3)]
        stg = xs
        pr, b_pr = SB("pr", [128, 18, 128])
        V1, b_V1 = SB("V1", [128, 16, 16])
        I1f, b_I1f = SB("I1f", [128, 16, 16])
        _v1f = V1[:].rearrange("p a b -> p (a b)")
        _i1f = I1f[:].rearrange("p a b -> p (a b)")
        brow = [(_v1f[0:1, 0:128], Buf("brow0")), (_v1f[0:1, 128:256], Buf("brow1"))]
        mrow = [(_i1f[0:1, 0:128], Buf("mrow0")), (_i1f[0:1, 128:256], Buf("mrow1"))]
        h2bs = [SB("h2b%d" % i, [128, D], BF16) for i in range(3)]
        g2_tmp, b_g2tmp = pr[:].rearrange("p c n -> p (c n)")[:, 0:D], b_pr
        g1_tmp, b_g1tmp = xs[2]
        tmpf, b_tmpf = pr[:].rearrange("p c n -> p (c n)")[:, 0:D], b_pr
        uvb_d = nc.dram_tensor("uvb", [16384, 2048], BF16, kind="Internal").ap()
        b_uvb_u = [Buf("uvb_u%d" % i) for i in range(4)]
        b_uvb_v = [Buf("uvb_v%d" % i) for i in range(6)]
        b_uvb_all = b_uvb_u + b_uvb_v

        PA, b_PA = PS("PA", [128, 512])
        PB, b_PB = PS("PB", [128, 512])
        PC, b_PC = PS("PC", [128, 512])
        PD, b_PD = PS("PD", [128, 512])
        PT, b_PT = PS("PT", [128, 1024], BF16)
        PT2, b_PT2 = PT, b_PT
        acc0, b_acc0 = PS("acc0", [128, 512])
        acc1, b_acc1 = PS("acc1", [128, 512])
        PSc, b_PSc0 = PS("PSc", [128, 2, 256])
        b_PSc = [b_PSc0, b_PSc0]
        PO, b_PO = PD, b_PD

        win_v = win_d.rearrange("(kc p) n -> p kc n", p=128)
        wout_v = wout_d.rearrange("(kc p) n -> p kc n", p=128)
        wq_v = wq_d.rearrange("(kc p) n -> p kc n", p=128)
        wada_v = wada_d.rearrange("(kc p) n -> p kc n", p=128)

        def ld(eng, out_ap, in_ap, slot, wbuf):
            P.dma(eng, lambda e: e.dma_start(out=out_ap, in_=in_ap), slot, writes=[wbuf])

        ld("pool", ident[:], ident_d, "c0", b_ident)
        ld("pool", bones[:], bones_d, "c1", b_bones)
        ld("sp", iota16[:], iota_d, "c2", b_iota)
        ld("sp", cw[:], cw_d, "c3", b_cw)
        ld("sp", qg[:], qg_d, "c4", b_qg)
        ld("sp", kg[:], kg_d, "c5", b_kg)
        ld("sp", cg[:], cg_d, "c6", b_cg)
        ld("sp", ag[:], ag_d, "c7", b_ag)
        ld("sp", c_t[:], c_d, "c8", b_ct)
        ld("sp", sink_b[:], sinks_d.partition_broadcast(128), "c9", b_sink)
        ld("sp", rb_b[:], rb_d.partition_broadcast(128), "c10", b_rb)
        for kc in range(8):
            ld("pool", Win[:, kc, :], win_v[:, kc, :], "w%d" % (kc % 4), b_Win)
        for kv in range(2):
            for r in range(2):
                ld("pool", Wk2[:, :, kv * 128 + r * 64: kv * 128 + r * 64 + 64],
                   win_v[:, :, 2048 + kv * 64: 2048 + kv * 64 + 64], "w%d" % (kv * 2 + r), b_Wk2)
        for kc in range(8):
            ld("pool", Wout[:, kc, :], wout_v[:, kc, :], "w%d" % (kc % 4), b_Wout)
        for kc in range(8):
            ld("pool", Wq[:, kc, :], wq_v[:, kc, :], "w%d" % (kc % 4), b_Wq)
        ld("pool", keysT[:].rearrange("p c n -> p (c n)"), keysT_d, "w0", b_keysT)
        CR = 256
        for cidx in range(16384 // CR):
            P.dma("pool", lambda e, cidx=cidx: e.dma_start(
                out=uvb_d[cidx * CR:(cidx + 1) * CR, 0:D], in_=uv_d[cidx * CR:(cidx + 1) * CR, 0:D]),
                "cv%d" % (cidx % 4), writes=[b_uvb_u[cidx % 4]])

        P.op("act", lambda e: e.activation(out=cond[:], in_=c_t[:], func=AF.Silu),
             reads=[b_ct], writes=[b_cond])
        P.op("dve", lambda e: e.memset(ones_row[:], 1.0), writes=[b_ones])
        CW = 128
        NCH = 6 * D // CW
        for ch in range(NCH):
            g_t, g_b = stg[ch % 2]
            wv = g_t[:].rearrange("p (k n) -> p k n", k=8)
            ld("sp", wv, wada_v[:, :, ch * CW:(ch + 1) * CW], "ada%d" % (ch % 2), g_b)
            br_t, br_b = brow[ch % 2]
            mr_t, mr_b = mrow[ch % 2]
            ld("sp", br_t[:, 0:CW], bada_d[:, ch * CW:(ch + 1) * CW], "bada%d" % (ch % 2), br_b)
            pbank, pbuf = (PA, b_PA) if ch % 2 == 0 else (PB, b_PB)
            for kc in range(8):
                P.op("pe", lambda e, kc=kc, wv=wv, pbank=pbank: e.matmul(
                    pbank[0:1, 0:CW], lhsT=cond[:, kc:kc + 1], rhs=wv[:, kc, :],
                    start=(kc == 0), stop=(kc == 7)),
                    reads=[b_cond, g_b], writes=[pbuf])
            P.op("dve", lambda e, pbank=pbank, br_t=br_t, mr_t=mr_t: e.tensor_tensor(
                out=mr_t[:, 0:CW], in0=pbank[0:1, 0:CW], in1=br_t[:, 0:CW], op=ALU.add),
                reads=[pbuf, br_b], writes=[mr_b])
            pbank2, pbuf2 = (PC, b_PC) if ch % 2 == 0 else (PD, b_PD)
            P.op("pe", lambda e, pbank2=pbank2, mr_t=mr_t: e.matmul(
                pbank2[:, 0:CW], lhsT=ones_row[0:1, :], rhs=mr_t[0:1, 0:CW],
                start=True, stop=True), reads=[b_ones, mr_b], writes=[pbuf2])
            col0 = ch * CW
            if col0 < 2 * D:
                dst = bc[:, col0:col0 + CW]
                dbuf = b_bc
            elif col0 < 3 * D:
                dst = g1_tmp[:, col0 - 2 * D:col0 - 2 * D + CW]
                dbuf = b_g1tmp
            elif col0 < 5 * D:
                dst = bc[:, col0 - D:col0 - D + CW]
                dbuf = b_bc
            else:
                dst = g2_tmp[:, col0 - 5 * D:col0 - 5 * D + CW]
                dbuf = b_g2tmp
            P.op("act", lambda e, pbank2=pbank2, dst=dst: e.copy(out=dst, in_=pbank2[:, 0:CW]),
                 reads=[pbuf2], writes=[dbuf])
        sh1_b, sc1_b = bc[:, 0:D], bc[:, D:2 * D]
        sh2_b, sc2_b = bc[:, 2 * D:3 * D], bc[:, 3 * D:4 * D]
        for kc in range(8):
            P.op("dve", lambda e, kc=kc: e.tensor_tensor(
                out=Wout[:, kc, :], in0=Wout[:, kc, :], in1=g1_tmp[:, 0:D], op=ALU.mult),
                reads=[b_Wout, b_g1tmp], writes=[b_Wout])
        for (sc_ap, ng_d, slot) in ((sc1_b, n1g_d, 0), (sc2_b, n2g_d, 1)):
            g_t, g_b = stg[slot]
            ld("sp", g_t[:, 0:D], ng_d.partition_broadcast(128), "ng%d" % slot, g_b)
            P.op("dve", lambda e, sc_ap=sc_ap, g_t=g_t: e.scalar_tensor_tensor(
                out=sc_ap, in0=sc_ap, scalar=1.0, in1=g_t[:, 0:D], op0=ALU.add, op1=ALU.mult),
                reads=[b_bc, g_b], writes=[b_bc])
        P.op("dve", lambda e: e.tensor_scalar(out=qg[:], in0=qg[:], scalar1=0.125, scalar2=None,
                                              op0=ALU.mult), reads=[b_qg], writes=[b_qg])

        P.op("dve", lambda e: e.memset(bias_all[:], 0.0), writes=[b_bias])
        for b in range(32):
            g_t, g_b = stg[b % 2]
            ld("sp", g_t[:, 0:256], ohs_d[b], "oh%d" % (b % 2), g_b)
            for h in range(8):
                P.op("dve", lambda e, b=b, h=h, g_t=g_t: e.scalar_tensor_tensor(
                    out=bias_all[:, h, :], in0=g_t[:, 0:256], scalar=rb_b[:, b * 8 + h: b * 8 + h + 1],
                    in1=bias_all[:, h, :], op0=ALU.mult, op1=ALU.add),
                    reads=[g_b, b_rb, b_bias], writes=[b_bias])
        g_t, g_b = stg[0]
        ld("sp", g_t[:, 0:256], negm_d, "oh0", g_b)
        for h in range(8):
            P.op("dve", lambda e, h=h, g_t=g_t: e.tensor_add(
                out=bias_all[:, h, :], in0=bias_all[:, h, :], in1=g_t[:, 0:256]),
                reads=[g_b, b_bias], writes=[b_bias])

        vin = [stg[0], stg[1], xs[2]]
        vout = [h2bs[0], h2bs[1]] + [(gb[i][0][:, 0:D], Buf("vo_a%d" % i)) for i in range(2)] \
            + [(gb[i][0][:, D:2 * D], Buf("vo_b%d" % i)) for i in range(2)]
        for cidx in range(128):
            s_t, s_b = vin[cidx % 3]
            o_t, o_b = vout[cidx % 6]
            ld("sp", s_t[:, :], uv_d[cidx * 128:(cidx + 1) * 128, D:2 * D], "vc%d" % (cidx % 3), s_b)
            P.op("dve", lambda e, s_t=s_t, o_t=o_t: e.tensor_tensor(out=o_t[:, :], in0=s_t[:, :], in1=g2_tmp, op=ALU.mult),
                 reads=[s_b, b_g2tmp], writes=[o_b])
            P.dma("act", lambda e, cidx=cidx, o_t=o_t: e.dma_start(
                out=uvb_d[cidx * 128:(cidx + 1) * 128, D:2 * D], in_=o_t[:, :]), "vo%d" % (cidx % 6),
                reads=[o_b], writes=[b_uvb_v[cidx % 6]])

        jh, b_jh = SB("jh", [128, 2 * D], BF16)
        junkb, b_junkb = jh[:, 0:D], b_jh
        hb, b_hb = jh[:, D:2 * D], b_jh
        hT, b_hT = SB("hT", [128, 8, 128], BF16)
        a2, b_a2 = SB("a2", [128, 128])
        ga2, b_ga2 = SB("ga2", [128, 128])
        a2_bufs = [Buf("a2_%d" % i) for i in range(8)]
        ga2_bufs = [Buf("ga2_%d" % i) for i in range(8)]
        wgtfs = [SB("wgtf%d" % i, [128, 128]) for i in range(2)]
        dg = [SB("dg%d" % i, [128, 128], BF16) for i in range(2)]
        st1, b_st1 = SB("st1", [128, 4])
        vbuf = [SB("vbuf%d" % i, [128, 128], BF16) for i in range(2)]
        kTb = [SB("kTb%d" % i, [128, 2, 128], BF16) for i in range(2)]
        _ub = SB("ub", [128, 4, 130])
        ub = [_ub, _ub]
        ucar, b_ucar = SB("ucar", [128, 4, 2])
        yc, b_yc = SB("yc", [128, 4, 128])
        sqb, b_sqb = SB("sqb", [128, 6, 128], BF16)
        rst, b_rst = SB("rst", [128, 6, 128])
        mixT, b_mixT = hT, b_hT
        qT, b_qT = SB("qT", [128, 4, 128], BF16)
        s_sb = [SB("s_sb%d" % i, [128, 256]) for i in range(2)]
        p_sb = [SB("p_sb%d" % i, [128, 256], BF16) for i in range(2)]
        pT_sb = [SB("pT_sb%d" % i, [128, 2, 128], BF16) for i in range(2)]
        hst = [SB("hst%d" % i, [128, 8]) for i in range(2)]
        rden, b_rden = SB("rden", [128, 8])
        on, b_on = yc[:].rearrange("p c (a b) -> p (c a) b", b=64), b_yc
        onb, b_onb = SB("onb", [128, 512], BF16)
        ssa, b_ssa = SB("ssa", [128, 8])
        qTp, b_qTp = jh[:].rearrange("p (c n) -> p c n", c=16), b_jh
        S, b_S = pr[:, 0:16, :], b_pr
        I1, b_I1 = SB("I1", [128, 16, 16], U32)
        rstf = rst[:].rearrange("p c n -> p (c n)")
        ycf = yc[:].rearrange("p c n -> p (c n)")
        onf = on[:].rearrange("p c n -> p (c n)")
        _cd = SB("cand", [128, 16, 16])
        cand = [_cd, _cd]
        _c2 = SB("cand2", [128, 256])
        cand2 = [_c2, _c2]
        S2 = [(_c2[0][:, 0:128], _c2[1])] * 2
        T2, b_T2 = SB("T2", [128, 8, 16])
        pos, b_pos = SB("pos", [128, 8, 16], U32)
        b_pos_h = [Buf("pos%d" % i) for i in range(8)]
        b_k2u_h = [Buf("k2u%d" % i) for i in range(8)]
        b_k1f_h = [Buf("k1f%d" % i) for i in range(8)]
        b_k2f_h = [Buf("k2f%d" % i) for i in range(8)]
        b_ia_h = [Buf("ia%d" % i) for i in range(8)]
        b_ib_h = [Buf("ib%d" % i) for i in range(8)]
        SEL_ENG = sel_eng
        k2u, b_k2u = SB("k2u", [128, 8, 16], U32)
        k1f, b_k1f = SB("k1f", [128, 8, 16])
        k2f, b_k2f = SB("k2f", [128, 8, 16])
        eq = [(ycf[:, 256:512].rearrange("p (a b) -> p a b", a=16), b_yc),
              (onf[:, 0:256].rearrange("p (a b) -> p a b", a=16), b_on)]
        ia_s, b_ia_s = SB("ia_s", [128, 8, 16])
        ib_s, b_ib_s = SB("ib_s", [128, 8, 16])
        eidf, b_eidf = s_sb[0][0][:, 0:128], s_sb[0][1]
        eidis = [SB("eidi%d" % i, [128, 128], I32) for i in range(2)]
        wgt, b_wgt = s_sb[1][0][:, 128:256].rearrange("p (h k) -> p h k", h=8), s_sb[1][1]
        wsum, b_wsum = SB("wsum", [128, 8])
        negt, b_negt = SB("negt", [128, 8])
        a_sb, b_a = s_sb[0][0][:, 128:256], s_sb[0][1]
        ga_sb, b_ga = s_sb[1][0][:, 0:128], s_sb[1][1]
        acc, b_acc = tmpf, b_tmpf

        def rms_stats(src_ap, src_buf, n_free, out_col_ap, out_buf):
            P.op("act", lambda e: e.activation(out=junkb[:, 0:n_free], in_=src_ap, func=AF.Square,
                                               accum_out=out_col_ap),
                 reads=[src_buf], writes=[b_junkb, out_buf])
            rsqrt_inplace(out_col_ap, out_buf, 1.0 / n_free)

        def rsqrt_inplace(ap, buf, scale, src_ap=None, src_buf=None):
            s_ap = ap if src_ap is None else src_ap
            rd = [buf] if src_buf is None else [src_buf]
            P.op("dve", lambda e: e.tensor_scalar(out=ap, in0=s_ap, scalar1=scale, scalar2=EPS,
                                                  op0=ALU.mult, op1=ALU.add), reads=rd, writes=[buf])
            P.op("act", lambda e: e.activation(out=ap, in_=ap, func=AF.Ln), reads=[buf], writes=[buf])
            P.op("act", lambda e: e.activation(out=ap, in_=ap, func=AF.Exp, scale=-0.5), reads=[buf], writes=[buf])

        def transpose8(src, src_buf, dst, dst_buf, nchunk=8, ptile=None, pbuf=None):
            ptile = PT if ptile is None else ptile
            pbuf = b_PT if pbuf is None else pbuf
            for kc in range(nchunk):
                P.op("pe", lambda e, kc=kc: e.transpose(
                    out=ptile[:, kc * 128:(kc + 1) * 128], in_=src[:, kc * 128:(kc + 1) * 128],
                    identity=ident[:]), reads=[src_buf, b_ident], writes=[pbuf])
            if dst is not None:
                P.op("act", lambda e: e.copy(out=dst[:].rearrange("p c n -> p (c n)"),
                                             in_=ptile[:, 0:nchunk * 128]),
                     reads=[pbuf], writes=[dst_buf])

        out_bufs = []

        def head(n):
            xt, b_xt = xs[n % 3]
            x1, b_x1 = xt, b_xt
            cur, prv = n % 2, (n - 1) % 2
            h2b, b_h2b = h2bs[n % 3]
            ld("sp", xt[:], x_d[n * T:(n + 1) * T, :], "x%d" % (n % 3), b_xt)

            rms_stats(xt[:], b_xt, D, st1[:, 0:1], b_st1)
            P.op("dve", lambda e, xt=xt: e.scalar_tensor_tensor(
                out=tmpf[:], in0=xt[:], scalar=st1[:, 0:1], in1=sc1_b, op0=ALU.mult, op1=ALU.mult),
                reads=[b_xt, b_st1, b_bc], writes=[b_tmpf])
            P.op("dve", lambda e: e.tensor_add(out=hb[:], in0=tmpf[:], in1=sh1_b),
                 reads=[b_tmpf, b_bc], writes=[b_hb])
            if n == 0:
                dtap("hb", hb[:], b_hb, [128, D], BF16)
                dtap("bc", bc[:], b_bc, [128, 4 * D])
            transpose8(hb, b_hb, hT, b_hT)
            if n == 0:
                dtap("hT", hT[:], b_hT, [128, 8, 128], BF16)

            def wcols(j):
                if j < 16:
                    return Win, b_Win, j * 128
                return Wk2, b_Wk2, (j - 16) * 128
            for g in range(5):
                pbank, pbuf = (PA, b_PA) if g % 2 == 0 else (PB, b_PB)
                chunks = list(range(g * 4, min(g * 4 + 4, 18)))
                for i, j in enumerate(chunks):
                    wt, wb, c0 = wcols(j)
                    for kc in range(8):
                        P.op("pe", lambda e, i=i, kc=kc, wt=wt, c0=c0, pbank=pbank: e.matmul(
                            pbank[:, i * 128:(i + 1) * 128], lhsT=wt[:, kc, c0:c0 + 128], rhs=hT[:, kc, :],
                            start=(kc == 0), stop=(kc == 7)), reads=[wb, b_hT], writes=[pbuf])
                nn = len(chunks)
                P.op("act", lambda e, g=g, nn=nn, pbank=pbank: e.copy(
                    out=pr[:, g * 4:g * 4 + nn, :].rearrange("p c n -> p (c n)"), in_=pbank[:, 0:nn * 128]),
                    reads=[pbuf], writes=[b_pr])
            vt, b_vt = vbuf[cur]
            for kc in range(8):
                P.op("pe", lambda e, kc=kc: e.matmul(
                    PC[:, 0:128], lhsT=hT[:, kc, :], rhs=Win[:, kc, 2176:2304],
                    start=(kc == 0), stop=(kc == 7)), reads=[b_Win, b_hT], writes=[b_PC])
            P.op("act", lambda e, vt=vt: e.copy(out=vt[:], in_=PC[:, 0:128]), reads=[b_PC], writes=[b_vt])

            if n == 0:
                dtap("pr", pr[:], b_pr, [128, 18, 128])
                dtap("vt", vt[:], b_vt, [128, 128], BF16)
            ut, b_ut = ub[cur]
            upt, b_upt = ub[prv]
            if n == 0:
                P.op("dve", lambda e, ut=ut: e.memset(ut[:, :, 0:2], 0.0), writes=[b_ut])
            else:
                P.op("dve", lambda e, ut=ut: e.tensor_copy(out=ut[:, :, 0:2], in_=ucar[:]),
                     reads=[b_ucar], writes=[b_ut])
            P.op("dve", lambda e, ut=ut: e.tensor_tensor(
                out=ut[:, :, 2:130], in0=pr[:, 4:8, :], in1=pr[:, 8:12, :], op=ALU.mult),
                reads=[b_pr], writes=[b_ut])
            for j in range(4):
                P.op("dve", lambda e, j=j, ut=ut: e.tensor_scalar(
                    out=yc[:, j, :], in0=ut[:, j, 2:130], scalar1=cw[:, j * 3 + 2:j * 3 + 3], scalar2=None,
                    op0=ALU.mult), reads=[b_ut, b_cw], writes=[b_yc])
                for tap in (1, 0):
                    P.op("dve", lambda e, j=j, tap=tap, ut=ut: e.scalar_tensor_tensor(
                        out=yc[:, j, :], in0=ut[:, j, tap:tap + 128], scalar=cw[:, j * 3 + tap:j * 3 + tap + 1],
                        in1=yc[:, j, :], op0=ALU.mult, op1=ALU.add), reads=[b_ut, b_cw, b_yc], writes=[b_yc])
            P.op("dve", lambda e: e.tensor_tensor(out=yc[:], in0=yc[:], in1=pr[:, 0:4, :], op=ALU.mult),
                 reads=[b_yc, b_pr], writes=[b_yc])
            P.op("dve", lambda e, ut=ut: e.tensor_copy(out=ucar[:], in_=ut[:, :, 128:130]),
                 reads=[b_ut], writes=[b_ucar])
            P.op("act", lambda e: e.activation(out=sqb[:, 0:4, :], in_=yc[:], func=AF.Square),
                 reads=[b_yc], writes=[b_sqb])
            for j in range(4):
                P.op("pe", lambda e, j=j: e.matmul(PD[:, j * 128:(j + 1) * 128], lhsT=bones[:], rhs=sqb[:, j, :],
                                                   start=True, stop=True), reads=[b_bones, b_sqb], writes=[b_PD])
            rsqrt_inplace(rst[:, 0:4, :].rearrange("p c n -> p (c n)"), b_rst, 1.0 / 64,
                          src_ap=PD[:, 0:512], src_buf=b_PD)
            for j in range(4):
                P.op("dve", lambda e, j=j: e.scalar_tensor_tensor(
                    out=mixT[:, j, :], in0=yc[:, j, :], scalar=cg[:, j:j + 1], in1=rst[:, j, :],
                    op0=ALU.mult, op1=ALU.mult), reads=[b_yc, b_cg, b_rst], writes=[b_mixT])

            P.op("act", lambda e: e.activation(out=sqb[:], in_=pr[:, 12:18, :], func=AF.Square),
                 reads=[b_pr], writes=[b_sqb])
            for j in range(4):
                P.op("pe", lambda e, j=j: e.matmul(PD[:, j * 128:(j + 1) * 128], lhsT=bones[:], rhs=sqb[:, j, :],
                                                   start=True, stop=True), reads=[b_bones, b_sqb], writes=[b_PD])
            rsqrt_inplace(rst[:, 0:4, :].rearrange("p c n -> p (c n)"), b_rst, 1.0 / 64,
                          src_ap=PD[:, 0:512], src_buf=b_PD)
            for j in range(2):
                P.op("pe", lambda e, j=j: e.matmul(PC[:, j * 128:(j + 1) * 128], lhsT=bones[:], rhs=sqb[:, 4 + j, :],
                                                   start=True, stop=True), reads=[b_bones, b_sqb], writes=[b_PC])
            rsqrt_inplace(rst[:, 4:6, :].rearrange("p c n -> p (c n)"), b_rst, 1.0 / 64,
                          src_ap=PC[:, 0:256], src_buf=b_PC)
            for j in range(4):
                P.op("dve", lambda e, j=j: e.scalar_tensor_tensor(
                    out=qT[:, j, :], in0=pr[:, 12 + j, :], scalar=qg[:, 0:1], in1=rst[:, j, :],
                    op0=ALU.mult, op1=ALU.mult), reads=[b_pr, b_qg, b_rst], writes=[b_qT])
            kt, b_kt = kTb[cur]
            kpt, b_kpt = kTb[prv]
            for j in range(2):
                P.op("dve", lambda e, j=j, kt=kt: e.scalar_tensor_tensor(
                    out=kt[:, j, :], in0=pr[:, 16 + j, :], scalar=kg[:, 0:1], in1=rst[:, 4 + j, :],
                    op0=ALU.mult, op1=ALU.mult), reads=[b_pr, b_kg, b_rst], writes=[b_kt])

            if n == 0:
                dtap("yc", yc[:], b_yc, [128, 4, 128])
                dtap("mixT_conv", mixT[:, 0:4, :], b_mixT, [128, 4, 128], BF16)
                dtap("qT", qT[:], b_qT, [128, 4, 128], BF16)
                dtap("kT", kt[:], b_kt, [128, 2, 128], BF16)
            vpt, b_vpt = vbuf[prv]
            for h in range(8):
                kv, r, qc = h // 4, h % 2, h // 2
                sl = h % 2
                pl, ph = 64 * r, 64 * r + 64
                st_, b_s = s_sb[sl]
                pt_, b_p = p_sb[sl]
                pTt, b_pTt = pT_sb[sl]
                hs, b_hs = hst[sl]
                c_lo = 0 if n > 0 else 128
                if n > 0:
                    P.op("pe", lambda e, sl=sl, qc=qc, kv=kv, pl=pl, ph=ph, kpt=kpt: e.matmul(
                        PSc[:, sl, 0:128], lhsT=qT[pl:ph, qc, :], rhs=kpt[pl:ph, kv, :], start=True, stop=True),
                        reads=[b_qT, b_kpt], writes=[b_PSc[sl]])
                P.op("pe", lambda e, sl=sl, qc=qc, kv=kv, pl=pl, ph=ph, kt=kt: e.matmul(
                    PSc[:, sl, 128:256], lhsT=qT[pl:ph, qc, :], rhs=kt[pl:ph, kv, :], start=True, stop=True),
                    reads=[b_qT, b_kt], writes=[b_PSc[sl]])
                P.op("dve", lambda e, sl=sl, h=h, st_=st_, c_lo=c_lo: e.tensor_tensor(
                    out=st_[:, c_lo:256], in0=PSc[:, sl, c_lo:256], in1=bias_all[:, h, c_lo:256], op=ALU.add),
                    reads=[b_PSc[sl], b_bias], writes=[b_s])
                P.op("dve", lambda e, st_=st_, hs=hs, c_lo=c_lo: e.reduce_max(
                    out=hs[:, 0:1], in_=st_[:, c_lo:256], axis=AX.X), reads=[b_s], writes=[b_hs])
                P.op("dve", lambda e, hs=hs, h=h: e.tensor_scalar(
                    out=hs[:, 1:2], in0=hs[:, 0:1], scalar1=sink_b[:, h:h + 1], scalar2=-1.0,
                    op0=ALU.max, op1=ALU.mult), reads=[b_hs, b_sink], writes=[b_hs])
                P.op("act", lambda e, st_=st_, pt_=pt_, hs=hs, c_lo=c_lo: e.activation(
                    out=pt_[:, c_lo:256], in_=st_[:, c_lo:256], func=AF.Exp, bias=hs[:, 1:2],
                    accum_out=hs[:, 2:3]), reads=[b_s, b_hs], writes=[b_p, b_hs])
                P.op("act", lambda e, hs=hs, h=h: e.activation(
                    out=hs[:, 3:4], in_=sink_b[:, h:h + 1], func=AF.Exp, bias=hs[:, 1:2]),
                    reads=[b_sink, b_hs], writes=[b_hs])
                P.op("dve", lambda e, hs=hs: e.tensor_add(out=hs[:, 4:5], in0=hs[:, 2:3], in1=hs[:, 3:4]),
                     reads=[b_hs], writes=[b_hs])
                P.op("dve", lambda e, hs=hs, h=h: e.reciprocal(out=rden[:, h:h + 1], in_=hs[:, 4:5]),
                     reads=[b_hs], writes=[b_rden])
                halves = (0, 1) if n > 0 else (1,)
                for hf in halves:
                    P.op("pe", lambda e, hf=hf, sl=sl, pt_=pt_: e.transpose(
                        out=PT2[:, (sl * 2 + hf) * 128:(sl * 2 + hf + 1) * 128], in_=pt_[:, hf * 128:(hf + 1) * 128],
                        identity=ident[:]), reads=[b_p, b_ident], writes=[b_PT2])
                lo = 0 if n > 0 else 1
                P.op("act", lambda e, sl=sl, pTt=pTt, lo=lo: e.copy(
                    out=pTt[:, lo:2, :].rearrange("p c n -> p (c n)"),
                    in_=PT2[:, (sl * 2 + lo) * 128:(sl * 2 + 2) * 128]), reads=[b_PT2], writes=[b_pTt])
                if n > 0:
                    P.op("pe", lambda e, h=h, kv=kv, pTt=pTt, vpt=vpt: e.matmul(
                        PO[:, h * 64:(h + 1) * 64], lhsT=pTt[:, 0, :], rhs=vpt[:, kv * 64:(kv + 1) * 64],
                        start=True, stop=False), reads=[b_pTt, b_vpt], writes=[b_PO])
                P.op("pe", lambda e, h=h, kv=kv, pTt=pTt, vt=vt, first=(n == 0): e.matmul(
                    PO[:, h * 64:(h + 1) * 64], lhsT=pTt[:, 1, :], rhs=vt[:, kv * 64:(kv + 1) * 64],
                    start=first, stop=True), reads=[b_pTt, b_vt], writes=[b_PO])
            for h in range(8):
                P.op("dve", lambda e, h=h: e.tensor_scalar(
                    out=on[:, h, :], in0=PO[:, h * 64:(h + 1) * 64], scalar1=rden[:, h:h + 1], scalar2=None,
                    op0=ALU.mult), reads=[b_PO, b_rden], writes=[b_on])
            for h in range(8):
                P.op("act", lambda e, h=h: e.activation(
                    out=junkb[:, 0:64], in_=on[:, h, :], func=AF.Square, accum_out=ssa[:, h:h + 1]),
                    reads=[b_on], writes=[b_junkb, b_ssa])
            rsqrt_inplace(ssa[:], b_ssa, 1.0 / 64)
            for h in range(8):
                P.op("dve", lambda e, h=h: e.tensor_scalar(
                    out=onb[:, h * 64:(h + 1) * 64], in0=on[:, h, :], scalar1=ssa[:, h:h + 1], scalar2=None,
                    op0=ALU.mult), reads=[b_on, b_ssa], writes=[b_onb])
            transpose8(onb, b_onb, None, None, nchunk=4)
            for j in range(4):
                P.op("dve", lambda e, j=j: e.tensor_scalar(
                    out=mixT[:, 4 + j, :], in0=PT[:, j * 128:(j + 1) * 128], scalar1=ag[:, j:j + 1], scalar2=None,
                    op0=ALU.mult), reads=[b_PT, b_ag], writes=[b_mixT])

            if n == 0:
                dtap("on", on[:], b_on, [128, 8, 64])
                dtap("rden", rden[:], b_rden, [128, 8])
                dtap("mixT", mixT[:], b_mixT, [128, 8, 128], BF16)
            for half in range(2):
                pbank, pbuf = (PA, b_PA) if half == 0 else (PB, b_PB)
                for c in range(8):
                    P.op("pe", lambda e, c=c, half=half, pbank=pbank: e.matmul(
                        pbank[:, :], lhsT=mixT[:, c, :], rhs=Wout[:, c, half * 512:(half + 1) * 512],
                        start=(c == 0), stop=(c == 7)), reads=[b_mixT, b_Wout], writes=[pbuf])
                P.op("dve", lambda e, half=half, pbank=pbank, xt=xt: e.tensor_tensor(
                    out=x1[:, half * 512:(half + 1) * 512], in0=pbank[:, :],
                    in1=xt[:, half * 512:(half + 1) * 512], op=ALU.add),
                    reads=[pbuf, b_xt], writes=[b_x1])

            rms_stats(x1[:], b_x1, D, st1[:, 1:2], b_st1)
            P.op("dve", lambda e: e.scalar_tensor_tensor(
                out=tmpf[:], in0=x1[:], scalar=st1[:, 1:2], in1=sc2_b, op0=ALU.mult, op1=ALU.mult),
                reads=[b_x1, b_st1, b_bc], writes=[b_tmpf])
            P.op("dve", lambda e: e.tensor_add(out=h2b[:], in0=tmpf[:], in1=sh2_b),
                 reads=[b_tmpf, b_bc], writes=[b_h2b])
            transpose8(h2b, b_h2b, hT, b_hT)

            for g in range(4):
                pbank, pbuf = (PA, b_PA) if g % 2 == 0 else (PB, b_PB)
                for i in range(4):
                    c = g * 4 + i
                    for kc in range(8):
                        P.op("pe", lambda e, i=i, c=c, kc=kc, pbank=pbank: e.matmul(
                            pbank[:, i * 128:(i + 1) * 128], lhsT=Wq[:, kc, c * 128:(c + 1) * 128], rhs=hT[:, kc, :],
                            start=(kc == 0), stop=(kc == 7)), reads=[b_Wq, b_hT], writes=[pbuf])
                P.op("act", lambda e, g=g, pbank=pbank: e.copy(
                    out=qTp[:, g * 4:(g + 1) * 4, :].rearrange("p c n -> p (c n)"), in_=pbank[:, :]),
                    reads=[pbuf], writes=[b_qTp])
            for g in range(4):
                pbank, pbuf = (PC, b_PC) if g % 2 == 0 else (PD, b_PD)
                for i in range(4):
                    c = g * 4 + i
                    P.op("pe", lambda e, i=i, c=c, pbank=pbank: e.matmul(
                        pbank[:, i * 128:(i + 1) * 128], lhsT=qTp[:, c, :], rhs=keysT[:, c, :],
                        start=True, stop=True), reads=[b_qTp, b_keysT], writes=[pbuf])
                P.op("act", lambda e, g=g, pbank=pbank: e.copy(
                    out=S[:, g * 4:(g + 1) * 4, :].rearrange("p c n -> p (c n)"), in_=pbank[:, :]),
                    reads=[pbuf], writes=[b_S])

        def tail(n):
            cur = n % 2
            h2b, b_h2b = h2bs[n % 3]
            eidi, b_eidi = eidis[cur]
            wgtf, b_wgtf = wgtfs[cur]
            for c in range(16):
                s2t, b_s2 = S2[c % 2]
                P.op("dve", lambda e, c=c: e.max(out=V1[:, c, 0:8], in_=S[:, c, :]), reads=[b_S], writes=[b_V1])
                P.op("dve", lambda e, c=c: e.max_index(out=I1[:, c, 0:8], in_max=V1[:, c, 0:8], in_values=S[:, c, :]),
                     reads=[b_S, b_V1], writes=[b_I1])
                P.op("dve", lambda e, c=c, s2t=s2t: e.match_replace(
                    out=s2t[:], in_to_replace=V1[:, c, 0:8], in_values=S[:, c, :], imm_value=-1e30),
                    reads=[b_S, b_V1], writes=[b_s2])
                P.op("dve", lambda e, c=c, s2t=s2t: e.max(out=V1[:, c, 8:16], in_=s2t[:]), reads=[b_s2], writes=[b_V1])
                P.op("dve", lambda e, c=c, s2t=s2t: e.max_index(
                    out=I1[:, c, 8:16], in_max=V1[:, c, 8:16], in_values=s2t[:]),
                    reads=[b_s2, b_V1], writes=[b_I1])
            P.op("dve", lambda e: e.tensor_copy(out=I1f[:], in_=I1[:]), reads=[b_I1], writes=[b_I1f])

            for h in range(8):
                ct, b_c = cand[h % 2]
                c2t, b_c2 = cand2[h % 2]
                P.op("dve", lambda e, h=h, ct=ct: e.tensor_tensor(
                    out=ct[:], in0=V1[:, 2 * h, :].unsqueeze(2).to_broadcast([128, 16, 16]),
                    in1=V1[:, 2 * h + 1, :].unsqueeze(1).to_broadcast([128, 16, 16]), op=ALU.add),
                    reads=[b_V1], writes=[b_c])
                cf = ct[:].rearrange("p a b -> p (a b)")
                P.op("dve", lambda e, h=h, cf=cf: e.max(out=T2[:, h, 0:8], in_=cf), reads=[b_c], writes=[b_T2])
                P.op("dve", lambda e, h=h, cf=cf: e.max_index(out=pos[:, h, 0:8], in_max=T2[:, h, 0:8], in_values=cf),
                     reads=[b_c, b_T2], writes=[b_pos_h[h]])
                P.op("dve", lambda e, h=h, cf=cf, c2t=c2t: e.match_replace(
                    out=c2t[:], in_to_replace=T2[:, h, 0:8], in_values=cf, imm_value=-1e30),
                    reads=[b_c, b_T2], writes=[b_c2])
                P.op("dve", lambda e, h=h, c2t=c2t: e.max(out=T2[:, h, 8:16], in_=c2t[:]), reads=[b_c2], writes=[b_T2])
                P.op("dve", lambda e, h=h, c2t=c2t: e.max_index(
                    out=pos[:, h, 8:16], in_max=T2[:, h, 8:16], in_values=c2t[:]),
                    reads=[b_c2, b_T2], writes=[b_pos_h[h]])
            cnt = 0
            for h in range(8):
                P.op("dve", lambda e, h=h: e.tensor_single_scalar(
                    out=k2u[:, h, :], in_=pos[:, h, :], scalar=15, op=ALU.bitwise_and),
                    reads=[b_pos_h[h]], writes=[b_k2u_h[h]])
                P.op("dve", lambda e, h=h: e.tensor_single_scalar(
                    out=pos[:, h, :], in_=pos[:, h, :], scalar=4, op=ALU.logical_shift_right),
                    reads=[b_pos_h[h]], writes=[b_pos_h[h]])
                P.op("dve", lambda e, h=h: e.tensor_copy(out=k1f[:, h, :], in_=pos[:, h, :]),
                     reads=[b_pos_h[h]], writes=[b_k1f_h[h]])
                P.op("dve", lambda e, h=h: e.tensor_copy(out=k2f[:, h, :], in_=k2u[:, h, :]),
                     reads=[b_k2u_h[h]], writes=[b_k2f_h[h]])
                for side, (kf, b_kf, dst, b_dst) in enumerate(((k1f, b_k1f_h[h], ia_s, b_ia_h[h]),
                                                               (k2f, b_k2f_h[h], ib_s, b_ib_h[h]))):
                    et, b_e = eq[cnt % 2]
                    cnt += 1
                    P.op(SEL_ENG, lambda e, h=h, kf=kf, et=et: e.tensor_tensor(
                        out=et[:], in0=iota16[:, :].unsqueeze(1).to_broadcast([128, 16, 16]),
                        in1=kf[:, h, :].unsqueeze(2).to_broadcast([128, 16, 16]), op=ALU.is_equal),
                        reads=[b_iota, b_kf], writes=[b_e])
                    P.op(SEL_ENG, lambda e, h=h, side=side, et=et: e.tensor_tensor(
                        out=et[:], in0=et[:],
                        in1=I1f[:, 2 * h + side, :].unsqueeze(1).to_broadcast([128, 16, 16]), op=ALU.mult),
                        reads=[b_e, b_I1f], writes=[b_e])
                    P.op("dve", lambda e, h=h, dst=dst, et=et: e.reduce_sum(
                        out=dst[:, h, :], in_=et[:], axis=AX.X), reads=[b_e], writes=[b_dst])
            P.op("dve", lambda e: e.scalar_tensor_tensor(
                out=eidf[:], in0=ia_s[:].rearrange("p h k -> p (h k)"), scalar=128.0,
                in1=ib_s[:].rearrange("p h k -> p (h k)"), op0=ALU.mult, op1=ALU.add),
                reads=b_ia_h + b_ib_h, writes=[b_eidf])
            P.op("dve", lambda e: e.tensor_copy(out=eidi[:], in_=eidf[:]), reads=[b_eidf], writes=[b_eidi])
            P.op("dve", lambda e: e.tensor_scalar(out=negt[:], in0=T2[:, :, 0], scalar1=-1.0, scalar2=None,
                                                  op0=ALU.mult), reads=[b_T2], writes=[b_negt])
            for h in range(8):
                P.op("act", lambda e, h=h: e.activation(
                    out=wgt[:, h, :], in_=T2[:, h, :], func=AF.Exp, bias=negt[:, h:h + 1],
                    accum_out=wsum[:, h:h + 1]), reads=[b_T2, b_negt], writes=[b_wgt, b_wsum])
            P.op("dve", lambda e: e.reciprocal(out=wsum[:], in_=wsum[:]), reads=[b_wsum], writes=[b_wsum])
            for h in range(8):
                P.op("dve", lambda e, h=h: e.tensor_scalar(
                    out=wgtf[:, h * 16:(h + 1) * 16], in0=wgt[:, h, :], scalar1=wsum[:, h:h + 1], scalar2=None,
                    op0=ALU.mult), reads=[b_wgt, b_wsum], writes=[b_wgtf])

            if dbg and n == 0:
                for nm, t_, b_, shp, dt_ in (("eidi", eidi, b_eidi, [128, 128], I32),
                                             ("wgt", wgtf, b_wgtf, [128, 128], F32),
                                             ("h2f", h2b, b_h2b, [128, D], BF16),
                                             ("S", S, b_S, [128, 16, 128], F32)):
                    od = dbg_out(nm, shp, dt_)
                    P.dma("sp", lambda e, od=od, t_=t_: e.dma_start(out=od, in_=t_[:]), "dbg_" + nm, reads=[b_])
                    out_bufs.append(b_)

        def back(n):
            xt, b_xt = xs[n % 3]
            cur = n % 2
            h2b, b_h2b = h2bs[n % 3]
            eidi, b_eidi = eidis[cur]
            wgtf, b_wgtf = wgtfs[cur]
            LAG = 2
            DOT_SPLIT = dot_split

            def stage_a(k):
                g_t, g_b = gb[k % NG]
                b_a2 = a2_bufs[k % 8]
                b_ga2 = ga2_bufs[k % 8]
                P.dma("pool", lambda e, k=k, g_t=g_t: e.indirect_dma_start(
                    out=g_t[:], out_offset=None, in_=uvb_d,
                    in_offset=bass.IndirectOffsetOnAxis(ap=eidi[:, k:k + 1], axis=0),
                    bounds_check=bc_reg(e), oob_is_err=False), "g%d" % (k % NG),
                    reads=[b_eidi] + b_uvb_all, writes=[g_b])
                if k % DOT_SPLIT == DOT_SPLIT - 1:
                    P.op("dve", lambda e, k=k, g_t=g_t: e.scalar_tensor_tensor(
                        out=g_t[:, 0:D], in0=g_t[:, 0:D], scalar=1.0, in1=h2b[:], op0=ALU.mult, op1=ALU.mult,
                        accum_out=a2[:, k:k + 1]), reads=[g_b, b_h2b], writes=[g_b, b_a2])
                else:
                    P.op("dve", lambda e, k=k, g_t=g_t: e.tensor_tensor(
                        out=g_t[:, 0:D], in0=g_t[:, 0:D], in1=h2b[:], op=ALU.mult),
                        reads=[g_b, b_h2b], writes=[g_b])
                    P.op("act", lambda e, k=k, g_t=g_t: e.activation(
                        out=g_t[:, 0:D], in_=g_t[:, 0:D], func=AF.Copy, accum_out=a2[:, k:k + 1]),
                        reads=[g_b], writes=[g_b, b_a2])
                P.op("act", lambda e, k=k: e.activation(
                    out=ga2[:, k:k + 1], in_=a2[:, k:k + 1], func=AF.Gelu_apprx_tanh),
                    reads=[b_a2], writes=[b_ga2])

            def stage_b(k):
                g_t, g_b = gb[k % NG]
                d_t, d_b = dg[k % 2]
                b_ga2 = ga2_bufs[k % 8]
                P.op("dve", lambda e, k=k, d_t=d_t: e.tensor_scalar(
                    out=d_t[:], in0=ident[:], scalar1=ga2[:, k:k + 1], scalar2=wgtf[:, k:k + 1],
                    op0=ALU.mult, op1=ALU.mult), reads=[b_ident, b_ga2, b_wgtf], writes=[d_b])
                for half, (ap_, ab_) in enumerate(((acc0, b_acc0), (acc1, b_acc1))):
                    P.op("pe", lambda e, k=k, half=half, ap_=ap_, d_t=d_t, g_t=g_t: e.matmul(
                        ap_[:, :], lhsT=d_t[:], rhs=g_t[:, D + half * 512:D + (half + 1) * 512],
                        start=(k == 0), stop=(k == 127)), reads=[d_b, g_b], writes=[ab_])

            for k in range(128 + LAG):
                if k < 128:
                    stage_a(k)
                if k >= LAG:
                    stage_b(k - LAG)
            for half, (ap_, ab_) in enumerate(((acc0, b_acc0), (acc1, b_acc1))):
                P.op("dve", lambda e, half=half, ap_=ap_: e.tensor_tensor(
                    out=xt[:, half * 512:(half + 1) * 512], in0=ap_[:, :],
                    in1=xt[:, half * 512:(half + 1) * 512], op=ALU.add),
                    reads=[ab_, b_xt], writes=[b_xt])
            P.dma("sp", lambda e: e.dma_start(out=y_d[n * T:(n + 1) * T, :], in_=xt[:]),
                  "y%d" % (n % 3), reads=[b_xt])
            out_bufs.append(b_xt)

        def run_window(recs):
            ops = []
            for r in recs:
                ops += r
            if sched:
                P.commit_scheduled(ops)
            else:
                for it in ops:
                    P.commit(it)

        run_window([P.record(head, 0)])
        run_window([P.record(tail, 0)] + ([P.record(head, 1)] if n_tiles > 1 else []))
        for n in range(n_tiles):
            recs = []
            if skip != "back":
                recs.append(P.record(back, n))
            if n + 1 < n_tiles:
                recs.append(P.record(tail, n + 1))
            if n + 2 < n_tiles:
                recs.append(P.record(head, n + 2))
            run_window(recs)

        P.final_wait("sp", out_bufs + tap_bufs)
        P.emit()
    return nc, list(dbg_d.keys())


def _t5_bucket_static():
    qi = np.arange(128)[:, None]
    kj = np.arange(256)[None, :]
    dist = qi + 128 - kj
    valid = (dist >= 0) & (dist < 128)
    d0 = np.maximum(dist, 0)
    max_exact = 16
    dd = np.maximum(d0, 1).astype(np.float32)
    large = max_exact + (np.log(dd / np.float32(max_exact)) / np.float32(math.log(128 / max_exact))
                         * np.float32(32 - max_exact)).astype(np.int32)
    large = np.minimum(large, 31)
    bucket = np.where(d0 < max_exact, d0, large)
    ohs = np.zeros((32, 128, 256), np.float32)
    for b in range(32):
        ohs[b] = ((bucket == b) & valid).astype(np.float32)
    negmask = np.where(valid, 0.0, NEG).astype(np.float32)
    return ohs, negmask


def _host_layout(inp, n_tok=SEQ):
    f = lambda a: np.ascontiguousarray(np.asarray(a, dtype=np.float32))
    ohs, negmask = _t5_bucket_static()
    bones = np.zeros((128, 128), np.float32)
    bones[:64, :64] = 1.0
    bones[64:, 64:] = 1.0
    keys = f(inp["peer_keys"])[0]
    keysT = np.ascontiguousarray(keys.transpose(3, 1, 0, 2)).reshape(128, 16 * 128)
    uv = np.ascontiguousarray(np.concatenate([f(inp["peer_u"])[0], f(inp["peer_v"])[0]], axis=1))
    shared = {
        "w_ada": f(inp["w_ada"])[0],
        "b_ada": f(inp["b_ada"]).reshape(1, 6 * D),
        "norm1_g": f(inp["norm1_g"]).reshape(1, D),
        "norm2_g": f(inp["norm2_g"]).reshape(1, D),
        "w_in": f(inp["w_in"])[0],
        "conv_w_t": np.ascontiguousarray(f(inp["conv_w"])[0].reshape(3, 4, 128).transpose(2, 1, 0)).reshape(128, 12),
        "qg_t": np.ascontiguousarray(np.tile(f(inp["q_norm_g"])[0], 2).reshape(128, 1)),
        "kg_t": np.ascontiguousarray(np.tile(f(inp["k_norm_g"])[0], 2).reshape(128, 1)),
        "sinks": f(inp["sinks"]).reshape(1, 8),
        "rel_bias": f(inp["rel_bias"]).reshape(1, 256),
        "conv_g_t": np.ascontiguousarray(f(inp["conv_out_g"])[0].reshape(4, 128).T),
        "attn_g_t": np.ascontiguousarray(f(inp["attn_out_g"])[0].reshape(4, 128).T),
        "w_out": f(inp["w_out"])[0],
        "peer_wq": f(inp["peer_wq"])[0],
        "keysT": keysT,
        "uv": uv,
        "ident": np.eye(128, dtype=np.float32),
        "blockones": bones,
        "iota16": np.ascontiguousarray(np.tile(np.arange(16, dtype=np.float32), (128, 1))),
        "ohs": ohs,
        "negmask": negmask,
    }
    x = f(inp["x"])
    c = f(inp["c"])
    maps = []
    for b in range(x.shape[0]):
        m = dict(shared)
        m["x"] = np.ascontiguousarray(x[b, :SEQ])
        m["c_t"] = np.ascontiguousarray(c[b].reshape(8, 128).T)
        maps.append(m)
    return maps


def kernel(**inputs):
    maps = _host_layout(inputs)
    nc, _ = build(n_tiles=SEQ // T)
    res = run_bass_kernel_spmd(nc, maps, core_ids=list(range(8)))
    out = np.stack([np.asarray(r["y"], dtype=np.float32) for r in res.results], axis=0)
    return out
```
